# Optimizing a Trainium2 kernel written in Bass

```python
import jax, jax.numpy as jnp
from jax import lax
import numpy as np

D_MODEL = 1024
BATCH = 4
SEQ = 8192
DEPTH = 2

N_HEADS = 16
N_KV_HEADS = 4
HEAD_DIM = D_MODEL // N_HEADS
GROUP = N_HEADS // N_KV_HEADS
QKV_DIM = (N_HEADS + 2 * N_KV_HEADS) * HEAD_DIM
ROT_DIM = HEAD_DIM // 4
ROPE_THETA = 500000.0
MOBA_BLOCK = 256
MOBA_TOPK = 3
MOBA_QCHUNK = 16
SWA_WINDOW = 128
D_FF = 3584
N_EXPERTS = 8
TOP_K = 2
PLE_DIM = 256
LN_EPS = 1e-5
N_EVEN = (DEPTH + 1) // 2
N_ODD = DEPTH // 2

kernel_name = "moba_swa_sink_deepnorm_moe_hybrid"


def rope_tables(positions):
    inv = 1.0 / (ROPE_THETA ** (jnp.arange(0, ROT_DIM, 2, dtype=jnp.float32) / ROT_DIM))
    ang = positions.astype(jnp.float32)[..., None] * inv
    return jnp.cos(ang), jnp.sin(ang)


def apply_partial_rope(x, cos, sin):
    half = ROT_DIM // 2
    c = cos[:, :, None, :].astype(x.dtype)
    s = sin[:, :, None, :].astype(x.dtype)
    x1, x2, xp = x[..., :half], x[..., half:ROT_DIM], x[..., ROT_DIM:]
    return jnp.concatenate([x1 * c - x2 * s, x2 * c + x1 * s, xp], axis=-1)


def layer_norm(x, g, b):
    xf = x.astype(jnp.float32)
    mu = jnp.mean(xf, axis=-1, keepdims=True)
    var = jnp.mean(jnp.square(xf - mu), axis=-1, keepdims=True)
    return ((xf - mu) * lax.rsqrt(var + LN_EPS) * g.astype(jnp.float32) + b.astype(jnp.float32)).astype(x.dtype)


def project_qkv(x, w_qkv, cos, sin):
    B, S, _ = x.shape
    qkv = x @ w_qkv
    nq = N_HEADS * HEAD_DIM
    nk = N_KV_HEADS * HEAD_DIM
    q = qkv[..., :nq].reshape(B, S, N_HEADS, HEAD_DIM)
    k = qkv[..., nq:nq + nk].reshape(B, S, N_KV_HEADS, HEAD_DIM)
    v = qkv[..., nq + nk:].reshape(B, S, N_KV_HEADS, HEAD_DIM)
    return apply_partial_rope(q, cos, sin), apply_partial_rope(k, cos, sin), v


def moba_attention(q, k, v):
    B, S, H, Dh = q.shape
    nb = -(-S // MOBA_BLOCK)
    S_pad = nb * MOBA_BLOCK
    pad = ((0, 0), (0, S_pad - S), (0, 0), (0, 0))
    q = jnp.pad(q * (HEAD_DIM ** -0.5), pad)
    k = jnp.pad(k, pad)
    v = jnp.pad(v, pad)
    kb = k.reshape(B, nb, MOBA_BLOCK, N_KV_HEADS, Dh).transpose(0, 1, 3, 2, 4)
    vb = v.reshape(B, nb, MOBA_BLOCK, N_KV_HEADS, Dh).transpose(0, 1, 3, 2, 4)
    kmean = jnp.mean(kb.astype(jnp.float32), axis=3)
    qg = q.reshape(B, S_pad, N_KV_HEADS, GROUP, Dh).astype(jnp.float32)
    gate = jnp.einsum('bskgd,bnkd->bskgn', qg, kmean).reshape(B, S_pad, H, nb)
    qblk = jnp.arange(S_pad) // MOBA_BLOCK
    past = jnp.arange(nb)[None, :] < qblk[:, None]
    gate = jnp.where(past[None, :, None, :], gate, -jnp.inf)
    n_sel = min(MOBA_TOPK, nb)
    _, sel = lax.top_k(gate, n_sel)

    QC = MOBA_QCHUNK
    NC = S_pad // QC
    q_c = q.reshape(B, NC, QC, H, Dh).transpose(1, 0, 2, 3, 4)
    sel_c = sel.reshape(B, NC, QC, H, n_sel).transpose(1, 0, 2, 3, 4)
    b_idx = jnp.arange(B)[:, None, None, None]
    g_idx = (jnp.arange(H) // GROUP)[None, None, :, None]

    def chunk(args):
        qc, selc, cid = args
        t = cid * QC + jnp.arange(QC)
        c = (cid * QC) // MOBA_BLOCK
        kg = kb[b_idx, selc, g_idx]
        vg = vb[b_idx, selc, g_idx]
        lp = jnp.einsum('bqhd,bqhnjd->bqhnj', qc, kg).astype(jnp.float32)
        valid = jnp.arange(n_sel)[None, :] < (t // MOBA_BLOCK)[:, None]
        lp = jnp.where(valid[None, :, None, :, None], lp, -jnp.inf).reshape(B, QC, H, n_sel * MOBA_BLOCK)
        ko = lax.dynamic_index_in_dim(kb, c, axis=1, keepdims=False)
        vo = lax.dynamic_index_in_dim(vb, c, axis=1, keepdims=False)
        qcg = qc.reshape(B, QC, N_KV_HEADS, GROUP, Dh)
        lo = jnp.einsum('bqkgd,bkjd->bqkgj', qcg, ko).astype(jnp.float32).reshape(B, QC, H, MOBA_BLOCK)
        kpos = c * MOBA_BLOCK + jnp.arange(MOBA_BLOCK)
        lo = jnp.where((kpos[None, :] <= t[:, None])[None, :, None, :], lo, -jnp.inf)
        probs = jax.nn.softmax(jnp.concatenate([lp, lo], axis=-1), axis=-1)
        pp = probs[..., :n_sel * MOBA_BLOCK].reshape(B, QC, H, n_sel, MOBA_BLOCK).astype(v.dtype)
        po = probs[..., n_sel * MOBA_BLOCK:].reshape(B, QC, N_KV_HEADS, GROUP, MOBA_BLOCK).astype(v.dtype)
        out = jnp.einsum('bqhnj,bqhnjd->bqhd', pp, vg) + \
            jnp.einsum('bqkgj,bkjd->bqkgd', po, vo).reshape(B, QC, H, Dh)
        return out

    out = lax.map(chunk, (q_c, sel_c, jnp.arange(NC)))
    return out.transpose(1, 0, 2, 3, 4).reshape(B, S_pad, H, Dh)[:, :S]


def sliding_window_attention(q, k, v, sinks):
    B, S, H, Dh = q.shape
    W = SWA_WINDOW
    n = S // W
    qb = (q * (HEAD_DIM ** -0.5)).reshape(B, n, W, N_KV_HEADS, GROUP, Dh)
    kb = k.reshape(B, n, W, N_KV_HEADS, Dh)
    vb = v.reshape(B, n, W, N_KV_HEADS, Dh)
    zpad = ((0, 0), (1, 0), (0, 0), (0, 0), (0, 0))
    kk = jnp.concatenate([jnp.pad(kb, zpad)[:, :-1], kb], axis=2)
    vv = jnp.concatenate([jnp.pad(vb, zpad)[:, :-1], vb], axis=2)
    logits = jnp.einsum('bnqkgd,bnjkd->bnkgqj', qb, kk).astype(jnp.float32)
    rel = (jnp.arange(W)[:, None] + W) - jnp.arange(2 * W)[None, :]
    band = (rel >= 0) & (rel < W)
    first = (jnp.arange(n) == 0)[:, None, None] & (jnp.arange(2 * W) < W)[None, None, :]
    mask = band[None] & ~first
    logits = jnp.where(mask[None, :, None, None, :, :], logits, -jnp.inf)
    sink = sinks.astype(jnp.float32).reshape(1, 1, N_KV_HEADS, GROUP, 1, 1)
    m = jnp.maximum(jnp.max(logits, axis=-1, keepdims=True), sink)
    e = jnp.exp(logits - m)
    probs = e / (jnp.sum(e, axis=-1, keepdims=True) + jnp.exp(sink - m))
    out = jnp.einsum('bnkgqj,bnjkd->bnqkgd', probs.astype(v.dtype), vv)
    return out.reshape(B, S, H, Dh)


def swiglu(x, w_gate, w_up, w_down):
    return (jax.nn.silu(x @ w_gate) * (x @ w_up)) @ w_down


def moe_swiglu(x, w_router, w_gate, w_up, w_down):
    logits = (x @ w_router).astype(jnp.float32)
    top_v, top_i = lax.top_k(logits, TOP_K)
    top_w = jax.nn.softmax(top_v, axis=-1)
    gates = jnp.sum(jax.nn.one_hot(top_i, N_EXPERTS, dtype=jnp.float32) * top_w[..., None], axis=-2)
    out = jnp.zeros_like(x)
    for e in range(N_EXPERTS):
        out = out + gates[..., e:e + 1].astype(x.dtype) * swiglu(x, w_gate[e], w_up[e], w_down[e])
    return out


def setup_inputs(seed: int = 0) -> dict:
    key = jax.random.key(seed)
    ks = jax.random.split(key, 20)
    f32 = jnp.float32
    beta = (8.0 * DEPTH) ** -0.25

    def nrm(k, shape, fan_in, scale=1.0):
        return jax.random.normal(k, shape, f32) * (scale * fan_in ** -0.5)

    x = jax.random.normal(ks[0], (BATCH, SEQ, D_MODEL), f32)
    p = jax.random.normal(ks[1], (DEPTH, BATCH, SEQ, PLE_DIM), f32)
    positions = jnp.broadcast_to(jnp.arange(SEQ, dtype=jnp.int32)[None, :], (BATCH, SEQ))
    w_qkv = nrm(ks[2], (DEPTH, D_MODEL, QKV_DIM), D_MODEL)
    w_o = nrm(ks[3], (DEPTH, N_HEADS * HEAD_DIM, D_MODEL), N_HEADS * HEAD_DIM, beta)
    ln_mix_g = 1.0 + 0.05 * jax.random.normal(ks[4], (DEPTH, D_MODEL), f32)
    ln_mix_b = 0.02 * jax.random.normal(ks[5], (DEPTH, D_MODEL), f32)
    ln_ffn_g = 1.0 + 0.05 * jax.random.normal(ks[6], (DEPTH, D_MODEL), f32)
    ln_ffn_b = 0.02 * jax.random.normal(ks[7], (DEPTH, D_MODEL), f32)
    sinks = 0.5 * jax.random.normal(ks[8], (N_ODD, N_HEADS), f32)
    w_ffn_gate = nrm(ks[9], (N_EVEN, D_MODEL, D_FF), D_MODEL)
    w_ffn_up = nrm(ks[10], (N_EVEN, D_MODEL, D_FF), D_MODEL)
    w_ffn_down = nrm(ks[11], (N_EVEN, D_FF, D_MODEL), D_FF, beta)
    w_router = nrm(ks[12], (N_ODD, D_MODEL, N_EXPERTS), D_MODEL)
    w_exp_gate = nrm(ks[13], (N_ODD, N_EXPERTS, D_MODEL, D_FF), D_MODEL)
    w_exp_up = nrm(ks[14], (N_ODD, N_EXPERTS, D_MODEL, D_FF), D_MODEL)
    w_exp_down = nrm(ks[15], (N_ODD, N_EXPERTS, D_FF, D_MODEL), D_FF, beta)
    w_ple_proj = nrm(ks[16], (DEPTH, PLE_DIM, D_MODEL), PLE_DIM, 0.5)
    w_ple_gate = nrm(ks[17], (DEPTH, D_MODEL, D_MODEL), D_MODEL)
    return {"x": x, "p": p, "positions": positions, "w_qkv": w_qkv, "w_o": w_o,
            "ln_mix_g": ln_mix_g, "ln_mix_b": ln_mix_b, "ln_ffn_g": ln_ffn_g, "ln_ffn_b": ln_ffn_b,
            "sinks": sinks, "w_ffn_gate": w_ffn_gate, "w_ffn_up": w_ffn_up, "w_ffn_down": w_ffn_down,
            "w_router": w_router, "w_exp_gate": w_exp_gate, "w_exp_up": w_exp_up, "w_exp_down": w_exp_down,
            "w_ple_proj": w_ple_proj, "w_ple_gate": w_ple_gate}


def reference(x, p, positions, w_qkv, w_o, ln_mix_g, ln_mix_b, ln_ffn_g, ln_ffn_b, sinks,
              w_ffn_gate, w_ffn_up, w_ffn_down, w_router, w_exp_gate, w_exp_up, w_exp_down,
              w_ple_proj, w_ple_gate):
    alpha = (2.0 * DEPTH) ** 0.25
    B, S, _ = x.shape
    cos, sin = rope_tables(positions)
    for i in range(DEPTH):
        j = i // 2
        q, k, v = project_qkv(x, w_qkv[i], cos, sin)
        if i % 2 == 0:
            a = moba_attention(q, k, v)
        else:
            a = sliding_window_attention(q, k, v, sinks[j])
        x = layer_norm(alpha * x + a.reshape(B, S, N_HEADS * HEAD_DIM) @ w_o[i], ln_mix_g[i], ln_mix_b[i])
        if i % 2 == 0:
            f = swiglu(x, w_ffn_gate[j], w_ffn_up[j], w_ffn_down[j])
        else:
            f = moe_swiglu(x, w_router[j], w_exp_gate[j], w_exp_up[j], w_exp_down[j])
        x = layer_norm(alpha * x + f, ln_ffn_g[i], ln_ffn_b[i])
        x = x + (p[i] @ w_ple_proj[i]) * jax.nn.sigmoid(x @ w_ple_gate[i])
    return x
```

```python
from contextlib import ExitStack
import os
import numpy as np
import concourse.bass as bass
import concourse.mybir as mybir
from concourse.bass_utils import run_bass_kernel_spmd

F32 = mybir.dt.float32
BF16 = mybir.dt.bfloat16
I32 = mybir.dt.int32
AF = mybir.ActivationFunctionType
ALU = mybir.AluOpType
AX = mybir.AxisListType

D = 1024
SEQ = 8192
HALF = 4096
NT = 33
NTOK = NT * 128
DFF = 3584
NFB = 7
ALPHA = 4.0 ** 0.25
EPS = 1e-5
TWO_PI = 6.283185307179586
NEG = -30000.0


class Trk:
    def __init__(self, nc, es):
        self.nc = nc
        self.es = es
        self.eng = {"pe": nc.tensor, "act": nc.scalar, "dve": nc.vector, "pool": nc.gpsimd, "sp": nc.sync}
        self.sem = {}
        self.cnt = {}
        self.seen = {e: {} for e in self.eng}
        self.lw = {}
        self.rd = {}

    def _sem(self, k):
        if k not in self.sem:
            self.sem[k] = self.es.enter_context(self.nc.semaphore("s%d" % len(self.sem)))
            self.cnt[k] = 0
        return self.sem[k]

    def _waits(self, e, reads, writes, own_sk=None):
        need = {}

        def add(sk, v, raw):
            if sk == e and not raw:
                return
            if sk == own_sk and not raw:
                return
            if v > need.get(sk, 0):
                need[sk] = v

        for k in reads:
            if k in self.lw:
                add(self.lw[k][0], self.lw[k][1], True)
        for k in writes:
            if k in self.lw:
                add(self.lw[k][0], self.lw[k][1], False)
            for sk, v in self.rd.get(k, {}).items():
                add(sk, v, False)
        for sk, v in need.items():
            if self.seen[e].get(sk, 0) < v:
                self.eng[e].wait_ge(self.sem[sk], v)
                self.seen[e][sk] = v

    def _record(self, sk, v, reads, writes):
        for k in reads:
            d = self.rd.setdefault(k, {})
            if d.get(sk, 0) < v:
                d[sk] = v
        for k in writes:
            self.lw[k] = (sk, v)
            self.rd[k] = {}

    def op(self, e, fn, reads=(), writes=()):
        self._sem(e)
        extra = [k for k in reads if isinstance(k, str) and k.startswith("ps") and k not in writes]
        if extra:
            writes = list(writes) + extra
        self._waits(e, reads, writes)
        ins = fn(self.eng[e])
        ins.then_inc(self.sem[e], 1)
        self.cnt[e] += 1
        self._record(e, self.cnt[e], reads, writes)

    def dma(self, e, out, in_, reads=(), writes=(), sk=None):
        if sk is None:
            w0 = writes[0]
            if isinstance(w0, str) and (w0[0].isupper() or w0.startswith("dbg") or w0 == "out"):
                sk = ("st", reads[0])
            else:
                sk = ("d", w0)
        self._sem(sk)
        self._waits(e, reads, writes, own_sk=sk)
        ins = self.eng[e].dma_start(out=out, in_=in_)
        ins.then_inc(self.sem[sk], 16)
        self.cnt[sk] += 16
        self._record(sk, self.cnt[sk], reads, writes)

    def barrier(self):
        for e in self.eng:
            for sk, v in self.cnt.items():
                if v > 0 and self.seen[e].get(sk, 0) < v:
                    self.eng[e].wait_ge(self.sem[sk], v)
                    self.seen[e][sk] = v
        self.lw = {}
        self.rd = {}

    def final_wait(self, e, key):
        sk = ("d", key)
        self.eng[e].wait_ge(self.sem[sk], self.cnt[sk])


def mm(trk, out, pairs, reads, writes):
    def fn(pe):
        n = len(pairs)
        ins = None
        for i, (l, r) in enumerate(pairs):
            ins = pe.matmul(out, l, r, start=(i == 0), stop=(i == n - 1))
        return ins
    trk.op("pe", fn, reads, writes)


def transposes(trk, outs_ins, ident, reads, writes):
    def fn(pe):
        ins = None
        for o, i in outs_ins:
            ins = pe.transpose(o, i, ident)
        return ins
    trk.op("pe", fn, reads, writes)


def layer_norm(trk, P, y, yk, g_sb, b_sb, out, outk, slot):
    st6 = P["st6"][slot]
    mv = P["mv"][slot]
    k6 = "st6_%d" % slot
    kmv = "mv_%d" % slot
    krs = "rs_%d" % slot
    trk.op("dve", lambda v: v.bn_stats(out=st6[:, 0:6], in_=y[:, 0:512]), [yk], [k6 + "a"])
    trk.op("dve", lambda v: v.bn_stats(out=st6[:, 6:12], in_=y[:, 512:1024]), [yk], [k6 + "b"])
    trk.op("dve", lambda v: v.bn_aggr(out=mv[:, 0:2], in_=st6[:, 0:12]), [k6 + "a", k6 + "b"], [kmv])
    trk.op("act", lambda a: a.activation(out=mv[:, 2:3], in_=mv[:, 1:2], func=AF.Sqrt, bias=EPS), [kmv], [krs + "s"])
    trk.op("dve", lambda v: v.reciprocal(out=mv[:, 3:4], in_=mv[:, 2:3]), [krs + "s"], [krs])
    trk.op("dve", lambda v: v.tensor_scalar(out=out[:, :], in0=y[:, :], scalar1=mv[:, 0:1], scalar2=mv[:, 3:4],
                                            op0=ALU.subtract, op1=ALU.mult), [yk, kmv, krs], [outk])
    trk.op("dve", lambda v: v.tensor_tensor(out=out[:, :], in0=out[:, :], in1=g_sb[:, :], op=ALU.mult), [outk, "lng"], [outk])
    trk.op("dve", lambda v: v.tensor_tensor(out=out[:, :], in0=out[:, :], in1=b_sb[:, :], op=ALU.add), [outk, "lnb"], [outk])


def build_program(stop_after=None, debug=False):
    nc = bass.Bass("TRN2", target_bir_lowering=False)
    es = ExitStack()
    trk = Trk(nc, es)

    def din(name, shape, dt=F32):
        return nc.dram_tensor(name, list(shape), dt, kind="ExternalInput").ap()

    def dscr(name, shape, dt):
        kind = "ExternalOutput" if debug else "Internal"
        return nc.dram_tensor(name, list(shape), dt, kind=kind).ap()

    xin = din("xin", [SEQ, D])
    pos = din("pos", [1, SEQ], I32)
    p0 = din("p0", [NTOK, 256])
    p1 = din("p1", [HALF, 256])
    cst64 = din("cst64", [64, 4])
    pmat_d = din("pmat", [64, 64])
    ident_d = din("ident", [128, 128])
    blkind = din("blkind", [32, SEQ])
    tri_d = din("tri", [128, 128])
    tric_d = din("tric", [128, 128])
    gbias_d = din("gbias", [1, 17 * 32])
    cb_d = din("cb", [1, 17 * 32])
    fv_d = din("fv", [128, 1])
    w_qkv = din("w_qkv", [2, D, 1536])
    w_o = din("w_o", [2, D, D])
    ln_mix_g = din("ln_mix_g", [2, D])
    ln_mix_b = din("ln_mix_b", [2, D])
    ln_ffn_g = din("ln_ffn_g", [2, D])
    ln_ffn_b = din("ln_ffn_b", [2, D])
    sinks = din("sinks", [1, 16])
    w_ffn_gate = din("w_ffn_gate", [1, D, DFF])
    w_ffn_up = din("w_ffn_up", [1, D, DFF])
    w_ffn_down = din("w_ffn_down", [1, DFF, D])
    w_router = din("w_router", [1, D, 8])
    if stop_after is None or stop_after >= 5:
        w_exp_gate = din("w_exp_gate", [1, 8, D, DFF])
        w_exp_up = din("w_exp_up", [1, 8, D, DFF])
        w_exp_down = din("w_exp_down", [1, 8, DFF, D])
    w_ple_proj = din("w_ple_proj", [2, 256, D])
    w_ple_gate = din("w_ple_gate", [2, D, D])
    out_d = nc.dram_tensor("out", [HALF, D], F32, kind="ExternalOutput").ap()

    CT = dscr("CT", [64, SEQ], F32)
    ST = dscr("ST", [64, SEQ], F32)
    QA = dscr("QA", [96, NT, 16 * 128], BF16)
    X1 = dscr("X1", [NTOK, D], F32)
    X1T = dscr("X1T", [128, 8, NTOK], BF16)
    X3 = dscr("X3", [NTOK, D], F32)
    X3T = dscr("X3T", [128, 8, NTOK], BF16)
    Q1 = dscr("Q1", [64, 32, 16 * 128], BF16)
    X4 = dscr("X4", [HALF, D], F32)
    X4T = dscr("X4T", [128, 8, HALF], BF16)
    GATES = dscr("GATES", [128, 32, 8], F32)

    uid = [0]

    def sb(stk, name, shape, dt):
        uid[0] += 1
        return stk.enter_context(nc.sbuf_tensor("s%d_%s" % (uid[0], name), list(shape), dt))

    def ps(stk, name, shape, dt=F32):
        uid[0] += 1
        return stk.enter_context(nc.psum_tensor("p%d_%s" % (uid[0], name), list(shape), dt))

    def done():
        trk.barrier()

    c64 = sb(es, "c64", [64, 4], F32)
    pm = sb(es, "pm", [64, 64], BF16)
    identb = sb(es, "identb", [128, 128], BF16)
    identf = sb(es, "identf", [128, 128], F32)
    trib = sb(es, "trib", [128, 128], BF16)
    tricb = sb(es, "tricb", [128, 128], BF16)
    tricfv = sb(es, "tricfv", [128, 128], BF16)
    fv = sb(es, "fv", [128, 1], F32)
    trk.dma("sp", c64[:, :], cst64[:, :], [], ["c64"])
    trk.dma("pool", pm[:, :], pmat_d[:, :], [], ["pm"])
    trk.dma("pool", identb[:, :], ident_d[:, :], [], ["identb"])
    trk.dma("sp", identf[:, :], ident_d[:, :], [], ["identf"])
    trk.dma("pool", trib[:, :], tri_d[:, :], [], ["trib"])
    trk.dma("pool", tricb[:, :], tric_d[:, :], [], ["tricb"])
    trk.dma("sp", fv[:, :], fv_d[:, :], [], ["fv"])
    trk.op("dve", lambda v: v.tensor_scalar(out=tricfv[:, :], in0=tricb[:, :], scalar1=fv[:, 0:1], scalar2=None,
                                            op0=ALU.mult), ["tricb", "fv"], ["tricfv"])

    if stop_after == -1:
        done()
        return nc, es, trk
    with ExitStack() as ph:
        CW = 2048
        pi = [sb(ph, "r_pi%d" % i, [64, CW], I32) for i in range(2)]
        u = [sb(ph, "r_u%d" % i, [64, CW], F32) for i in range(2)]
        ni = [sb(ph, "r_ni%d" % i, [64, CW], I32) for i in range(2)]
        nf = [sb(ph, "r_nf%d" % i, [64, CW], F32) for i in range(2)]
        to = [sb(ph, "r_to%d" % i, [64, CW], F32) for i in range(2)]
        it = 0
        for c in range(SEQ // CW):
            cols = slice(c * CW, (c + 1) * CW)
            s = c % 2
            trk.dma("sp", pi[s][:, :], pos[0:1, cols].partition_broadcast(64), [], ["r_pi%d" % s])
            trk.op("dve", lambda v: v.tensor_copy(out=u[s][:, :], in_=pi[s][:, :]), ["r_pi%d" % s], ["r_u%d" % s])
            trk.op("dve", lambda v: v.tensor_scalar(out=u[s][:, :], in0=u[s][:, :], scalar1=c64[:, 0:1], scalar2=None,
                                                    op0=ALU.mult), ["r_u%d" % s, "c64"], ["r_u%d" % s])
            for which in range(2):
                k = it % 2
                it += 1
                if which == 1:
                    trk.op("dve", lambda v: v.tensor_scalar(out=u[s][:, :], in0=u[s][:, :], scalar1=0.25, scalar2=None,
                                                            op0=ALU.add), ["r_u%d" % s], ["r_u%d" % s])
                trk.op("dve", lambda v: v.tensor_copy(out=ni[k][:, :], in_=u[s][:, :]), ["r_u%d" % s], ["r_ni%d" % k])
                trk.op("dve", lambda v: v.tensor_copy(out=nf[k][:, :], in_=ni[k][:, :]), ["r_ni%d" % k], ["r_nf%d" % k])
                trk.op("dve", lambda v: v.tensor_tensor(out=nf[k][:, :], in0=u[s][:, :], in1=nf[k][:, :], op=ALU.subtract),
                       ["r_u%d" % s, "r_nf%d" % k], ["r_nf%d" % k])
                PH0 = int(os.environ.get("PH0", "9"))
                if PH0 == 1:
                    trk.dma("sp", (ST if which == 0 else CT)[:, cols], nf[k][:, :], ["r_nf%d" % k], ["ST" if which == 0 else "CT"])
                    continue
                if PH0 == 2:
                    trk.op("act", lambda a: a.activation(out=to[k][:, :], in_=nf[k][:, :], func=AF.Sin, scale=TWO_PI),
                           ["r_nf%d" % k], ["r_to%d" % k])
                    trk.dma("sp", (ST if which == 0 else CT)[:, cols], to[k][:, :], ["r_to%d" % k], ["ST" if which == 0 else "CT"])
                    continue
                if which == 0:
                    trk.op("dve", lambda v: v.tensor_scalar(out=nf[k][:, :], in0=nf[k][:, :], scalar1=c64[:, 2:3], scalar2=None,
                                                            op0=ALU.mult), ["r_nf%d" % k, "c64"], ["r_nf%d" % k])
                    trk.op("act", lambda a: a.activation(out=to[k][:, :], in_=nf[k][:, :], func=AF.Sin, scale=TWO_PI),
                           ["r_nf%d" % k], ["r_to%d" % k])
                    trk.dma("sp", ST[:, cols], to[k][:, :], ["r_to%d" % k], ["ST"])
                else:
                    trk.op("act", lambda a: a.activation(out=to[k][:, :], in_=nf[k][:, :], func=AF.Sin, scale=TWO_PI),
                           ["r_nf%d" % k], ["r_to%d" % k])
                    trk.dma("sp", CT[:, cols], to[k][:, :], ["r_to%d" % k], ["CT"])
        done()
    if stop_after == 0:
        return nc, es, trk

    def rope_head(P, psrc, psk, ncols, scale, ct_ap, st_ap, ctk, out_ap, outk, slot):
        t1 = P["t1"][slot]
        t2 = P["t2"][slot]
        krb = P["krb"][slot]
        psP = P["psP"][slot]
        k1, k2, kk, kp = "t1_%d" % slot, "t2_%d" % slot, "krb_%d" % slot, "psP_%d" % slot
        trk.op("dve", lambda v: v.scalar_tensor_tensor(out=t1[:, 0:ncols], in0=psrc, scalar=scale, in1=ct_ap,
                                                       op0=ALU.mult, op1=ALU.mult), [psk, ctk], [k1])
        trk.op("act", lambda a: a.activation(out=krb[:, 0:ncols], in_=psrc, func=AF.Copy, scale=scale), [psk], [kk])
        mm(trk, psP[:, 0:ncols], [(pm[:, :], krb[:, 0:ncols])], [kk, "pm"], [kp])
        trk.op("dve", lambda v: v.tensor_tensor(out=t2[:, 0:ncols], in0=psP[:, 0:ncols], in1=st_ap, op=ALU.mult),
               [kp, ctk], [k2])
        trk.op("dve", out_ap_writer(out_ap, t1, t2, ncols), [k1, k2], [outk])

    def out_ap_writer(out_ap, t1, t2, ncols):
        nd = len(out_ap.shape)
        if nd == 2:
            return lambda v: v.tensor_tensor(out=out_ap, in0=t1[:, 0:ncols], in1=t2[:, 0:ncols], op=ALU.add)
        nsub = out_ap.shape[1]
        a = t1[:, 0:ncols].rearrange("p (s c) -> p s c", s=nsub)
        b = t2[:, 0:ncols].rearrange("p (s c) -> p s c", s=nsub)
        return lambda v: v.tensor_tensor(out=out_ap, in0=a, in1=b, op=ALU.add)

    def load_w(eng, dst, src, key, nsplit=1):
        v = src.rearrange("(k p) n -> p k n", p=128)
        kc = v.shape[1]
        step = kc // nsplit
        for i in range(nsplit):
            trk.dma(eng, dst[:, i * step:(i + 1) * step, :], v[:, i * step:(i + 1) * step, :], [], [key], sk=("d", key))

    with ExitStack() as kv:
        kaug = sb(kv, "kaug", [96, 4, SEQ], BF16)
        vaug = sb(kv, "vaug", [128, 64, 4, 128], BF16)
        with ExitStack() as ph:
            wqkv = sb(ph, "wqkv", [128, 8, 1536], BF16)
            load_w("pool", wqkv, w_qkv[0], "wqkv", nsplit=4)
            for kvh in range(4):
                trk.dma("pool", kaug[64:96, kvh, :], blkind[:, :], [], ["kaug_ind"], sk=("d", "kaug_ind"))
            trk.op("pool", lambda g: g.memset(vaug[:, :, :, :], 1.0), [], ["vaug_init"])
            kms = sb(ph, "kms", [64, 4, 32], F32)
            kmb = sb(ph, "kmb", [64, 4, 32], BF16)
            trk.op("pool", lambda g: g.memset(kms[:, :, :], 0.0), [], ["kms"])
            trk.op("pool", lambda g: g.memset(kmb[:, :, :], 0.0), [], ["kmb"])
            gbias_sb = sb(ph, "gbias_sb", [128, 17, 32], F32)
            cb_sb = sb(ph, "cb_sb", [128, 17, 32], F32)
            trk.dma("sp", gbias_sb[:, :, :].rearrange("p a b -> p (a b)"), gbias_d.partition_broadcast(128), [], ["gbias"])
            trk.dma("sp", cb_sb[:, :, :].rearrange("p a b -> p (a b)"), cb_d.partition_broadcast(128), [], ["cb"])
            xb = [sb(ph, "xb%d" % i, [128, D], BF16) for i in range(2)]
            XT = [sb(ph, "XT%d" % i, [128, 8, 512], BF16) for i in range(1)] * 2
            ctt = [sb(ph, "ct%d" % i, [64, 512], F32) for i in range(1)] * 2
            stt = [sb(ph, "st%d" % i, [64, 512], F32) for i in range(1)] * 2
            P = {
                "t1": [sb(ph, "t1_%d" % i, [64, 512], F32) for i in range(1)] * 2,
                "t2": [sb(ph, "t2_%d" % i, [64, 512], F32) for i in range(1)] * 2,
                "krb": [sb(ph, "krb_%d" % i, [64, 512], BF16) for i in range(2)],
                "psP": [ps(ph, "psP_%d" % i, [64, 512]) for i in range(2)],
            }
            qa = [sb(ph, "qa%d" % i, [96, 4, 16, 128], BF16) for i in range(1)] * 2
            gb = sb(ph, "gb", [128, 16, 32], F32)
            m8 = sb(ph, "m8", [128, 16, 8], F32)
            sel = sb(ph, "sel", [128, 16, 32], F32)
            stage = sb(ph, "stage", [128, 16, 128], BF16)
            trk.op("pool", lambda g: g.memset(stage[:, :, :], 0.0), [], ["stage"])
            psT = [ps(ph, "psT%d" % i, [128, 1024], BF16) for i in range(1)]
            psK = [ps(ph, "psK%d" % i, [64, 512]) for i in range(2)]
            psV = ps(ph, "psV", [128, 512])
            psG = ps(ph, "psG", [128, 512])
            psM = ps(ph, "psM", [128, 8, 128], BF16)
            xi = 0
            ki = 0
            PHA = int(os.environ.get("PHA", "99"))
            for g in range(16 if PHA > 5 else (1 if PHA > 1 else 0)):
                gs = 0
                gcols = slice(g * 512, (g + 1) * 512)
                trk.dma("sp", ctt[gs][:, :], CT[:, gcols], ["CT"], ["ct0"])
                trk.dma("sp", stt[gs][:, :], ST[:, gcols], ["ST"], ["st0"])
                ctk = "ctst%d" % gs
                for sub in range(4):
                    sl = xi % 2
                    tp = 0
                    xi += 1
                    r0 = g * 512 + sub * 128
                    trk.dma("pool", xb[sl][:, :], xin[r0:r0 + 128, :], [], ["xb%d" % sl])
                    transposes(trk, [(psT[tp][:, kc * 128:(kc + 1) * 128], xb[sl][:, kc * 128:(kc + 1) * 128]) for kc in range(8)],
                               identb[:, :], ["xb%d" % sl, "identb"], ["psT%d" % tp])
                    trk.op("act", lambda a: a.activation(out=XT[gs][:, :, sub * 128:(sub + 1) * 128],
                                                         in_=psT[tp][:, :].rearrange("p (k c) -> p k c", k=8), func=AF.Copy),
                           ["psT%d" % tp], ["XT%d_%d" % (gs, sub)])
                xtk = ["XT%d_%d" % (gs, s_) for s_ in range(4)]
                for kvh in range(4 if PHA >= 3 else 0):
                    s2 = ki % 2
                    ki += 1
                    c0 = 1024 + kvh * 64
                    mm(trk, psK[s2][:, :], [(wqkv[:, kc, c0:c0 + 64], XT[gs][:, kc, :]) for kc in range(8)],
                       xtk + ["wqkv"], ["psK%d" % s2])
                    rope_head_keys = ["ct0", "st0"]
                    t1 = P["t1"][s2]; t2 = P["t2"][s2]; krb = P["krb"][s2]; psP = P["psP"][s2]
                    trk.op("dve", lambda v: v.tensor_tensor(out=t1[:, :], in0=psK[s2][:, :], in1=ctt[gs][:, :], op=ALU.mult),
                           ["psK%d" % s2, "ct0"], ["t1_0"])
                    trk.op("act", lambda a: a.activation(out=krb[:, :], in_=psK[s2][:, :], func=AF.Copy),
                           ["psK%d" % s2], ["krb_%d" % s2])
                    mm(trk, psP[:, :], [(pm[:, :], krb[:, :])], ["krb_%d" % s2, "pm"], ["psP_%d" % s2])
                    trk.op("dve", lambda v: v.tensor_tensor(out=t2[:, :], in0=psP[:, :], in1=stt[gs][:, :], op=ALU.mult),
                           ["psP_%d" % s2, "st0"], ["t2_0"])
                    trk.op("dve", lambda v: v.tensor_tensor(out=kaug[0:64, kvh, gcols], in0=t1[:, :], in1=t2[:, :], op=ALU.add),
                           ["t1_0", "t2_0"], ["kaug_%d_%d" % (g, kvh)])
                for sub in range(4 if PHA >= 4 else 0):
                    mm(trk, psV[:, 0:256], [(XT[gs][:, kc, sub * 128:(sub + 1) * 128], wqkv[:, kc, 1280:1536]) for kc in range(8)],
                       xtk + ["wqkv"], ["psV"])
                    trk.op("act", lambda a: a.activation(out=vaug[:, g * 4 + sub, :, 0:64],
                                                         in_=psV[:, 0:256].rearrange("p (h d) -> p h d", h=4), func=AF.Copy),
                           ["psV", "vaug_init"], ["vaug_%d" % (g * 4 + sub)])
                if PHA < 5:
                    continue
                trk.op("dve", lambda v: v.tensor_reduce(out=kms[:, :, 2 * g:2 * g + 2],
                                                        in_=kaug[0:64, :, gcols].rearrange("p h (b c) -> p h b c", b=2),
                                                        axis=AX.X, op=ALU.add),
                       ["kaug_%d_%d" % (g, k_) for k_ in range(4)] + ["kms"], ["kms"])
                trk.op("dve", lambda v: v.tensor_copy(out=kmb[:, :, :], in_=kms[:, :, :]), ["kms"], ["kmb"])
                if g < 7 or PHA < 7:
                    continue
                subs = [3] if g == 7 else [0, 1, 2, 3]
                c0q = subs[0] * 128
                ncols = len(subs) * 128
                qs = 0
                for h in range(16):
                    s2 = ki % 2
                    ki += 1
                    mm(trk, psK[s2][:, 0:ncols], [(wqkv[:, kc, h * 64:(h + 1) * 64], XT[gs][:, kc, c0q:512]) for kc in range(8)],
                       xtk + ["wqkv"], ["psK%d" % s2])
                    t1 = P["t1"][s2]; t2 = P["t2"][s2]; krb = P["krb"][s2]; psP = P["psP"][s2]
                    trk.op("dve", lambda v: v.scalar_tensor_tensor(out=t1[:, 0:ncols], in0=psK[s2][:, 0:ncols], scalar=0.125,
                                                                   in1=ctt[gs][:, c0q:512], op0=ALU.mult, op1=ALU.mult),
                           ["psK%d" % s2, "ct0"], ["t1_0"])
                    trk.op("act", lambda a: a.activation(out=krb[:, 0:ncols], in_=psK[s2][:, 0:ncols], func=AF.Copy, scale=0.125),
                           ["psK%d" % s2], ["krb_%d" % s2])
                    mm(trk, psP[:, 0:ncols], [(pm[:, :], krb[:, 0:ncols])], ["krb_%d" % s2, "pm"], ["psP_%d" % s2])
                    trk.op("dve", lambda v: v.tensor_tensor(out=t2[:, 0:ncols], in0=psP[:, 0:ncols], in1=stt[gs][:, c0q:512], op=ALU.mult),
                           ["psP_%d" % s2, "st0"], ["t2_0"])
                    trk.op("dve", lambda v: v.tensor_tensor(out=qa[qs][0:64, subs[0]:subs[-1] + 1, h, :],
                                                            in0=t1[:, 0:ncols].rearrange("p (s c) -> p s c", c=128),
                                                            in1=t2[:, 0:ncols].rearrange("p (s c) -> p s c", c=128), op=ALU.add),
                           ["t1_0", "t2_0"], ["qa%d_q" % qs])
                for sub in (subs if PHA >= 8 else []):
                    et = g * 4 + sub
                    jj = et // 2 - 15
                    def gate_mm(pe):
                        ins = None
                        for h in range(16):
                            ins = pe.matmul(psG[:, h * 32:(h + 1) * 32], qa[qs][0:64, sub, h, :], kmb[0:64, h // 4, :],
                                            start=True, stop=True)
                        return ins
                    trk.op("pe", gate_mm, ["qa%d_q" % qs, "kmb"], ["psG"])
                    trk.op("dve", lambda v: v.tensor_tensor(out=gb[:, :, :], in0=psG[:, :].rearrange("p (h n) -> p h n", h=16),
                                                            in1=gbias_sb[:, jj, :].unsqueeze(1).broadcast_to([128, 16, 32]), op=ALU.add),
                           ["psG", "gbias"], ["gb"])
                    for h in range(16):
                        trk.op("dve", lambda v: v.max(out=m8[:, h, :], in_=gb[:, h, :]), ["gb"], ["m8_%d" % h])
                    for h in range(16):
                        trk.op("dve", lambda v: v.tensor_scalar(out=sel[:, h, :], in0=gb[:, h, :], scalar1=m8[:, h, 2:3], scalar2=-NEG,
                                                                op0=ALU.is_ge, op1=ALU.mult), ["gb", "m8_%d" % h], ["sel_%d" % h])
                    trk.op("dve", lambda v: v.tensor_tensor(out=stage[:, :, 64:96], in0=sel[:, :, :],
                                                            in1=cb_sb[:, jj, :].unsqueeze(1).broadcast_to([128, 16, 32]), op=ALU.add),
                           ["sel_%d" % h for h in range(16)] + ["cb", "stage"], ["stage"])
                    for rnd in range(2):
                        transposes(trk, [(psM[:, hh, :], stage[:, rnd * 8 + hh, :]) for hh in range(8)], identb[:, :],
                                   ["stage", "identb"], ["psM"])
                        trk.op("act", lambda a: a.activation(out=qa[qs][64:96, sub, rnd * 8:(rnd + 1) * 8, :], in_=psM[64:96, :, :], func=AF.Copy),
                               ["psM"], ["qa%d_m%d_%d" % (qs, sub, rnd)])
                qkeys = ["qa%d_q" % qs] + (["qa%d_m%d_%d" % (qs, s_, r_) for s_ in subs for r_ in range(2)] if PHA >= 8 else [])
                if g == 7:
                    trk.dma("sp", QA[:, 0, :], qa[qs][:, 3, :, :].rearrange("p h c -> p (h c)"), qkeys, ["QA"])
                else:
                    t0 = 1 + 4 * (g - 8)
                    trk.dma("sp", QA[:, t0:t0 + 4, :], qa[qs][:, :, :, :].rearrange("p s h c -> p s (h c)"), qkeys, ["QA"])
            done()
            if debug:
                dk = nc.dram_tensor("dbg_kaug", [96, 4, SEQ], BF16, kind="ExternalOutput").ap()
                dv_ = nc.dram_tensor("dbg_vaug", [128, 64, 4, 128], BF16, kind="ExternalOutput").ap()
                trk.dma("sp", dk[:, :, :], kaug[:, :, :], [], ["dbg_k"])
                trk.dma("sp", dv_[:, :, :, :], vaug[:, :, :, :], [], ["dbg_v"])
                done()
        if stop_after == 1:
            return nc, es, trk
        with ExitStack() as ph:
            wo = sb(ph, "wo", [128, 8, D], BF16)
            load_w("pool", wo, w_o[0], "wo", nsplit=2)
            lng = sb(ph, "lng", [128, D], F32)
            lnb = sb(ph, "lnb", [128, D], F32)
            trk.dma("sp", lng[:, :], ln_mix_g[0:1, :].partition_broadcast(128), [], ["lng"])
            trk.dma("sp", lnb[:, :], ln_mix_b[0:1, :].partition_broadcast(128), [], ["lnb"])
            qt = [sb(ph, "qt%d" % i, [96, 2048], BF16) for i in range(2)]
            PT = [sb(ph, "PT%d" % i, [128, 512], BF16) for i in range(3)]
            rden = [sb(ph, "rden%d" % i, [128, 512], F32) for i in range(2)]
            aT = [sb(ph, "aT%d" % i, [128, 8, 128], BF16) for i in range(2)]
            xf = [sb(ph, "xf%d" % i, [128, D], F32) for i in range(2)]
            yb = [sb(ph, "y%d" % i, [128, D], F32) for i in range(2)]
            x1b = [sb(ph, "x1b%d" % i, [128, D], BF16) for i in range(2)]
            x1T = [sb(ph, "x1T%d" % i, [128, 8, 128], BF16) for i in range(2)]
            LNP = {"st6": [sb(ph, "st6_%d" % i, [128, 12], F32) for i in range(2)],
                   "mv": [sb(ph, "mv_%d" % i, [128, 4], F32) for i in range(2)]}
            psS = [ps(ph, "psS%d" % i, [128, 512]) for i in range(2)]
            psO = [ps(ph, "psO%d" % i, [128, 512]) for i in range(2)]
            psW = ps(ph, "psW", [128, 2, 512])
            psT2 = ps(ph, "psT2", [128, 1024], BF16)
            cnt = {"s": 0, "p": 0, "o": 0}

            def attention(ti):
                sl = ti % 2
                et = 31 + ti
                J, s_ = et // 2, et % 2
                trk.dma("sp", qt[sl][:, :], QA[:, ti, :], ["QA"], ["qt%d" % sl])
                r0 = 3968 + 128 * ti
                trk.dma("sp", xf[sl][:, :], xin[r0:r0 + 128, :], [], ["xf%d" % sl])
                ktiles = [(kt, False) for kt in range(2 * J)]
                if s_ == 0:
                    ktiles.append((2 * J, True))
                else:
                    ktiles.append((2 * J, False))
                    ktiles.append((2 * J + 1, True))
                for kvh in range(4):
                    so = cnt["o"] % 2
                    cnt["o"] += 1
                    n = len(ktiles)
                    for idx, (kt, diag) in enumerate(ktiles):
                        ss = cnt["s"] % 2
                        cnt["s"] += 1
                        pp = cnt["p"] % 3
                        cnt["p"] += 1
                        mm(trk, psS[ss][:, :], [(kaug[0:96, kvh, kt * 128:(kt + 1) * 128], qt[sl][0:96, kvh * 512:(kvh + 1) * 512])],
                           ["qt%d" % sl], ["psS%d" % ss])
                        trk.op("act", lambda a: a.activation(out=PT[pp][:, :], in_=psS[ss][:, :], func=AF.Exp),
                               ["psS%d" % ss], ["PT%d" % pp])
                        if diag:
                            trk.op("dve", lambda v: v.tensor_tensor(out=PT[pp][:, :].rearrange("p (h q) -> p h q", h=4),
                                                                    in0=PT[pp][:, :].rearrange("p (h q) -> p h q", h=4),
                                                                    in1=trib[:, :].unsqueeze(1).broadcast_to([128, 4, 128]), op=ALU.mult),
                                   ["PT%d" % pp, "trib"], ["PT%d" % pp])
                        def pv(pe):
                            return pe.matmul(psO[so][:, :], vaug[:, kt, kvh, :], PT[pp][:, :], start=(idx == 0), stop=(idx == n - 1))
                        trk.op("pe", pv, ["PT%d" % pp], ["psO%d" % so])
                    rs = so
                    trk.op("dve", lambda v: v.reciprocal(out=rden[rs][64:128, :], in_=psO[so][64:128, :]), ["psO%d" % so], ["rden%d" % rs])
                    if debug:
                        trk.dma("sp", dbg_rd[ti, kvh, 64:128, :], rden[rs][64:128, :], ["rden%d" % rs], ["dbg_rd"])
                    for i in range(4):
                        pb = (i % 2) * 64
                        trk.op("dve", lambda v: v.tensor_tensor(out=aT[sl][pb:pb + 64, kvh * 2 + i // 2, :],
                                                                in0=psO[so][0:64, i * 128:(i + 1) * 128],
                                                                in1=rden[rs][64:128, i * 128:(i + 1) * 128], op=ALU.mult),
                               ["psO%d" % so, "rden%d" % rs], ["aT%d_%d_%d" % (sl, kvh, i)])

            if debug:
                dbg_aT = nc.dram_tensor("dbg_aT", [NT, 128, 8, 128], BF16, kind="ExternalOutput").ap()
                dbg_rd = nc.dram_tensor("dbg_rd", [NT, 4, 128, 512], F32, kind="ExternalOutput").ap()

            def tail(ti):
                sl = ti % 2
                akeys = ["aT%d_%d_%d" % (sl, k_, i_) for k_ in range(4) for i_ in range(4)]
                if debug:
                    trk.dma("sp", dbg_aT[ti], aT[sl][:, :, :], akeys, ["dbg_aT"])
                for half in range(2):
                    mm(trk, psW[:, half, :], [(aT[sl][:, c, :], wo[:, c, half * 512:(half + 1) * 512]) for c in range(8)],
                       akeys + ["wo"], ["psW"])
                for half in range(2):
                    trk.op("dve", lambda v: v.scalar_tensor_tensor(out=yb[sl][:, half * 512:(half + 1) * 512],
                                                                   in0=xf[sl][:, half * 512:(half + 1) * 512], scalar=ALPHA,
                                                                   in1=psW[:, half, :], op0=ALU.mult, op1=ALU.add),
                           ["xf%d" % sl, "psW"], ["y%d" % sl])
                layer_norm(trk, LNP, yb[sl], "y%d" % sl, lng, lnb, yb[sl], "y%d" % sl, sl)
                trk.dma("sp", X1[ti * 128:(ti + 1) * 128, :], yb[sl][:, :], ["y%d" % sl], ["X1"])
                trk.op("act", lambda a: a.activation(out=x1b[sl][:, :], in_=yb[sl][:, :], func=AF.Copy), ["y%d" % sl], ["x1b%d" % sl])
                transposes(trk, [(psT2[:, kc * 128:(kc + 1) * 128], x1b[sl][:, kc * 128:(kc + 1) * 128]) for kc in range(8)],
                           identb[:, :], ["x1b%d" % sl, "identb"], ["psT2"])
                trk.op("act", lambda a: a.activation(out=x1T[sl][:, :, :], in_=psT2[:, :].rearrange("p (k c) -> p k c", k=8), func=AF.Copy),
                       ["psT2"], ["x1T%d" % sl])
                trk.dma("sp", X1T[:, :, ti * 128:(ti + 1) * 128], x1T[sl][:, :, :], ["x1T%d" % sl], ["X1T"])

            NTB = int(os.environ.get("NTB", str(NT)))
            for ti in range(NTB):
                attention(ti)
                if ti >= 1:
                    tail(ti - 1)
            tail(NTB - 1)
            done()
    if stop_after == 2:
        return nc, es, trk

    def ffn_layer(layer, XTd, Xd, pd, chunks, experts, use_gates, Yd, YTd):
        for (t0, nt) in chunks:
            with ExitStack() as ck:
                ncol = nt * 128
                XTc = sb(ck, "XTc", [128, 8, ncol], BF16)
                acc = sb(ck, "acc", [128, nt, D], F32)
                for kc in range(8):
                    trk.dma("sp", XTc[:, kc, :], XTd[:, kc, t0 * 128:t0 * 128 + ncol], [], ["XTc"], sk=("d", "XTc"))
                for t in range(nt):
                    r0 = (t0 + t) * 128
                    trk.dma("sp", acc[:, t, :], Xd[r0:r0 + 128, :], [], ["accld"], sk=("d", "accld"))
                for t in range(nt):
                    trk.op("act", lambda a: a.mul(out=acc[:, t, :], in_=acc[:, t, :], mul=ALPHA), ["accld"], ["acc%d" % t])
                if use_gates:
                    gt = sb(ck, "gt", [128, nt, 8], F32)
                    trk.dma("sp", gt[:, :, :], GATES[:, t0:t0 + nt, :], [], ["gt"])
                with ExitStack() as ph:
                    wg = [sb(ph, "wg%d" % i, [128, 8, 512], BF16) for i in range(2)]
                    wu = [sb(ph, "wu%d" % i, [128, 8, 512], BF16) for i in range(2)]
                    wd = [sb(ph, "wd%d" % i, [128, 4, D], BF16) for i in range(2)]
                    sg = [sb(ph, "sg%d" % i, [128, 512], F32) for i in range(2)]
                    hh = [sb(ph, "hh%d" % i, [128, 4, 512], BF16) for i in range(2)]
                    psG = [ps(ph, "psG%d" % i, [128, 512]) for i in range(2)]
                    psU = [ps(ph, "psU%d" % i, [128, 512]) for i in range(2)]
                    psY = [ps(ph, "psY%d" % i, [128, 2, 512]) for i in range(2)]
                    steps = [(e, fb) for e in range(len(experts)) for fb in range(NFB)]

                    def load(i):
                        e, fb = steps[i]
                        sl = i % 2
                        wg_ap, wu_ap, wd_ap = experts[e]
                        fs = slice(fb * 512, (fb + 1) * 512)
                        trk.dma("pool", wg[sl][:, :, :], wg_ap.rearrange("(k p) n -> p k n", p=128)[:, :, fs], [], ["wg%d" % sl])
                        trk.dma("pool", wu[sl][:, :, :], wu_ap.rearrange("(k p) n -> p k n", p=128)[:, :, fs], [], ["wu%d" % sl])
                        trk.dma("pool", wd[sl][:, :, :], wd_ap[fs, :].rearrange("(c p) n -> p c n", p=128), [], ["wd%d" % sl])

                    groups = []
                    tt = 0
                    while tt < nt:
                        gn = min(4, nt - tt)
                        groups.append((tt, gn))
                        tt += gn
                    c1 = 0
                    c2 = 0
                    c3 = 0
                    load(0)
                    for i, (e, fb) in enumerate(steps):
                        if i + 1 < len(steps):
                            load(i + 1)
                        sl = i % 2
                        for (g0, gn) in groups:
                            cols = slice(g0 * 128, (g0 + gn) * 128)
                            ncl = gn * 128
                            hs = c2 % 2
                            c2 += 1
                            for fc in range(4):
                                a_ = c1 % 2
                                c1 += 1
                                fsl = slice(fc * 128, (fc + 1) * 128)
                                mm(trk, psG[a_][:, 0:ncl], [(wg[sl][:, kc, fsl], XTc[:, kc, cols]) for kc in range(8)],
                                   ["wg%d" % sl, "XTc"], ["psG%d" % a_])
                                mm(trk, psU[a_][:, 0:ncl], [(wu[sl][:, kc, fsl], XTc[:, kc, cols]) for kc in range(8)],
                                   ["wu%d" % sl, "XTc"], ["psU%d" % a_])
                                trk.op("act", lambda a: a.activation(out=sg[a_][:, 0:ncl], in_=psG[a_][:, 0:ncl], func=AF.Silu),
                                       ["psG%d" % a_], ["sg%d" % a_])
                                trk.op("dve", lambda v: v.tensor_tensor(out=hh[hs][:, fc, 0:ncl], in0=sg[a_][:, 0:ncl], in1=psU[a_][:, 0:ncl],
                                                                        op=ALU.mult), ["sg%d" % a_, "psU%d" % a_], ["hh%d_%d" % (hs, fc)])
                            hkeys = ["hh%d_%d" % (hs, fc) for fc in range(4)]
                            for tl in range(gn):
                                t = g0 + tl
                                ys = c3 % 2
                                c3 += 1
                                for half in range(2):
                                    mm(trk, psY[ys][:, half, :], [(hh[hs][:, fc, tl * 128:(tl + 1) * 128], wd[sl][:, fc, half * 512:(half + 1) * 512])
                                                                 for fc in range(4)], hkeys + ["wd%d" % sl], ["psY%d" % ys])
                                for half in range(2):
                                    hsl = slice(half * 512, (half + 1) * 512)
                                    if use_gates:
                                        trk.op("dve", lambda v: v.scalar_tensor_tensor(out=acc[:, t, hsl], in0=psY[ys][:, half, :],
                                                                                       scalar=gt[:, t, e:e + 1], in1=acc[:, t, hsl],
                                                                                       op0=ALU.mult, op1=ALU.add),
                                               ["psY%d" % ys, "acc%d" % t, "gt"], ["acc%d" % t])
                                    else:
                                        trk.op("dve", lambda v: v.tensor_tensor(out=acc[:, t, hsl], in0=psY[ys][:, half, :], in1=acc[:, t, hsl],
                                                                                op=ALU.add), ["psY%d" % ys, "acc%d" % t], ["acc%d" % t])
                    done()
                with ExitStack() as ph:
                    wpg = sb(ph, "wpg", [128, 8, D], BF16)
                    wpp = sb(ph, "wpp", [128, 2, D], BF16)
                    load_w("pool", wpg, w_ple_gate[layer], "wpg", nsplit=2)
                    load_w("pool", wpp, w_ple_proj[layer], "wpp")
                    lng = sb(ph, "lng", [128, D], F32)
                    lnb = sb(ph, "lnb", [128, D], F32)
                    trk.dma("sp", lng[:, :], ln_ffn_g[layer:layer + 1, :].partition_broadcast(128), [], ["lng"])
                    trk.dma("sp", lnb[:, :], ln_ffn_b[layer:layer + 1, :].partition_broadcast(128), [], ["lnb"])
                    x2b = [sb(ph, "x2b%d" % i, [128, D], BF16) for i in range(2)]
                    x2T = [sb(ph, "x2T%d" % i, [128, 8, 128], BF16) for i in range(2)]
                    pb = [sb(ph, "pb%d" % i, [128, 256], BF16) for i in range(2)]
                    pT = [sb(ph, "pT%d" % i, [128, 2, 128], BF16) for i in range(2)]
                    sig = [sb(ph, "sig%d" % i, [128, 2, 512], F32) for i in range(2)]
                    ob = [sb(ph, "ob%d" % i, [128, D], BF16) for i in range(2)]
                    oT = [sb(ph, "oT%d" % i, [128, 8, 128], BF16) for i in range(2)]
                    LNP = {"st6": [sb(ph, "st6_%d" % i, [128, 12], F32) for i in range(2)],
                           "mv": [sb(ph, "mv_%d" % i, [128, 4], F32) for i in range(2)]}
                    psT = ps(ph, "psT", [128, 1024], BF16)
                    psA = ps(ph, "psA", [128, 2, 512])
                    psB = ps(ph, "psB", [128, 2, 512])
                    psTp = ps(ph, "psTp", [128, 1024], BF16)
                    psT3 = ps(ph, "psT3", [128, 1024], BF16)
                    for t in range(nt):
                        sl = t % 2
                        r0 = (t0 + t) * 128
                        ak = "acc%d" % t
                        x2 = acc[:, t, :]
                        trk.dma("pool", pb[sl][:, :], pd[r0:r0 + 128, :], [], ["pb%d" % sl])
                        layer_norm(trk, LNP, x2, ak, lng, lnb, x2, ak, sl)
                        trk.op("act", lambda a: a.activation(out=x2b[sl][:, :], in_=x2, func=AF.Copy), [ak], ["x2b%d" % sl])
                        transposes(trk, [(psT[:, kc * 128:(kc + 1) * 128], x2b[sl][:, kc * 128:(kc + 1) * 128]) for kc in range(8)],
                                   identb[:, :], ["x2b%d" % sl, "identb"], ["psT"])
                        trk.op("act", lambda a: a.activation(out=x2T[sl][:, :, :], in_=psT[:, :].rearrange("p (k c) -> p k c", k=8), func=AF.Copy),
                               ["psT"], ["x2T%d" % sl])
                        for half in range(2):
                            mm(trk, psA[:, half, :], [(x2T[sl][:, kc, :], wpg[:, kc, half * 512:(half + 1) * 512]) for kc in range(8)],
                               ["x2T%d" % sl, "wpg"], ["psA"])
                        trk.op("act", lambda a: a.activation(out=sig[sl][:, :, :], in_=psA[:, :, :], func=AF.Sigmoid), ["psA"], ["sig%d" % sl])
                        transposes(trk, [(psTp[:, c * 128:(c + 1) * 128], pb[sl][:, c * 128:(c + 1) * 128]) for c in range(2)],
                                   identb[:, :], ["pb%d" % sl, "identb"], ["psTp"])
                        trk.op("act", lambda a: a.activation(out=pT[sl][:, :, :], in_=psTp[:, 0:256].rearrange("p (k c) -> p k c", k=2), func=AF.Copy),
                               ["psTp"], ["pT%d" % sl])
                        for half in range(2):
                            mm(trk, psB[:, half, :], [(pT[sl][:, c, :], wpp[:, c, half * 512:(half + 1) * 512]) for c in range(2)],
                               ["pT%d" % sl, "wpp"], ["psB"])
                        trk.op("dve", lambda v: v.tensor_tensor(out=sig[sl][:, :, :], in0=psB[:, :, :], in1=sig[sl][:, :, :], op=ALU.mult),
                               ["psB", "sig%d" % sl], ["sig%d" % sl])
                        trk.op("dve", lambda v: v.tensor_tensor(out=x2, in0=x2, in1=sig[sl][:, :, :].rearrange("p a b -> p (a b)"), op=ALU.add),
                               [ak, "sig%d" % sl], [ak])
                        okey = "out" if Yd is out_d else "Yd%d" % layer
                        trk.dma("sp", Yd[r0:r0 + 128, :], x2, [ak], [okey])
                        if YTd is not None:
                            trk.op("act", lambda a: a.activation(out=ob[sl][:, :], in_=x2, func=AF.Copy), [ak], ["ob%d" % sl])
                            transposes(trk, [(psT3[:, kc * 128:(kc + 1) * 128], ob[sl][:, kc * 128:(kc + 1) * 128]) for kc in range(8)],
                                       identb[:, :], ["ob%d" % sl, "identb"], ["psT3"])
                            trk.op("act", lambda a: a.activation(out=oT[sl][:, :, :], in_=psT3[:, :].rearrange("p (k c) -> p k c", k=8), func=AF.Copy),
                                   ["psT3"], ["oT%d" % sl])
                            trk.dma("sp", YTd[:, :, r0:r0 + 128], oT[sl][:, :, :], ["oT%d" % sl], ["YTd%d" % layer])
                    done()

    ffn_layer(0, X1T, X1, p0, [(0, 17), (17, 16)], [(w_ffn_gate[0], w_ffn_up[0], w_ffn_down[0])], False, X3, X3T)
    if stop_after == 3:
        return nc, es, trk

    with ExitStack() as kv1:
        k1 = sb(kv1, "k1", [64, 4, NTOK], BF16)
        v1 = sb(kv1, "v1", [128, NT, 4, 128], BF16)
        with ExitStack() as ph:
            wqkv = sb(ph, "wqkv1", [128, 8, 1536], BF16)
            load_w("pool", wqkv, w_qkv[1], "wqkv", nsplit=4)
            trk.op("pool", lambda g: g.memset(v1[:, :, :, :], 1.0), [], ["v1_init"])
            XT = [sb(ph, "XTd%d" % i, [128, 8, 512], BF16) for i in range(2)]
            ctt = [sb(ph, "ctd%d" % i, [64, 512], F32) for i in range(2)]
            stt = [sb(ph, "std%d" % i, [64, 512], F32) for i in range(2)]
            t1s = [sb(ph, "t1d%d" % i, [64, 512], F32) for i in range(2)]
            t2s = [sb(ph, "t2d%d" % i, [64, 512], F32) for i in range(2)]
            krbs = [sb(ph, "krbd%d" % i, [64, 512], BF16) for i in range(2)]
            qg = [sb(ph, "qg%d" % i, [64, 4, 16, 128], BF16) for i in range(2)]
            psK = [ps(ph, "psK%d" % i, [64, 512]) for i in range(2)]
            psPp = [ps(ph, "psP%d" % i, [64, 512]) for i in range(2)]
            psV = [ps(ph, "psV%d" % i, [128, 512]) for i in range(2)]
            ki = 0
            vi = 0
            groups = [(0, 1)] + [(1 + 4 * i, 4) for i in range(8)]
            for gi, (t0, gn) in enumerate(groups):
                gs = gi % 2
                ncl = gn * 128
                cols = slice(t0 * 128, t0 * 128 + ncl)
                ecols = slice(3968 + t0 * 128, 3968 + t0 * 128 + ncl)
                for kc in range(8):
                    trk.dma("sp", XT[gs][:, kc, 0:ncl], X3T[:, kc, cols], [], ["XT%d" % gs], sk=("d", "XTd%d" % gs))
                trk.dma("sp", ctt[gs][:, 0:ncl], CT[:, ecols], [], ["ct%d" % gs])
                trk.dma("sp", stt[gs][:, 0:ncl], ST[:, ecols], [], ["st%d" % gs])

                def proj_rope(c0, scale, out_ap, outk):
                    nonlocal ki
                    s2 = ki % 2
                    ki += 1
                    t1 = t1s[s2]; t2 = t2s[s2]; krb = krbs[s2]; psP = psPp[s2]
                    mm(trk, psK[s2][:, 0:ncl], [(wqkv[:, kc, c0:c0 + 64], XT[gs][:, kc, 0:ncl]) for kc in range(8)],
                       ["XT%d" % gs, "wqkv"], ["psK%d" % s2])
                    trk.op("dve", lambda v: v.scalar_tensor_tensor(out=t1[:, 0:ncl], in0=psK[s2][:, 0:ncl], scalar=scale,
                                                                   in1=ctt[gs][:, 0:ncl], op0=ALU.mult, op1=ALU.mult),
                           ["psK%d" % s2, "ct%d" % gs], ["t1_%d" % s2])
                    trk.op("act", lambda a: a.activation(out=krb[:, 0:ncl], in_=psK[s2][:, 0:ncl], func=AF.Copy, scale=scale),
                           ["psK%d" % s2], ["krb_%d" % s2])
                    mm(trk, psP[:, 0:ncl], [(pm[:, :], krb[:, 0:ncl])], ["krb_%d" % s2, "pm"], ["psP%d" % s2])
                    trk.op("dve", lambda v: v.tensor_tensor(out=t2[:, 0:ncl], in0=psP[:, 0:ncl], in1=stt[gs][:, 0:ncl], op=ALU.mult),
                           ["psP%d" % s2, "st%d" % gs], ["t2_%d" % s2])
                    if len(out_ap.shape) == 2:
                        trk.op("dve", lambda v: v.tensor_tensor(out=out_ap, in0=t1[:, 0:ncl], in1=t2[:, 0:ncl], op=ALU.add),
                               ["t1_%d" % s2, "t2_%d" % s2], [outk])
                    else:
                        trk.op("dve", lambda v: v.tensor_tensor(out=out_ap, in0=t1[:, 0:ncl].rearrange("p (s c) -> p s c", c=128),
                                                                in1=t2[:, 0:ncl].rearrange("p (s c) -> p s c", c=128), op=ALU.add),
                               ["t1_%d" % s2, "t2_%d" % s2], [outk])

                for kvh in range(4):
                    proj_rope(1024 + kvh * 64, 1.0, k1[0:64, kvh, cols], "k1_%d_%d" % (gi, kvh))
                for tl in range(gn):
                    vs = vi % 2
                    vi += 1
                    mm(trk, psV[vs][:, 0:256], [(XT[gs][:, kc, tl * 128:(tl + 1) * 128], wqkv[:, kc, 1280:1536]) for kc in range(8)],
                       ["XT%d" % gs, "wqkv"], ["psV%d" % vs])
                    trk.op("act", lambda a: a.activation(out=v1[:, t0 + tl, :, 0:64], in_=psV[vs][:, 0:256].rearrange("p (h d) -> p h d", h=4),
                                                         func=AF.Copy), ["psV%d" % vs, "v1_init"], ["v1_%d" % (t0 + tl)])
                if t0 == 0:
                    continue
                qs = gi % 2
                for h in range(16):
                    proj_rope(h * 64, 0.125, qg[qs][0:64, :, h, :], "qg%d" % qs)
                trk.dma("sp", Q1[:, t0 - 1:t0 + 3, :], qg[qs][:, :, :, :].rearrange("p s h c -> p s (h c)"), ["qg%d" % qs], ["Q1"])
            done()
        if stop_after == 4:
            return nc, es, trk
        with ExitStack() as ph:
            wo = sb(ph, "wo1", [128, 8, D], BF16)
            load_w("pool", wo, w_o[1], "wo", nsplit=2)
            lng = sb(ph, "lng1", [128, D], F32)
            lnb = sb(ph, "lnb1", [128, D], F32)
            trk.dma("sp", lng[:, :], ln_mix_g[1:2, :].partition_broadcast(128), [], ["lng"])
            trk.dma("sp", lnb[:, :], ln_mix_b[1:2, :].partition_broadcast(128), [], ["lnb"])
            wr = sb(ph, "wr", [128, 8, 8], F32)
            trk.dma("sp", wr[:, :, :], w_router[0].rearrange("(k p) n -> p k n", p=128), [], ["wr"])
            esr = sb(ph, "esr", [128, 16], F32)
            esink = sb(ph, "esink", [128, 16], F32)
            trk.dma("sp", esr[:, :], sinks[0:1, :].partition_broadcast(128), [], ["esr"])
            trk.op("act", lambda a: a.activation(out=esink[:, :], in_=esr[:, :], func=AF.Exp), ["esr"], ["esink"])
            qt = [sb(ph, "qt1_%d" % i, [64, 2048], BF16) for i in range(2)]
            PT = [sb(ph, "PT1_%d" % i, [128, 512], BF16) for i in range(4)]
            den = [sb(ph, "den%d" % i, [128, 512], F32) for i in range(2)]
            rden = [sb(ph, "rden1_%d" % i, [128, 512], F32) for i in range(2)]
            aT = [sb(ph, "aT1_%d" % i, [128, 8, 128], BF16) for i in range(2)]
            xf = [sb(ph, "xf1_%d" % i, [128, D], F32) for i in range(2)]
            yb = [sb(ph, "y1_%d" % i, [128, D], F32) for i in range(2)]
            x4b = [sb(ph, "x4b%d" % i, [128, D], BF16) for i in range(2)]
            x4T = [sb(ph, "x4T%d" % i, [128, 8, 128], BF16) for i in range(2)]
            x4T32 = sb(ph, "x4T32", [128, 8, 128], F32)
            lg = sb(ph, "lg", [128, 8], F32)
            m8 = sb(ph, "m8r", [128, 8], F32)
            d8 = sb(ph, "d8", [128, 8], F32)
            e8 = sb(ph, "e8", [128, 8], F32)
            msk = sb(ph, "msk", [128, 8], F32)
            ss = sb(ph, "ss", [128, 2], F32)
            gtl = [sb(ph, "gtl%d" % i, [128, 8], F32) for i in range(2)]
            LNP = {"st6": [sb(ph, "st6d_%d" % i, [128, 12], F32) for i in range(2)],
                   "mv": [sb(ph, "mvd_%d" % i, [128, 4], F32) for i in range(2)]}
            psS = [ps(ph, "psS%d" % i, [128, 512]) for i in range(2)]
            psO = [ps(ph, "psO%d" % i, [128, 512]) for i in range(2)]
            psW = ps(ph, "psW", [128, 2, 512])
            psT2 = ps(ph, "psT2", [128, 1024], BF16)
            psL = ps(ph, "psL", [128, 512])
            cnt = {"s": 0, "p": 0, "o": 0}

            def attention1(ti):
                sl = ti % 2
                o_ = ti - 1
                trk.dma("sp", qt[sl][:, :], Q1[:, o_, :], [], ["qt%d" % sl])
                trk.dma("sp", xf[sl][:, :], X3[ti * 128:(ti + 1) * 128, :], [], ["xf%d" % sl])
                for kvh in range(4):
                    so = cnt["o"] % 2
                    cnt["o"] += 1
                    parts = [(ti - 1, tricfv if ti == 1 else tricb, "tricfv" if ti == 1 else "tricb"), (ti, trib, "trib")]
                    for idx, (kt, mask, mkey) in enumerate(parts):
                        ss_ = cnt["s"] % 2
                        cnt["s"] += 1
                        pp = cnt["p"] % 4
                        cnt["p"] += 1
                        mm(trk, psS[ss_][:, :], [(k1[0:64, kvh, kt * 128:(kt + 1) * 128], qt[sl][0:64, kvh * 512:(kvh + 1) * 512])],
                           ["qt%d" % sl], ["psS%d" % ss_])
                        trk.op("act", lambda a: a.activation(out=PT[pp][:, :], in_=psS[ss_][:, :], func=AF.Exp), ["psS%d" % ss_], ["PT%d" % pp])
                        trk.op("dve", lambda v: v.tensor_tensor(out=PT[pp][:, :].rearrange("p (h q) -> p h q", h=4),
                                                                in0=PT[pp][:, :].rearrange("p (h q) -> p h q", h=4),
                                                                in1=mask[:, :].unsqueeze(1).broadcast_to([128, 4, 128]), op=ALU.mult),
                               ["PT%d" % pp, mkey], ["PT%d" % pp])
                        def pv(pe):
                            return pe.matmul(psO[so][:, :], v1[:, kt, kvh, :], PT[pp][:, :], start=(idx == 0), stop=(idx == 1))
                        trk.op("pe", pv, ["PT%d" % pp], ["psO%d" % so])
                    rs = so
                    for i in range(4):
                        h = kvh * 4 + i
                        trk.op("dve", lambda v: v.tensor_scalar(out=den[rs][64:128, i * 128:(i + 1) * 128], in0=psO[so][64:128, i * 128:(i + 1) * 128],
                                                                scalar1=esink[64:128, h:h + 1], scalar2=None, op0=ALU.add),
                               ["psO%d" % so, "esink"], ["den%d_%d" % (rs, i)])
                    trk.op("dve", lambda v: v.reciprocal(out=rden[rs][64:128, :], in_=den[rs][64:128, :]),
                           ["den%d_%d" % (rs, i) for i in range(4)], ["rden%d" % rs])
                    for i in range(4):
                        pb_ = (i % 2) * 64
                        trk.op("dve", lambda v: v.tensor_tensor(out=aT[sl][pb_:pb_ + 64, kvh * 2 + i // 2, :],
                                                                in0=psO[so][0:64, i * 128:(i + 1) * 128],
                                                                in1=rden[rs][64:128, i * 128:(i + 1) * 128], op=ALU.mult),
                               ["psO%d" % so, "rden%d" % rs], ["aT%d_%d_%d" % (sl, kvh, i)])

            def tail1(ti):
                sl = ti % 2
                o_ = ti - 1
                akeys = ["aT%d_%d_%d" % (sl, k_, i_) for k_ in range(4) for i_ in range(4)]
                for half in range(2):
                    mm(trk, psW[:, half, :], [(aT[sl][:, c, :], wo[:, c, half * 512:(half + 1) * 512]) for c in range(8)],
                       akeys + ["wo"], ["psW"])
                for half in range(2):
                    trk.op("dve", lambda v: v.scalar_tensor_tensor(out=yb[sl][:, half * 512:(half + 1) * 512],
                                                                   in0=xf[sl][:, half * 512:(half + 1) * 512], scalar=ALPHA,
                                                                   in1=psW[:, half, :], op0=ALU.mult, op1=ALU.add),
                           ["xf%d" % sl, "psW"], ["y%d" % sl])
                layer_norm(trk, LNP, yb[sl], "y%d" % sl, lng, lnb, yb[sl], "y%d" % sl, sl)
                trk.dma("sp", X4[o_ * 128:(o_ + 1) * 128, :], yb[sl][:, :], ["y%d" % sl], ["X4"])
                trk.op("act", lambda a: a.activation(out=x4b[sl][:, :], in_=yb[sl][:, :], func=AF.Copy), ["y%d" % sl], ["x4b%d" % sl])
                transposes(trk, [(psT2[:, kc * 128:(kc + 1) * 128], x4b[sl][:, kc * 128:(kc + 1) * 128]) for kc in range(8)],
                           identb[:, :], ["x4b%d" % sl, "identb"], ["psT2"])
                trk.op("act", lambda a: a.activation(out=x4T[sl][:, :, :], in_=psT2[:, :].rearrange("p (k c) -> p k c", k=8), func=AF.Copy),
                       ["psT2"], ["x4T%d" % sl])
                trk.dma("sp", X4T[:, :, o_ * 128:(o_ + 1) * 128], x4T[sl][:, :, :], ["x4T%d" % sl], ["X4T"])
                transposes(trk, [(psW[:, kc // 4, (kc % 4) * 128:(kc % 4 + 1) * 128], yb[sl][:, kc * 128:(kc + 1) * 128]) for kc in range(8)],
                           identf[:, :], ["y%d" % sl, "identf"], ["psW"])
                trk.op("act", lambda a: a.activation(out=x4T32[:, :, :].rearrange("p (a b) c -> p a b c", a=2),
                                                     in_=psW[:, :, :].rearrange("p a (b c) -> p a b c", b=4), func=AF.Copy),
                       ["psW"], ["x4T32"])
                mm(trk, psL[:, 0:8], [(x4T32[:, kc, :], wr[:, kc, :]) for kc in range(8)], ["x4T32", "wr"], ["psL"])
                trk.op("dve", lambda v: v.tensor_copy(out=lg[:, :], in_=psL[:, 0:8]), ["psL"], ["lg"])
                trk.op("dve", lambda v: v.max(out=m8[:, :], in_=lg[:, :]), ["lg"], ["m8r"])
                trk.op("dve", lambda v: v.tensor_scalar(out=d8[:, :], in0=lg[:, :], scalar1=m8[:, 0:1], scalar2=None, op0=ALU.subtract),
                       ["lg", "m8r"], ["d8"])
                trk.op("act", lambda a: a.activation(out=e8[:, :], in_=d8[:, :], func=AF.Exp), ["d8"], ["e8"])
                trk.op("dve", lambda v: v.tensor_scalar(out=msk[:, :], in0=lg[:, :], scalar1=m8[:, 1:2], scalar2=None, op0=ALU.is_ge),
                       ["lg", "m8r"], ["msk"])
                trk.op("dve", lambda v: v.tensor_tensor(out=e8[:, :], in0=e8[:, :], in1=msk[:, :], op=ALU.mult), ["e8", "msk"], ["e8"])
                trk.op("dve", lambda v: v.tensor_reduce(out=ss[:, 0:1], in_=e8[:, :], axis=AX.X, op=ALU.add), ["e8"], ["ss0"])
                trk.op("dve", lambda v: v.reciprocal(out=ss[:, 1:2], in_=ss[:, 0:1]), ["ss0"], ["ss1"])
                trk.op("dve", lambda v: v.tensor_scalar(out=gtl[sl][:, :], in0=e8[:, :], scalar1=ss[:, 1:2], scalar2=None, op0=ALU.mult),
                       ["e8", "ss1"], ["gtl%d" % sl])
                trk.dma("sp", GATES[:, o_, :], gtl[sl][:, :], ["gtl%d" % sl], ["GATES"])

            for ti in range(1, NT):
                attention1(ti)
                if ti >= 2:
                    tail1(ti - 1)
            tail1(NT - 1)
            done()
    if stop_after == 5:
        return nc, es, trk

    experts = [(w_exp_gate[0, e], w_exp_up[0, e], w_exp_down[0, e]) for e in range(8)]
    ffn_layer(1, X4T, X4, p1, [(0, 16), (16, 16)], experts, True, out_d, None)
    return nc, es, trk


def _consts():
    d = np.arange(64)
    inv = np.zeros(64, np.float64)
    i = d % 8
    invf = 1.0 / (500000.0 ** (np.arange(0, 16, 2, dtype=np.float32) / np.float32(16)))
    inv[:16] = invf.astype(np.float64)[i[:16]]
    sgn = np.zeros(64); sgn[:8] = -1.0; sgn[8:16] = 1.0
    c64 = np.zeros((64, 4), np.float32)
    c64[:, 0] = (inv / (2 * np.pi)).astype(np.float32)
    c64[:, 1] = (sgn * 2 * np.pi).astype(np.float32)
    c64[:, 2] = sgn.astype(np.float32)
    pmat = np.zeros((64, 64), np.float32)
    for a in range(8):
        pmat[a + 8, a] = 1.0
        pmat[a, a + 8] = 1.0
    ident = np.eye(128, dtype=np.float32)
    blk = np.zeros((32, SEQ), np.float32)
    for n in range(32):
        blk[n, n * 256:(n + 1) * 256] = 1.0
    k = np.arange(128)[:, None]; q = np.arange(128)[None, :]
    tri = (q >= k).astype(np.float32)
    return c64, pmat, ident, blk, tri, (1.0 - tri).astype(np.float32)


def _core_inputs(inputs, c, consts, stop_after=None):
    b, h = c // 2, c % 2
    c64, pmat, ident, blk, tri, tric = consts
    x = inputs["x"][b]
    posb = inputs["positions"][b]
    lo = slice(0, HALF)
    own = slice(h * HALF, (h + 1) * HALF)
    xin = np.concatenate([x[lo], x[own]], axis=0)
    pos = np.concatenate([posb[lo], posb[own]])[None, :].astype(np.int32)
    p0e = np.concatenate([inputs["p"][0, b][lo], inputs["p"][0, b][own]], axis=0)[SEQ - NTOK:]
    p1 = inputs["p"][1, b][own]
    gbias = np.zeros((17, 32), np.float32)
    cb = np.zeros((17, 32), np.float32)
    for jj in range(17):
        J = 15 + jj
        for n in range(32):
            valid = (n < J) and (n >= 16 or h == 1)
            gbias[jj, n] = 0.0 if valid else -1e9
            cb[jj, n] = NEG if valid else 3 * NEG
        gbias[jj, J] = -2e9
        cb[jj, J] = 0.0
    m = {
        "xin": np.ascontiguousarray(xin), "pos": pos, "p0": np.ascontiguousarray(p0e), "p1": np.ascontiguousarray(p1),
        "cst64": c64, "pmat": pmat, "ident": ident, "blkind": blk, "tri": tri, "tric": tric,
        "gbias": gbias.reshape(1, -1), "cb": cb.reshape(1, -1), "fv": np.full((128, 1), float(h), np.float32),
    }
    for k in ("w_qkv", "w_o", "ln_mix_g", "ln_mix_b", "ln_ffn_g", "ln_ffn_b", "sinks", "w_ffn_gate", "w_ffn_up",
              "w_ffn_down", "w_router", "w_exp_gate", "w_exp_up", "w_exp_down", "w_ple_proj", "w_ple_gate"):
        if k.startswith("w_exp") and not (stop_after is None or stop_after >= 5):
            continue
        m[k] = np.ascontiguousarray(inputs[k], dtype=np.float32)
    return m


def run(inputs, stop_after=None, debug=False, cores=8, trace=False):
    inputs = {k: np.asarray(v) for k, v in inputs.items()}
    consts = _consts()
    nc, es, trk = build_program(stop_after=stop_after, debug=debug)
    in_maps = [_core_inputs(inputs, c, consts, stop_after) for c in range(cores)]
    res = run_bass_kernel_spmd(nc, in_maps, core_ids=list(range(cores)), trace=trace)
    es.close()
    return res


def kernel(**inputs):
    res = run(inputs)
    out = np.zeros((4, SEQ, D), np.float32)
    for c in range(8):
        b, h = c // 2, c % 2
        out[b, h * HALF:(h + 1) * HALF] = res.results[c]["out"]
    return out
```

```python
from contextlib import ExitStack
import os
import numpy as np
import concourse.bass as bass
import concourse.mybir as mybir
from concourse.bass_utils import run_bass_kernel_spmd

F32 = mybir.dt.float32
BF16 = mybir.dt.bfloat16
I32 = mybir.dt.int32
AF = mybir.ActivationFunctionType
ALU = mybir.AluOpType
AX = mybir.AxisListType

D = 1024
SEQ = 8192
HALF = 4096
NT = 33
NTOK = NT * 128
DFF = 3584
NFB = 7
ALPHA = 4.0 ** 0.25
EPS = 1e-5
TWO_PI = 6.283185307179586
NEG = -30000.0


class Trk:
    def __init__(self, nc, es):
        self.nc = nc
        self.es = es
        self.eng = {"pe": nc.tensor, "act": nc.scalar, "dve": nc.vector, "pool": nc.gpsimd, "sp": nc.sync}
        self.sem = {}
        self.cnt = {}
        self.seen = {e: {} for e in self.eng}
        self.lw = {}
        self.rd = {}

    def _sem(self, k):
        if k not in self.sem:
            self.sem[k] = self.es.enter_context(self.nc.semaphore("s%d" % len(self.sem)))
            self.cnt[k] = 0
        return self.sem[k]

    def _waits(self, e, reads, writes, own_sk=None):
        need = {}

        def add(sk, v, raw):
            if sk == e and not raw:
                return
            if sk == own_sk and not raw:
                return
            if v > need.get(sk, 0):
                need[sk] = v

        for k in reads:
            if k in self.lw:
                add(self.lw[k][0], self.lw[k][1], True)
        for k in writes:
            if k in self.lw:
                add(self.lw[k][0], self.lw[k][1], False)
            for sk, v in self.rd.get(k, {}).items():
                add(sk, v, False)
        for sk, v in need.items():
            if self.seen[e].get(sk, 0) < v:
                self.eng[e].wait_ge(self.sem[sk], v)
                self.seen[e][sk] = v

    def _record(self, sk, v, reads, writes):
        for k in reads:
            d = self.rd.setdefault(k, {})
            if d.get(sk, 0) < v:
                d[sk] = v
        for k in writes:
            self.lw[k] = (sk, v)
            self.rd[k] = {}

    def op(self, e, fn, reads=(), writes=()):
        self._sem(e)
        extra = [k for k in reads if isinstance(k, str) and k.startswith("ps") and k not in writes]
        if extra:
            writes = list(writes) + extra
        self._waits(e, reads, writes)
        ins = fn(self.eng[e])
        ins.then_inc(self.sem[e], 1)
        self.cnt[e] += 1
        self._record(e, self.cnt[e], reads, writes)

    def dma(self, e, out, in_, reads=(), writes=(), sk=None):
        if sk is None:
            w0 = writes[0]
            if isinstance(w0, str) and (w0[0].isupper() or w0.startswith("dbg") or w0 == "out"):
                sk = ("st", reads[0])
            else:
                sk = ("d", w0)
        self._sem(sk)
        self._waits(e, reads, writes, own_sk=sk)
        ins = self.eng[e].dma_start(out=out, in_=in_)
        ins.then_inc(self.sem[sk], 16)
        self.cnt[sk] += 16
        self._record(sk, self.cnt[sk], reads, writes)

    def barrier(self):
        for e in self.eng:
            for sk, v in self.cnt.items():
                if v > 0 and self.seen[e].get(sk, 0) < v:
                    self.eng[e].wait_ge(self.sem[sk], v)
                    self.seen[e][sk] = v
        self.lw = {}
        self.rd = {}

    def final_wait(self, e, key):
        sk = ("d", key)
        self.eng[e].wait_ge(self.sem[sk], self.cnt[sk])


def mm(trk, out, pairs, reads, writes):
    def fn(pe):
        n = len(pairs)
        ins = None
        for i, (l, r) in enumerate(pairs):
            ins = pe.matmul(out, l, r, start=(i == 0), stop=(i == n - 1))
        return ins
    trk.op("pe", fn, reads, writes)


def transposes(trk, outs_ins, ident, reads, writes):
    def fn(pe):
        ins = None
        for o, i in outs_ins:
            ins = pe.transpose(o, i, ident)
        return ins
    trk.op("pe", fn, reads, writes)


def layer_norm(trk, P, y, yk, g_sb, b_sb, out, outk, slot):
    st6 = P["st6"][slot]
    mv = P["mv"][slot]
    k6 = "st6_%d" % slot
    kmv = "mv_%d" % slot
    krs = "rs_%d" % slot
    trk.op("dve", lambda v: v.bn_stats(out=st6[:, 0:6], in_=y[:, 0:512]), [yk], [k6 + "a"])
    trk.op("dve", lambda v: v.bn_stats(out=st6[:, 6:12], in_=y[:, 512:1024]), [yk], [k6 + "b"])
    trk.op("dve", lambda v: v.bn_aggr(out=mv[:, 0:2], in_=st6[:, 0:12]), [k6 + "a", k6 + "b"], [kmv])
    trk.op("act", lambda a: a.activation(out=mv[:, 2:3], in_=mv[:, 1:2], func=AF.Sqrt, bias=EPS), [kmv], [krs + "s"])
    trk.op("dve", lambda v: v.reciprocal(out=mv[:, 3:4], in_=mv[:, 2:3]), [krs + "s"], [krs])
    trk.op("dve", lambda v: v.tensor_scalar(out=out[:, :], in0=y[:, :], scalar1=mv[:, 0:1], scalar2=mv[:, 3:4],
                                            op0=ALU.subtract, op1=ALU.mult), [yk, kmv, krs], [outk])
    trk.op("dve", lambda v: v.tensor_tensor(out=out[:, :], in0=out[:, :], in1=g_sb[:, :], op=ALU.mult), [outk, "lng"], [outk])
    trk.op("dve", lambda v: v.tensor_tensor(out=out[:, :], in0=out[:, :], in1=b_sb[:, :], op=ALU.add), [outk, "lnb"], [outk])


def build_program(stop_after=None, debug=False):
    nc = bass.Bass("TRN2", target_bir_lowering=False)
    es = ExitStack()
    trk = Trk(nc, es)

    def din(name, shape, dt=F32):
        return nc.dram_tensor(name, list(shape), dt, kind="ExternalInput").ap()

    def dscr(name, shape, dt):
        kind = "ExternalOutput" if debug else "Internal"
        return nc.dram_tensor(name, list(shape), dt, kind=kind).ap()

    xin = din("xin", [SEQ, D])
    pos = din("pos", [1, SEQ], I32)
    p0 = din("p0", [NTOK, 256])
    p1 = din("p1", [HALF, 256])
    cst64 = din("cst64", [64, 4])
    pmat_d = din("pmat", [64, 64])
    ident_d = din("ident", [128, 128])
    blkind = din("blkind", [32, SEQ])
    tri_d = din("tri", [128, 128])
    tric_d = din("tric", [128, 128])
    gbias_d = din("gbias", [1, 17 * 32])
    cb_d = din("cb", [1, 17 * 32])
    fv_d = din("fv", [128, 1])
    w_qkv = din("w_qkv", [2, D, 1536])
    w_o = din("w_o", [2, D, D])
    ln_mix_g = din("ln_mix_g", [2, D])
    ln_mix_b = din("ln_mix_b", [2, D])
    ln_ffn_g = din("ln_ffn_g", [2, D])
    ln_ffn_b = din("ln_ffn_b", [2, D])
    sinks = din("sinks", [1, 16])
    w_ffn_gate = din("w_ffn_gate", [1, D, DFF])
    w_ffn_up = din("w_ffn_up", [1, D, DFF])
    w_ffn_down = din("w_ffn_down", [1, DFF, D])
    w_router = din("w_router", [1, D, 8])
    if stop_after is None or stop_after >= 5:
        w_exp_gate = din("w_exp_gate", [1, 8, D, DFF])
        w_exp_up = din("w_exp_up", [1, 8, D, DFF])
        w_exp_down = din("w_exp_down", [1, 8, DFF, D])
    w_ple_proj = din("w_ple_proj", [2, 256, D])
    w_ple_gate = din("w_ple_gate", [2, D, D])
    out_d = nc.dram_tensor("out", [HALF, D], F32, kind="ExternalOutput").ap()

    CT = dscr("CT", [64, SEQ], F32)
    ST = dscr("ST", [64, SEQ], F32)
    QA = dscr("QA", [96, NT, 16 * 128], BF16)
    X1 = dscr("X1", [NTOK, D], F32)
    X1T = dscr("X1T", [128, 8, NTOK], BF16)
    X3 = dscr("X3", [NTOK, D], F32)
    X3T = dscr("X3T", [128, 8, NTOK], BF16)
    Q1 = dscr("Q1", [64, 32, 16 * 128], BF16)
    X4 = dscr("X4", [HALF, D], F32)
    X4T = dscr("X4T", [128, 8, HALF], BF16)
    GATES = dscr("GATES", [128, 32, 8], F32)

    uid = [0]

    def sb(stk, name, shape, dt):
        uid[0] += 1
        return stk.enter_context(nc.sbuf_tensor("s%d_%s" % (uid[0], name), list(shape), dt))

    def ps(stk, name, shape, dt=F32):
        uid[0] += 1
        return stk.enter_context(nc.psum_tensor("p%d_%s" % (uid[0], name), list(shape), dt))

    def done():
        trk.barrier()

    c64 = sb(es, "c64", [64, 4], F32)
    pm = sb(es, "pm", [64, 64], BF16)
    identb = sb(es, "identb", [128, 128], BF16)
    identf = sb(es, "identf", [128, 128], F32)
    trib = sb(es, "trib", [128, 128], BF16)
    tricb = sb(es, "tricb", [128, 128], BF16)
    tricfv = sb(es, "tricfv", [128, 128], BF16)
    fv = sb(es, "fv", [128, 1], F32)
    trk.dma("sp", c64[:, :], cst64[:, :], [], ["c64"])
    trk.dma("pool", pm[:, :], pmat_d[:, :], [], ["pm"])
    trk.dma("pool", identb[:, :], ident_d[:, :], [], ["identb"])
    trk.dma("sp", identf[:, :], ident_d[:, :], [], ["identf"])
    trk.dma("pool", trib[:, :], tri_d[:, :], [], ["trib"])
    trk.dma("pool", tricb[:, :], tric_d[:, :], [], ["tricb"])
    trk.dma("sp", fv[:, :], fv_d[:, :], [], ["fv"])
    trk.op("dve", lambda v: v.tensor_scalar(out=tricfv[:, :], in0=tricb[:, :], scalar1=fv[:, 0:1], scalar2=None,
                                            op0=ALU.mult), ["tricb", "fv"], ["tricfv"])

    if stop_after == -1:
        done()
        return nc, es, trk
    with ExitStack() as ph:
        CW = 2048
        pi = [sb(ph, "r_pi%d" % i, [64, CW], I32) for i in range(2)]
        u = [sb(ph, "r_u%d" % i, [64, CW], F32) for i in range(2)]
        ni = [sb(ph, "r_ni%d" % i, [64, CW], I32) for i in range(2)]
        nf = [sb(ph, "r_nf%d" % i, [64, CW], F32) for i in range(2)]
        to = [sb(ph, "r_to%d" % i, [64, CW], F32) for i in range(2)]
        it = 0
        for c in range(SEQ // CW):
            cols = slice(c * CW, (c + 1) * CW)
            s = c % 2
            trk.dma("sp", pi[s][:, :], pos[0:1, cols].partition_broadcast(64), [], ["r_pi%d" % s])
            trk.op("dve", lambda v: v.tensor_copy(out=u[s][:, :], in_=pi[s][:, :]), ["r_pi%d" % s], ["r_u%d" % s])
            trk.op("dve", lambda v: v.tensor_scalar(out=u[s][:, :], in0=u[s][:, :], scalar1=c64[:, 0:1], scalar2=None,
                                                    op0=ALU.mult), ["r_u%d" % s, "c64"], ["r_u%d" % s])
            for which in range(2):
                k = it % 2
                it += 1
                if which == 1:
                    trk.op("dve", lambda v: v.tensor_scalar(out=u[s][:, :], in0=u[s][:, :], scalar1=0.25, scalar2=None,
                                                            op0=ALU.add), ["r_u%d" % s], ["r_u%d" % s])
                trk.op("dve", lambda v: v.tensor_copy(out=ni[k][:, :], in_=u[s][:, :]), ["r_u%d" % s], ["r_ni%d" % k])
                trk.op("dve", lambda v: v.tensor_copy(out=nf[k][:, :], in_=ni[k][:, :]), ["r_ni%d" % k], ["r_nf%d" % k])
                trk.op("dve", lambda v: v.tensor_tensor(out=nf[k][:, :], in0=u[s][:, :], in1=nf[k][:, :], op=ALU.subtract),
                       ["r_u%d" % s, "r_nf%d" % k], ["r_nf%d" % k])
                PH0 = int(os.environ.get("PH0", "9"))
                if PH0 == 1:
                    trk.dma("sp", (ST if which == 0 else CT)[:, cols], nf[k][:, :], ["r_nf%d" % k], ["ST" if which == 0 else "CT"])
                    continue
                if PH0 == 2:
                    trk.op("act", lambda a: a.activation(out=to[k][:, :], in_=nf[k][:, :], func=AF.Sin, scale=TWO_PI),
                           ["r_nf%d" % k], ["r_to%d" % k])
                    trk.dma("sp", (ST if which == 0 else CT)[:, cols], to[k][:, :], ["r_to%d" % k], ["ST" if which == 0 else "CT"])
                    continue
                if which == 0:
                    trk.op("dve", lambda v: v.tensor_scalar(out=nf[k][:, :], in0=nf[k][:, :], scalar1=c64[:, 2:3], scalar2=None,
                                                            op0=ALU.mult), ["r_nf%d" % k, "c64"], ["r_nf%d" % k])
                    trk.op("act", lambda a: a.activation(out=to[k][:, :], in_=nf[k][:, :], func=AF.Sin, scale=TWO_PI),
                           ["r_nf%d" % k], ["r_to%d" % k])
                    trk.dma("sp", ST[:, cols], to[k][:, :], ["r_to%d" % k], ["ST"])
                else:
                    trk.op("act", lambda a: a.activation(out=to[k][:, :], in_=nf[k][:, :], func=AF.Sin, scale=TWO_PI),
                           ["r_nf%d" % k], ["r_to%d" % k])
                    trk.dma("sp", CT[:, cols], to[k][:, :], ["r_to%d" % k], ["CT"])
        done()
    if stop_after == 0:
        return nc, es, trk

    def rope_head(P, psrc, psk, ncols, scale, ct_ap, st_ap, ctk, out_ap, outk, slot):
        t1 = P["t1"][slot]
        t2 = P["t2"][slot]
        krb = P["krb"][slot]
        psP = P["psP"][slot]
        k1, k2, kk, kp = "t1_%d" % slot, "t2_%d" % slot, "krb_%d" % slot, "psP_%d" % slot
        trk.op("dve", lambda v: v.scalar_tensor_tensor(out=t1[:, 0:ncols], in0=psrc, scalar=scale, in1=ct_ap,
                                                       op0=ALU.mult, op1=ALU.mult), [psk, ctk], [k1])
        trk.op("act", lambda a: a.activation(out=krb[:, 0:ncols], in_=psrc, func=AF.Copy, scale=scale), [psk], [kk])
        mm(trk, psP[:, 0:ncols], [(pm[:, :], krb[:, 0:ncols])], [kk, "pm"], [kp])
        trk.op("dve", lambda v: v.tensor_tensor(out=t2[:, 0:ncols], in0=psP[:, 0:ncols], in1=st_ap, op=ALU.mult),
               [kp, ctk], [k2])
        trk.op("dve", out_ap_writer(out_ap, t1, t2, ncols), [k1, k2], [outk])

    def out_ap_writer(out_ap, t1, t2, ncols):
        nd = len(out_ap.shape)
        if nd == 2:
            return lambda v: v.tensor_tensor(out=out_ap, in0=t1[:, 0:ncols], in1=t2[:, 0:ncols], op=ALU.add)
        nsub = out_ap.shape[1]
        a = t1[:, 0:ncols].rearrange("p (s c) -> p s c", s=nsub)
        b = t2[:, 0:ncols].rearrange("p (s c) -> p s c", s=nsub)
        return lambda v: v.tensor_tensor(out=out_ap, in0=a, in1=b, op=ALU.add)

    def load_w(eng, dst, src, key, nsplit=1):
        v = src.rearrange("(k p) n -> p k n", p=128)
        kc = v.shape[1]
        step = kc // nsplit
        for i in range(nsplit):
            trk.dma(eng, dst[:, i * step:(i + 1) * step, :], v[:, i * step:(i + 1) * step, :], [], [key], sk=("d", key))

    with ExitStack() as kv:
        kaug = sb(kv, "kaug", [96, 4, SEQ], BF16)
        vaug = sb(kv, "vaug", [128, 64, 4, 128], BF16)
        with ExitStack() as ph:
            wqkv = sb(ph, "wqkv", [128, 8, 1536], BF16)
            load_w("pool", wqkv, w_qkv[0], "wqkv", nsplit=4)
            for kvh in range(4):
                trk.dma("pool", kaug[64:96, kvh, :], blkind[:, :], [], ["kaug_ind"], sk=("d", "kaug_ind"))
            trk.op("pool", lambda g: g.memset(vaug[:, :, :, :], 1.0), [], ["vaug_init"])
            kms = sb(ph, "kms", [64, 4, 32], F32)
            kmb = sb(ph, "kmb", [64, 4, 32], BF16)
            trk.op("pool", lambda g: g.memset(kms[:, :, :], 0.0), [], ["kms"])
            trk.op("pool", lambda g: g.memset(kmb[:, :, :], 0.0), [], ["kmb"])
            gbias_sb = sb(ph, "gbias_sb", [128, 17, 32], F32)
            cb_sb = sb(ph, "cb_sb", [128, 17, 32], F32)
            trk.dma("sp", gbias_sb[:, :, :].rearrange("p a b -> p (a b)"), gbias_d.partition_broadcast(128), [], ["gbias"])
            trk.dma("sp", cb_sb[:, :, :].rearrange("p a b -> p (a b)"), cb_d.partition_broadcast(128), [], ["cb"])
            xb = [sb(ph, "xb%d" % i, [128, D], BF16) for i in range(2)]
            XT = [sb(ph, "XT%d" % i, [128, 8, 512], BF16) for i in range(1)] * 2
            ctt = [sb(ph, "ct%d" % i, [64, 512], F32) for i in range(1)] * 2
            stt = [sb(ph, "st%d" % i, [64, 512], F32) for i in range(1)] * 2
            P = {
                "t1": [sb(ph, "t1_%d" % i, [64, 512], F32) for i in range(1)] * 2,
                "t2": [sb(ph, "t2_%d" % i, [64, 512], F32) for i in range(1)] * 2,
                "krb": [sb(ph, "krb_%d" % i, [64, 512], BF16) for i in range(2)],
                "psP": [ps(ph, "psP_%d" % i, [64, 512]) for i in range(2)],
            }
            qa = [sb(ph, "qa%d" % i, [96, 4, 16, 128], BF16) for i in range(1)] * 2
            gb = sb(ph, "gb", [128, 16, 32], F32)
            m8 = sb(ph, "m8", [128, 16, 8], F32)
            sel = sb(ph, "sel", [128, 16, 32], F32)
            stage = sb(ph, "stage", [128, 16, 128], BF16)
            trk.op("pool", lambda g: g.memset(stage[:, :, :], 0.0), [], ["stage"])
            psT = [ps(ph, "psT%d" % i, [128, 1024], BF16) for i in range(1)]
            psK = [ps(ph, "psK%d" % i, [64, 512]) for i in range(2)]
            psV = ps(ph, "psV", [128, 512])
            psG = ps(ph, "psG", [128, 512])
            psM = ps(ph, "psM", [128, 8, 128], BF16)
            xi = 0
            ki = 0
            PHA = int(os.environ.get("PHA", "99"))
            for g in range(16 if PHA > 5 else (1 if PHA > 1 else 0)):
                gs = 0
                gcols = slice(g * 512, (g + 1) * 512)
                trk.dma("sp", ctt[gs][:, :], CT[:, gcols], ["CT"], ["ct0"])
                trk.dma("sp", stt[gs][:, :], ST[:, gcols], ["ST"], ["st0"])
                ctk = "ctst%d" % gs
                for sub in range(4):
                    sl = xi % 2
                    tp = 0
                    xi += 1
                    r0 = g * 512 + sub * 128
                    trk.dma("pool", xb[sl][:, :], xin[r0:r0 + 128, :], [], ["xb%d" % sl])
                    transposes(trk, [(psT[tp][:, kc * 128:(kc + 1) * 128], xb[sl][:, kc * 128:(kc + 1) * 128]) for kc in range(8)],
                               identb[:, :], ["xb%d" % sl, "identb"], ["psT%d" % tp])
                    trk.op("act", lambda a: a.activation(out=XT[gs][:, :, sub * 128:(sub + 1) * 128],
                                                         in_=psT[tp][:, :].rearrange("p (k c) -> p k c", k=8), func=AF.Copy),
                           ["psT%d" % tp], ["XT%d_%d" % (gs, sub)])
                xtk = ["XT%d_%d" % (gs, s_) for s_ in range(4)]
                for kvh in range(4 if PHA >= 3 else 0):
                    s2 = ki % 2
                    ki += 1
                    c0 = 1024 + kvh * 64
                    mm(trk, psK[s2][:, :], [(wqkv[:, kc, c0:c0 + 64], XT[gs][:, kc, :]) for kc in range(8)],
                       xtk + ["wqkv"], ["psK%d" % s2])
                    rope_head_keys = ["ct0", "st0"]
                    t1 = P["t1"][s2]; t2 = P["t2"][s2]; krb = P["krb"][s2]; psP = P["psP"][s2]
                    trk.op("dve", lambda v: v.tensor_tensor(out=t1[:, :], in0=psK[s2][:, :], in1=ctt[gs][:, :], op=ALU.mult),
                           ["psK%d" % s2, "ct0"], ["t1_0"])
                    trk.op("act", lambda a: a.activation(out=krb[:, :], in_=psK[s2][:, :], func=AF.Copy),
                           ["psK%d" % s2], ["krb_%d" % s2])
                    mm(trk, psP[:, :], [(pm[:, :], krb[:, :])], ["krb_%d" % s2, "pm"], ["psP_%d" % s2])
                    trk.op("dve", lambda v: v.tensor_tensor(out=t2[:, :], in0=psP[:, :], in1=stt[gs][:, :], op=ALU.mult),
                           ["psP_%d" % s2, "st0"], ["t2_0"])
                    trk.op("dve", lambda v: v.tensor_tensor(out=kaug[0:64, kvh, gcols], in0=t1[:, :], in1=t2[:, :], op=ALU.add),
                           ["t1_0", "t2_0"], ["kaug_%d_%d" % (g, kvh)])
                for sub in range(4 if PHA >= 4 else 0):
                    mm(trk, psV[:, 0:256], [(XT[gs][:, kc, sub * 128:(sub + 1) * 128], wqkv[:, kc, 1280:1536]) for kc in range(8)],
                       xtk + ["wqkv"], ["psV"])
                    trk.op("act", lambda a: a.activation(out=vaug[:, g * 4 + sub, :, 0:64],
                                                         in_=psV[:, 0:256].rearrange("p (h d) -> p h d", h=4), func=AF.Copy),
                           ["psV", "vaug_init"], ["vaug_%d" % (g * 4 + sub)])
                if PHA < 5:
                    continue
                trk.op("dve", lambda v: v.tensor_reduce(out=kms[:, :, 2 * g:2 * g + 2],
                                                        in_=kaug[0:64, :, gcols].rearrange("p h (b c) -> p h b c", b=2),
                                                        axis=AX.X, op=ALU.add),
                       ["kaug_%d_%d" % (g, k_) for k_ in range(4)] + ["kms"], ["kms"])
                trk.op("dve", lambda v: v.tensor_copy(out=kmb[:, :, :], in_=kms[:, :, :]), ["kms"], ["kmb"])
                if g < 7 or PHA < 7:
                    continue
                subs = [3] if g == 7 else [0, 1, 2, 3]
                c0q = subs[0] * 128
                ncols = len(subs) * 128
                qs = 0
                for h in range(16):
                    s2 = ki % 2
                    ki += 1
                    mm(trk, psK[s2][:, 0:ncols], [(wqkv[:, kc, h * 64:(h + 1) * 64], XT[gs][:, kc, c0q:512]) for kc in range(8)],
                       xtk + ["wqkv"], ["psK%d" % s2])
                    t1 = P["t1"][s2]; t2 = P["t2"][s2]; krb = P["krb"][s2]; psP = P["psP"][s2]
                    trk.op("dve", lambda v: v.scalar_tensor_tensor(out=t1[:, 0:ncols], in0=psK[s2][:, 0:ncols], scalar=0.125,
                                                                   in1=ctt[gs][:, c0q:512], op0=ALU.mult, op1=ALU.mult),
                           ["psK%d" % s2, "ct0"], ["t1_0"])
                    trk.op("act", lambda a: a.activation(out=krb[:, 0:ncols], in_=psK[s2][:, 0:ncols], func=AF.Copy, scale=0.125),
                           ["psK%d" % s2], ["krb_%d" % s2])
                    mm(trk, psP[:, 0:ncols], [(pm[:, :], krb[:, 0:ncols])], ["krb_%d" % s2, "pm"], ["psP_%d" % s2])
                    trk.op("dve", lambda v: v.tensor_tensor(out=t2[:, 0:ncols], in0=psP[:, 0:ncols], in1=stt[gs][:, c0q:512], op=ALU.mult),
                           ["psP_%d" % s2, "st0"], ["t2_0"])
                    trk.op("dve", lambda v: v.tensor_tensor(out=qa[qs][0:64, subs[0]:subs[-1] + 1, h, :],
                                                            in0=t1[:, 0:ncols].rearrange("p (s c) -> p s c", c=128),
                                                            in1=t2[:, 0:ncols].rearrange("p (s c) -> p s c", c=128), op=ALU.add),
                           ["t1_0", "t2_0"], ["qa%d_q" % qs])
                for sub in (subs if PHA >= 8 else []):
                    et = g * 4 + sub
                    jj = et // 2 - 15
                    def gate_mm(pe):
                        ins = None
                        for h in range(16):
                            ins = pe.matmul(psG[:, h * 32:(h + 1) * 32], qa[qs][0:64, sub, h, :], kmb[0:64, h // 4, :],
                                            start=True, stop=True)
                        return ins
                    trk.op("pe", gate_mm, ["qa%d_q" % qs, "kmb"], ["psG"])
                    trk.op("dve", lambda v: v.tensor_tensor(out=gb[:, :, :], in0=psG[:, :].rearrange("p (h n) -> p h n", h=16),
                                                            in1=gbias_sb[:, jj, :].unsqueeze(1).broadcast_to([128, 16, 32]), op=ALU.add),
                           ["psG", "gbias"], ["gb"])
                    for h in range(16):
                        trk.op("dve", lambda v: v.max(out=m8[:, h, :], in_=gb[:, h, :]), ["gb"], ["m8_%d" % h])
                    for h in range(16):
                        trk.op("dve", lambda v: v.tensor_scalar(out=sel[:, h, :], in0=gb[:, h, :], scalar1=m8[:, h, 2:3], scalar2=-NEG,
                                                                op0=ALU.is_ge, op1=ALU.mult), ["gb", "m8_%d" % h], ["sel_%d" % h])
                    trk.op("dve", lambda v: v.tensor_tensor(out=stage[:, :, 64:96], in0=sel[:, :, :],
                                                            in1=cb_sb[:, jj, :].unsqueeze(1).broadcast_to([128, 16, 32]), op=ALU.add),
                           ["sel_%d" % h for h in range(16)] + ["cb", "stage"], ["stage"])
                    for rnd in range(2):
                        transposes(trk, [(psM[:, hh, :], stage[:, rnd * 8 + hh, :]) for hh in range(8)], identb[:, :],
                                   ["stage", "identb"], ["psM"])
                        trk.op("act", lambda a: a.activation(out=qa[qs][64:96, sub, rnd * 8:(rnd + 1) * 8, :], in_=psM[64:96, :, :], func=AF.Copy),
                               ["psM"], ["qa%d_m%d_%d" % (qs, sub, rnd)])
                qkeys = ["qa%d_q" % qs] + (["qa%d_m%d_%d" % (qs, s_, r_) for s_ in subs for r_ in range(2)] if PHA >= 8 else [])
                if g == 7:
                    trk.dma("sp", QA[:, 0, :], qa[qs][:, 3, :, :].rearrange("p h c -> p (h c)"), qkeys, ["QA"])
                else:
                    t0 = 1 + 4 * (g - 8)
                    trk.dma("sp", QA[:, t0:t0 + 4, :], qa[qs][:, :, :, :].rearrange("p s h c -> p s (h c)"), qkeys, ["QA"])
            done()
            if debug:
                dk = nc.dram_tensor("dbg_kaug", [96, 4, SEQ], BF16, kind="ExternalOutput").ap()
                dv_ = nc.dram_tensor("dbg_vaug", [128, 64, 4, 128], BF16, kind="ExternalOutput").ap()
                trk.dma("sp", dk[:, :, :], kaug[:, :, :], [], ["dbg_k"])
                trk.dma("sp", dv_[:, :, :, :], vaug[:, :, :, :], [], ["dbg_v"])
                done()
        if stop_after == 1:
            return nc, es, trk
        with ExitStack() as ph:
            wo = sb(ph, "wo", [128, 8, D], BF16)
            load_w("pool", wo, w_o[0], "wo", nsplit=2)
            lng = sb(ph, "lng", [128, D], F32)
            lnb = sb(ph, "lnb", [128, D], F32)
            trk.dma("sp", lng[:, :], ln_mix_g[0:1, :].partition_broadcast(128), [], ["lng"])
            trk.dma("sp", lnb[:, :], ln_mix_b[0:1, :].partition_broadcast(128), [], ["lnb"])
            qt = [sb(ph, "qt%d" % i, [96, 2048], BF16) for i in range(2)]
            PT = [sb(ph, "PT%d" % i, [128, 512], BF16) for i in range(3)]
            rden = [sb(ph, "rden%d" % i, [128, 512], F32) for i in range(2)]
            aT = [sb(ph, "aT%d" % i, [128, 8, 128], BF16) for i in range(2)]
            xf = [sb(ph, "xf%d" % i, [128, D], F32) for i in range(2)]
            yb = [sb(ph, "y%d" % i, [128, D], F32) for i in range(2)]
            x1b = [sb(ph, "x1b%d" % i, [128, D], BF16) for i in range(2)]
            x1T = [sb(ph, "x1T%d" % i, [128, 8, 128], BF16) for i in range(2)]
            LNP = {"st6": [sb(ph, "st6_%d" % i, [128, 12], F32) for i in range(2)],
                   "mv": [sb(ph, "mv_%d" % i, [128, 4], F32) for i in range(2)]}
            psS = [ps(ph, "psS%d" % i, [128, 512]) for i in range(3)]
            psO = [ps(ph, "psO%d" % i, [128, 512]) for i in range(2)]
            psW = ps(ph, "psW", [128, 2, 512])
            psT2 = ps(ph, "psT2", [128, 1024], BF16)
            cnt = {"s": 0, "p": 0, "o": 0}
            if debug:
                dbg_aT = nc.dram_tensor("dbg_aT", [NT, 128, 8, 128], BF16, kind="ExternalOutput").ap()
                dbg_rd = nc.dram_tensor("dbg_rd", [NT, 4, 128, 512], F32, kind="ExternalOutput").ap()

            def attention(ti):
                sl = ti % 2
                et = 31 + ti
                J, s_ = et // 2, et % 2
                trk.dma("sp", qt[sl][:, :], QA[:, ti, :], ["QA"], ["qt%d" % sl])
                r0 = 3968 + 128 * ti
                trk.dma("sp", xf[sl][:, :], xin[r0:r0 + 128, :], [], ["xf%d" % sl])
                ktiles = [(kt, False) for kt in range(2 * J)]
                if s_ == 0:
                    ktiles.append((2 * J, True))
                else:
                    ktiles.append((2 * J, False))
                    ktiles.append((2 * J + 1, True))
                steps = []
                for kvh in range(4):
                    so = cnt["o"] % 2
                    cnt["o"] += 1
                    n = len(ktiles)
                    for idx, (kt, diag) in enumerate(ktiles):
                        ss = cnt["s"] % 3
                        cnt["s"] += 1
                        pp = cnt["p"] % 3
                        cnt["p"] += 1
                        steps.append((kvh, so, idx, n, kt, diag, ss, pp))

                def S(j):
                    kvh, so, idx, n, kt, diag, ss, pp = steps[j]
                    mm(trk, psS[ss][:, :], [(kaug[0:96, kvh, kt * 128:(kt + 1) * 128], qt[sl][0:96, kvh * 512:(kvh + 1) * 512])],
                       ["qt%d" % sl], ["psS%d" % ss])

                def E(j):
                    kvh, so, idx, n, kt, diag, ss, pp = steps[j]
                    trk.op("act", lambda a: a.activation(out=PT[pp][:, :], in_=psS[ss][:, :], func=AF.Exp),
                           ["psS%d" % ss], ["PT%d" % pp])
                    if diag:
                        trk.op("dve", lambda v: v.tensor_tensor(out=PT[pp][:, :].rearrange("p (h q) -> p h q", h=4),
                                                                in0=PT[pp][:, :].rearrange("p (h q) -> p h q", h=4),
                                                                in1=trib[:, :].unsqueeze(1).broadcast_to([128, 4, 128]), op=ALU.mult),
                               ["PT%d" % pp, "trib"], ["PT%d" % pp])

                def PV(j):
                    kvh, so, idx, n, kt, diag, ss, pp = steps[j]
                    def pv(pe):
                        return pe.matmul(psO[so][:, :], vaug[:, kt, kvh, :], PT[pp][:, :], start=(idx == 0), stop=(idx == n - 1))
                    trk.op("pe", pv, ["PT%d" % pp], ["psO%d" % so])
                    if idx != n - 1:
                        return
                    rs = so
                    trk.op("dve", lambda v: v.reciprocal(out=rden[rs][64:128, :], in_=psO[so][64:128, :]), ["psO%d" % so], ["rden%d" % rs])
                    if debug:
                        trk.dma("sp", dbg_rd[ti, kvh, 64:128, :], rden[rs][64:128, :], ["rden%d" % rs], ["dbg_rd"])
                    for i in range(4):
                        pb = (i % 2) * 64
                        trk.op("dve", lambda v: v.tensor_tensor(out=aT[sl][pb:pb + 64, kvh * 2 + i // 2, :],
                                                                in0=psO[so][0:64, i * 128:(i + 1) * 128],
                                                                in1=rden[rs][64:128, i * 128:(i + 1) * 128], op=ALU.mult),
                               ["psO%d" % so, "rden%d" % rs], ["aT%d_%d_%d" % (sl, kvh, i)])

                N = len(steps)
                S(0)
                if N > 1:
                    S(1)
                for j in range(N):
                    E(j)
                    if j + 2 < N:
                        S(j + 2)
                    PV(j)


            def tail(ti):
                sl = ti % 2
                akeys = ["aT%d_%d_%d" % (sl, k_, i_) for k_ in range(4) for i_ in range(4)]
                if debug:
                    trk.dma("sp", dbg_aT[ti], aT[sl][:, :, :], akeys, ["dbg_aT"])
                for half in range(2):
                    mm(trk, psW[:, half, :], [(aT[sl][:, c, :], wo[:, c, half * 512:(half + 1) * 512]) for c in range(8)],
                       akeys + ["wo"], ["psW"])
                for half in range(2):
                    trk.op("dve", lambda v: v.scalar_tensor_tensor(out=yb[sl][:, half * 512:(half + 1) * 512],
                                                                   in0=xf[sl][:, half * 512:(half + 1) * 512], scalar=ALPHA,
                                                                   in1=psW[:, half, :], op0=ALU.mult, op1=ALU.add),
                           ["xf%d" % sl, "psW"], ["y%d" % sl])
                layer_norm(trk, LNP, yb[sl], "y%d" % sl, lng, lnb, yb[sl], "y%d" % sl, sl)
                trk.dma("sp", X1[ti * 128:(ti + 1) * 128, :], yb[sl][:, :], ["y%d" % sl], ["X1"])
                trk.op("act", lambda a: a.activation(out=x1b[sl][:, :], in_=yb[sl][:, :], func=AF.Copy), ["y%d" % sl], ["x1b%d" % sl])
                transposes(trk, [(psT2[:, kc * 128:(kc + 1) * 128], x1b[sl][:, kc * 128:(kc + 1) * 128]) for kc in range(8)],
                           identb[:, :], ["x1b%d" % sl, "identb"], ["psT2"])
                trk.op("act", lambda a: a.activation(out=x1T[sl][:, :, :], in_=psT2[:, :].rearrange("p (k c) -> p k c", k=8), func=AF.Copy),
                       ["psT2"], ["x1T%d" % sl])
                trk.dma("sp", X1T[:, :, ti * 128:(ti + 1) * 128], x1T[sl][:, :, :], ["x1T%d" % sl], ["X1T"])

            NTB = int(os.environ.get("NTB", str(NT)))
            for ti in range(NTB):
                attention(ti)
                if ti >= 1:
                    tail(ti - 1)
            tail(NTB - 1)
            done()
    if stop_after == 2:
        return nc, es, trk

    def ffn_layer(layer, XTd, Xd, pd, chunks, experts, use_gates, Yd, YTd):
        for (t0, nt) in chunks:
            with ExitStack() as ck:
                ncol = nt * 128
                XTc = sb(ck, "XTc", [128, 8, ncol], BF16)
                acc = sb(ck, "acc", [128, nt, D], F32)
                for kc in range(8):
                    trk.dma("sp", XTc[:, kc, :], XTd[:, kc, t0 * 128:t0 * 128 + ncol], [], ["XTc"], sk=("d", "XTc"))
                for t in range(nt):
                    r0 = (t0 + t) * 128
                    trk.dma("sp", acc[:, t, :], Xd[r0:r0 + 128, :], [], ["accld"], sk=("d", "accld"))
                for t in range(nt):
                    trk.op("act", lambda a: a.mul(out=acc[:, t, :], in_=acc[:, t, :], mul=ALPHA), ["accld"], ["acc%d" % t])
                if use_gates:
                    gt = sb(ck, "gt", [128, nt, 8], F32)
                    trk.dma("sp", gt[:, :, :], GATES[:, t0:t0 + nt, :], [], ["gt"])
                with ExitStack() as ph:
                    wg = [sb(ph, "wg%d" % i, [128, 8, 512], BF16) for i in range(2)]
                    wu = [sb(ph, "wu%d" % i, [128, 8, 512], BF16) for i in range(2)]
                    wd = [sb(ph, "wd%d" % i, [128, 4, D], BF16) for i in range(2)]
                    sg = [sb(ph, "sg%d" % i, [128, 512], F32) for i in range(2)]
                    hh = [sb(ph, "hh%d" % i, [128, 4, 512], BF16) for i in range(2)]
                    psG = [ps(ph, "psG%d" % i, [128, 512]) for i in range(2)]
                    psU = [ps(ph, "psU%d" % i, [128, 512]) for i in range(2)]
                    psY = [ps(ph, "psY%d" % i, [128, 2, 512]) for i in range(2)]
                    steps = [(e, fb) for e in range(len(experts)) for fb in range(NFB)]

                    def load(i):
                        e, fb = steps[i]
                        sl = i % 2
                        wg_ap, wu_ap, wd_ap = experts[e]
                        fs = slice(fb * 512, (fb + 1) * 512)
                        trk.dma("pool", wg[sl][:, :, :], wg_ap.rearrange("(k p) n -> p k n", p=128)[:, :, fs], [], ["wg%d" % sl])
                        trk.dma("pool", wu[sl][:, :, :], wu_ap.rearrange("(k p) n -> p k n", p=128)[:, :, fs], [], ["wu%d" % sl])
                        trk.dma("pool", wd[sl][:, :, :], wd_ap[fs, :].rearrange("(c p) n -> p c n", p=128), [], ["wd%d" % sl])

                    groups = []
                    tt = 0
                    while tt < nt:
                        gn = min(4, nt - tt)
                        groups.append((tt, gn))
                        tt += gn
                    c1 = 0
                    c2 = 0
                    c3 = 0
                    load(0)
                    for i, (e, fb) in enumerate(steps):
                        if i + 1 < len(steps):
                            load(i + 1)
                        sl = i % 2
                        for (g0, gn) in groups:
                            cols = slice(g0 * 128, (g0 + gn) * 128)
                            ncl = gn * 128
                            hs = c2 % 2
                            c2 += 1
                            for fc in range(4):
                                a_ = c1 % 2
                                c1 += 1
                                fsl = slice(fc * 128, (fc + 1) * 128)
                                mm(trk, psG[a_][:, 0:ncl], [(wg[sl][:, kc, fsl], XTc[:, kc, cols]) for kc in range(8)],
                                   ["wg%d" % sl, "XTc"], ["psG%d" % a_])
                                mm(trk, psU[a_][:, 0:ncl], [(wu[sl][:, kc, fsl], XTc[:, kc, cols]) for kc in range(8)],
                                   ["wu%d" % sl, "XTc"], ["psU%d" % a_])
                                trk.op("act", lambda a: a.activation(out=sg[a_][:, 0:ncl], in_=psG[a_][:, 0:ncl], func=AF.Silu),
                                       ["psG%d" % a_], ["sg%d" % a_])
                                trk.op("dve", lambda v: v.tensor_tensor(out=hh[hs][:, fc, 0:ncl], in0=sg[a_][:, 0:ncl], in1=psU[a_][:, 0:ncl],
                                                                        op=ALU.mult), ["sg%d" % a_, "psU%d" % a_], ["hh%d_%d" % (hs, fc)])
                            hkeys = ["hh%d_%d" % (hs, fc) for fc in range(4)]
                            for tl in range(gn):
                                t = g0 + tl
                                ys = c3 % 2
                                c3 += 1
                                for half in range(2):
                                    mm(trk, psY[ys][:, half, :], [(hh[hs][:, fc, tl * 128:(tl + 1) * 128], wd[sl][:, fc, half * 512:(half + 1) * 512])
                                                                 for fc in range(4)], hkeys + ["wd%d" % sl], ["psY%d" % ys])
                                for half in range(2):
                                    hsl = slice(half * 512, (half + 1) * 512)
                                    if use_gates:
                                        trk.op("dve", lambda v: v.scalar_tensor_tensor(out=acc[:, t, hsl], in0=psY[ys][:, half, :],
                                                                                       scalar=gt[:, t, e:e + 1], in1=acc[:, t, hsl],
                                                                                       op0=ALU.mult, op1=ALU.add),
                                               ["psY%d" % ys, "acc%d" % t, "gt"], ["acc%d" % t])
                                    else:
                                        trk.op("dve", lambda v: v.tensor_tensor(out=acc[:, t, hsl], in0=psY[ys][:, half, :], in1=acc[:, t, hsl],
                                                                                op=ALU.add), ["psY%d" % ys, "acc%d" % t], ["acc%d" % t])
                    done()
                with ExitStack() as ph:
                    wpg = sb(ph, "wpg", [128, 8, D], BF16)
                    wpp = sb(ph, "wpp", [128, 2, D], BF16)
                    load_w("pool", wpg, w_ple_gate[layer], "wpg", nsplit=2)
                    load_w("pool", wpp, w_ple_proj[layer], "wpp")
                    lng = sb(ph, "lng", [128, D], F32)
                    lnb = sb(ph, "lnb", [128, D], F32)
                    trk.dma("sp", lng[:, :], ln_ffn_g[layer:layer + 1, :].partition_broadcast(128), [], ["lng"])
                    trk.dma("sp", lnb[:, :], ln_ffn_b[layer:layer + 1, :].partition_broadcast(128), [], ["lnb"])
                    x2b = [sb(ph, "x2b%d" % i, [128, D], BF16) for i in range(2)]
                    x2T = [sb(ph, "x2T%d" % i, [128, 8, 128], BF16) for i in range(2)]
                    pb = [sb(ph, "pb%d" % i, [128, 256], BF16) for i in range(2)]
                    pT = [sb(ph, "pT%d" % i, [128, 2, 128], BF16) for i in range(2)]
                    sig = [sb(ph, "sig%d" % i, [128, 2, 512], F32) for i in range(2)]
                    ob = [sb(ph, "ob%d" % i, [128, D], BF16) for i in range(2)]
                    oT = [sb(ph, "oT%d" % i, [128, 8, 128], BF16) for i in range(2)]
                    LNP = {"st6": [sb(ph, "st6_%d" % i, [128, 12], F32) for i in range(2)],
                           "mv": [sb(ph, "mv_%d" % i, [128, 4], F32) for i in range(2)]}
                    psT = ps(ph, "psT", [128, 1024], BF16)
                    psA = ps(ph, "psA", [128, 2, 512])
                    psB = ps(ph, "psB", [128, 2, 512])
                    psTp = ps(ph, "psTp", [128, 1024], BF16)
                    psT3 = ps(ph, "psT3", [128, 1024], BF16)
                    for t in range(nt):
                        sl = t % 2
                        r0 = (t0 + t) * 128
                        ak = "acc%d" % t
                        x2 = acc[:, t, :]
                        trk.dma("pool", pb[sl][:, :], pd[r0:r0 + 128, :], [], ["pb%d" % sl])
                        layer_norm(trk, LNP, x2, ak, lng, lnb, x2, ak, sl)
                        trk.op("act", lambda a: a.activation(out=x2b[sl][:, :], in_=x2, func=AF.Copy), [ak], ["x2b%d" % sl])
                        transposes(trk, [(psT[:, kc * 128:(kc + 1) * 128], x2b[sl][:, kc * 128:(kc + 1) * 128]) for kc in range(8)],
                                   identb[:, :], ["x2b%d" % sl, "identb"], ["psT"])
                        trk.op("act", lambda a: a.activation(out=x2T[sl][:, :, :], in_=psT[:, :].rearrange("p (k c) -> p k c", k=8), func=AF.Copy),
                               ["psT"], ["x2T%d" % sl])
                        for half in range(2):
                            mm(trk, psA[:, half, :], [(x2T[sl][:, kc, :], wpg[:, kc, half * 512:(half + 1) * 512]) for kc in range(8)],
                               ["x2T%d" % sl, "wpg"], ["psA"])
                        trk.op("act", lambda a: a.activation(out=sig[sl][:, :, :], in_=psA[:, :, :], func=AF.Sigmoid), ["psA"], ["sig%d" % sl])
                        transposes(trk, [(psTp[:, c * 128:(c + 1) * 128], pb[sl][:, c * 128:(c + 1) * 128]) for c in range(2)],
                                   identb[:, :], ["pb%d" % sl, "identb"], ["psTp"])
                        trk.op("act", lambda a: a.activation(out=pT[sl][:, :, :], in_=psTp[:, 0:256].rearrange("p (k c) -> p k c", k=2), func=AF.Copy),
                               ["psTp"], ["pT%d" % sl])
                        for half in range(2):
                            mm(trk, psB[:, half, :], [(pT[sl][:, c, :], wpp[:, c, half * 512:(half + 1) * 512]) for c in range(2)],
                               ["pT%d" % sl, "wpp"], ["psB"])
                        trk.op("dve", lambda v: v.tensor_tensor(out=sig[sl][:, :, :], in0=psB[:, :, :], in1=sig[sl][:, :, :], op=ALU.mult),
                               ["psB", "sig%d" % sl], ["sig%d" % sl])
                        trk.op("dve", lambda v: v.tensor_tensor(out=x2, in0=x2, in1=sig[sl][:, :, :].rearrange("p a b -> p (a b)"), op=ALU.add),
                               [ak, "sig%d" % sl], [ak])
                        okey = "out" if Yd is out_d else "Yd%d" % layer
                        trk.dma("sp", Yd[r0:r0 + 128, :], x2, [ak], [okey])
                        if YTd is not None:
                            trk.op("act", lambda a: a.activation(out=ob[sl][:, :], in_=x2, func=AF.Copy), [ak], ["ob%d" % sl])
                            transposes(trk, [(psT3[:, kc * 128:(kc + 1) * 128], ob[sl][:, kc * 128:(kc + 1) * 128]) for kc in range(8)],
                                       identb[:, :], ["ob%d" % sl, "identb"], ["psT3"])
                            trk.op("act", lambda a: a.activation(out=oT[sl][:, :, :], in_=psT3[:, :].rearrange("p (k c) -> p k c", k=8), func=AF.Copy),
                                   ["psT3"], ["oT%d" % sl])
                            trk.dma("sp", YTd[:, :, r0:r0 + 128], oT[sl][:, :, :], ["oT%d" % sl], ["YTd%d" % layer])
                    done()

    ffn_layer(0, X1T, X1, p0, [(0, 17), (17, 16)], [(w_ffn_gate[0], w_ffn_up[0], w_ffn_down[0])], False, X3, X3T)
    if stop_after == 3:
        return nc, es, trk

    with ExitStack() as kv1:
        k1 = sb(kv1, "k1", [64, 4, NTOK], BF16)
        v1 = sb(kv1, "v1", [128, NT, 4, 128], BF16)
        with ExitStack() as ph:
            wqkv = sb(ph, "wqkv1", [128, 8, 1536], BF16)
            load_w("pool", wqkv, w_qkv[1], "wqkv", nsplit=4)
            trk.op("pool", lambda g: g.memset(v1[:, :, :, :], 1.0), [], ["v1_init"])
            XT = [sb(ph, "XTd%d" % i, [128, 8, 512], BF16) for i in range(2)]
            ctt = [sb(ph, "ctd%d" % i, [64, 512], F32) for i in range(2)]
            stt = [sb(ph, "std%d" % i, [64, 512], F32) for i in range(2)]
            t1s = [sb(ph, "t1d%d" % i, [64, 512], F32) for i in range(2)]
            t2s = [sb(ph, "t2d%d" % i, [64, 512], F32) for i in range(2)]
            krbs = [sb(ph, "krbd%d" % i, [64, 512], BF16) for i in range(2)]
            qg = [sb(ph, "qg%d" % i, [64, 4, 16, 128], BF16) for i in range(2)]
            psK = [ps(ph, "psK%d" % i, [64, 512]) for i in range(2)]
            psPp = [ps(ph, "psP%d" % i, [64, 512]) for i in range(2)]
            psV = [ps(ph, "psV%d" % i, [128, 512]) for i in range(2)]
            ki = 0
            vi = 0
            groups = [(0, 1)] + [(1 + 4 * i, 4) for i in range(8)]
            for gi, (t0, gn) in enumerate(groups):
                gs = gi % 2
                ncl = gn * 128
                cols = slice(t0 * 128, t0 * 128 + ncl)
                ecols = slice(3968 + t0 * 128, 3968 + t0 * 128 + ncl)
                for kc in range(8):
                    trk.dma("sp", XT[gs][:, kc, 0:ncl], X3T[:, kc, cols], [], ["XT%d" % gs], sk=("d", "XTd%d" % gs))
                trk.dma("sp", ctt[gs][:, 0:ncl], CT[:, ecols], [], ["ct%d" % gs])
                trk.dma("sp", stt[gs][:, 0:ncl], ST[:, ecols], [], ["st%d" % gs])

                def proj_rope(c0, scale, out_ap, outk):
                    nonlocal ki
                    s2 = ki % 2
                    ki += 1
                    t1 = t1s[s2]; t2 = t2s[s2]; krb = krbs[s2]; psP = psPp[s2]
                    mm(trk, psK[s2][:, 0:ncl], [(wqkv[:, kc, c0:c0 + 64], XT[gs][:, kc, 0:ncl]) for kc in range(8)],
                       ["XT%d" % gs, "wqkv"], ["psK%d" % s2])
                    trk.op("dve", lambda v: v.scalar_tensor_tensor(out=t1[:, 0:ncl], in0=psK[s2][:, 0:ncl], scalar=scale,
                                                                   in1=ctt[gs][:, 0:ncl], op0=ALU.mult, op1=ALU.mult),
                           ["psK%d" % s2, "ct%d" % gs], ["t1_%d" % s2])
                    trk.op("act", lambda a: a.activation(out=krb[:, 0:ncl], in_=psK[s2][:, 0:ncl], func=AF.Copy, scale=scale),
                           ["psK%d" % s2], ["krb_%d" % s2])
                    mm(trk, psP[:, 0:ncl], [(pm[:, :], krb[:, 0:ncl])], ["krb_%d" % s2, "pm"], ["psP%d" % s2])
                    trk.op("dve", lambda v: v.tensor_tensor(out=t2[:, 0:ncl], in0=psP[:, 0:ncl], in1=stt[gs][:, 0:ncl], op=ALU.mult),
                           ["psP%d" % s2, "st%d" % gs], ["t2_%d" % s2])
                    if len(out_ap.shape) == 2:
                        trk.op("dve", lambda v: v.tensor_tensor(out=out_ap, in0=t1[:, 0:ncl], in1=t2[:, 0:ncl], op=ALU.add),
                               ["t1_%d" % s2, "t2_%d" % s2], [outk])
                    else:
                        trk.op("dve", lambda v: v.tensor_tensor(out=out_ap, in0=t1[:, 0:ncl].rearrange("p (s c) -> p s c", c=128),
                                                                in1=t2[:, 0:ncl].rearrange("p (s c) -> p s c", c=128), op=ALU.add),
                               ["t1_%d" % s2, "t2_%d" % s2], [outk])

                for kvh in range(4):
                    proj_rope(1024 + kvh * 64, 1.0, k1[0:64, kvh, cols], "k1_%d_%d" % (gi, kvh))
                for tl in range(gn):
                    vs = vi % 2
                    vi += 1
                    mm(trk, psV[vs][:, 0:256], [(XT[gs][:, kc, tl * 128:(tl + 1) * 128], wqkv[:, kc, 1280:1536]) for kc in range(8)],
                       ["XT%d" % gs, "wqkv"], ["psV%d" % vs])
                    trk.op("act", lambda a: a.activation(out=v1[:, t0 + tl, :, 0:64], in_=psV[vs][:, 0:256].rearrange("p (h d) -> p h d", h=4),
                                                         func=AF.Copy), ["psV%d" % vs, "v1_init"], ["v1_%d" % (t0 + tl)])
                if t0 == 0:
                    continue
                qs = gi % 2
                for h in range(16):
                    proj_rope(h * 64, 0.125, qg[qs][0:64, :, h, :], "qg%d" % qs)
                trk.dma("sp", Q1[:, t0 - 1:t0 + 3, :], qg[qs][:, :, :, :].rearrange("p s h c -> p s (h c)"), ["qg%d" % qs], ["Q1"])
            done()
        if stop_after == 4:
            return nc, es, trk
        with ExitStack() as ph:
            wo = sb(ph, "wo1", [128, 8, D], BF16)
            load_w("pool", wo, w_o[1], "wo", nsplit=2)
            lng = sb(ph, "lng1", [128, D], F32)
            lnb = sb(ph, "lnb1", [128, D], F32)
            trk.dma("sp", lng[:, :], ln_mix_g[1:2, :].partition_broadcast(128), [], ["lng"])
            trk.dma("sp", lnb[:, :], ln_mix_b[1:2, :].partition_broadcast(128), [], ["lnb"])
            wr = sb(ph, "wr", [128, 8, 8], F32)
            trk.dma("sp", wr[:, :, :], w_router[0].rearrange("(k p) n -> p k n", p=128), [], ["wr"])
            esr = sb(ph, "esr", [128, 16], F32)
            esink = sb(ph, "esink", [128, 16], F32)
            trk.dma("sp", esr[:, :], sinks[0:1, :].partition_broadcast(128), [], ["esr"])
            trk.op("act", lambda a: a.activation(out=esink[:, :], in_=esr[:, :], func=AF.Exp), ["esr"], ["esink"])
            qt = [sb(ph, "qt1_%d" % i, [64, 2048], BF16) for i in range(2)]
            PT = [sb(ph, "PT1_%d" % i, [128, 512], BF16) for i in range(4)]
            den = [sb(ph, "den%d" % i, [128, 512], F32) for i in range(2)]
            rden = [sb(ph, "rden1_%d" % i, [128, 512], F32) for i in range(2)]
            aT = [sb(ph, "aT1_%d" % i, [128, 8, 128], BF16) for i in range(2)]
            xf = [sb(ph, "xf1_%d" % i, [128, D], F32) for i in range(2)]
            yb = [sb(ph, "y1_%d" % i, [128, D], F32) for i in range(2)]
            x4b = [sb(ph, "x4b%d" % i, [128, D], BF16) for i in range(2)]
            x4T = [sb(ph, "x4T%d" % i, [128, 8, 128], BF16) for i in range(2)]
            x4T32 = sb(ph, "x4T32", [128, 8, 128], F32)
            lg = sb(ph, "lg", [128, 8], F32)
            m8 = sb(ph, "m8r", [128, 8], F32)
            d8 = sb(ph, "d8", [128, 8], F32)
            e8 = sb(ph, "e8", [128, 8], F32)
            msk = sb(ph, "msk", [128, 8], F32)
            ss = sb(ph, "ss", [128, 2], F32)
            gtl = [sb(ph, "gtl%d" % i, [128, 8], F32) for i in range(2)]
            LNP = {"st6": [sb(ph, "st6d_%d" % i, [128, 12], F32) for i in range(2)],
                   "mv": [sb(ph, "mvd_%d" % i, [128, 4], F32) for i in range(2)]}
            psS = [ps(ph, "psS%d" % i, [128, 512]) for i in range(2)]
            psO = [ps(ph, "psO%d" % i, [128, 512]) for i in range(2)]
            psW = ps(ph, "psW", [128, 2, 512])
            psT2 = ps(ph, "psT2", [128, 1024], BF16)
            psL = ps(ph, "psL", [128, 512])
            cnt = {"s": 0, "p": 0, "o": 0}

            def attention1(ti):
                sl = ti % 2
                o_ = ti - 1
                trk.dma("sp", qt[sl][:, :], Q1[:, o_, :], [], ["qt%d" % sl])
                trk.dma("sp", xf[sl][:, :], X3[ti * 128:(ti + 1) * 128, :], [], ["xf%d" % sl])
                steps = []
                for kvh in range(4):
                    so = cnt["o"] % 2
                    cnt["o"] += 1
                    parts = [(ti - 1, tricfv if ti == 1 else tricb, "tricfv" if ti == 1 else "tricb"), (ti, trib, "trib")]
                    for idx, (kt, mask, mkey) in enumerate(parts):
                        ss_ = cnt["s"] % 2
                        cnt["s"] += 1
                        pp = cnt["p"] % 4
                        cnt["p"] += 1
                        steps.append((kvh, so, idx, kt, mask, mkey, ss_, pp))

                def S(j):
                    kvh, so, idx, kt, mask, mkey, ss_, pp = steps[j]
                    mm(trk, psS[ss_][:, :], [(k1[0:64, kvh, kt * 128:(kt + 1) * 128], qt[sl][0:64, kvh * 512:(kvh + 1) * 512])],
                       ["qt%d" % sl], ["psS%d" % ss_])

                def E(j):
                    kvh, so, idx, kt, mask, mkey, ss_, pp = steps[j]
                    trk.op("act", lambda a: a.activation(out=PT[pp][:, :], in_=psS[ss_][:, :], func=AF.Exp), ["psS%d" % ss_], ["PT%d" % pp])
                    trk.op("dve", lambda v: v.tensor_tensor(out=PT[pp][:, :].rearrange("p (h q) -> p h q", h=4),
                                                            in0=PT[pp][:, :].rearrange("p (h q) -> p h q", h=4),
                                                            in1=mask[:, :].unsqueeze(1).broadcast_to([128, 4, 128]), op=ALU.mult),
                           ["PT%d" % pp, mkey], ["PT%d" % pp])

                def PV(j):
                    kvh, so, idx, kt, mask, mkey, ss_, pp = steps[j]
                    def pv(pe):
                        return pe.matmul(psO[so][:, :], v1[:, kt, kvh, :], PT[pp][:, :], start=(idx == 0), stop=(idx == 1))
                    trk.op("pe", pv, ["PT%d" % pp], ["psO%d" % so])
                    if idx != 1:
                        return
                    rs = so
                    for i in range(4):
                        h = kvh * 4 + i
                        trk.op("dve", lambda v: v.tensor_scalar(out=den[rs][64:128, i * 128:(i + 1) * 128], in0=psO[so][64:128, i * 128:(i + 1) * 128],
                                                                scalar1=esink[64:128, h:h + 1], scalar2=None, op0=ALU.add),
                               ["psO%d" % so, "esink"], ["den%d_%d" % (rs, i)])
                    trk.op("dve", lambda v: v.reciprocal(out=rden[rs][64:128, :], in_=den[rs][64:128, :]),
                           ["den%d_%d" % (rs, i) for i in range(4)], ["rden%d" % rs])
                    for i in range(4):
                        pb_ = (i % 2) * 64
                        trk.op("dve", lambda v: v.tensor_tensor(out=aT[sl][pb_:pb_ + 64, kvh * 2 + i // 2, :],
                                                                in0=psO[so][0:64, i * 128:(i + 1) * 128],
                                                                in1=rden[rs][64:128, i * 128:(i + 1) * 128], op=ALU.mult),
                               ["psO%d" % so, "rden%d" % rs], ["aT%d_%d_%d" % (sl, kvh, i)])

                N = len(steps)
                S(0)
                S(1)
                for j in range(N):
                    E(j)
                    if j + 2 < N:
                        S(j + 2)
                    PV(j)

            def tail1(ti):
                sl = ti % 2
                o_ = ti - 1
                akeys = ["aT%d_%d_%d" % (sl, k_, i_) for k_ in range(4) for i_ in range(4)]
                for half in range(2):
                    mm(trk, psW[:, half, :], [(aT[sl][:, c, :], wo[:, c, half * 512:(half + 1) * 512]) for c in range(8)],
                       akeys + ["wo"], ["psW"])
                for half in range(2):
                    trk.op("dve", lambda v: v.scalar_tensor_tensor(out=yb[sl][:, half * 512:(half + 1) * 512],
                                                                   in0=xf[sl][:, half * 512:(half + 1) * 512], scalar=ALPHA,
                                                                   in1=psW[:, half, :], op0=ALU.mult, op1=ALU.add),
                           ["xf%d" % sl, "psW"], ["y%d" % sl])
                layer_norm(trk, LNP, yb[sl], "y%d" % sl, lng, lnb, yb[sl], "y%d" % sl, sl)
                trk.dma("sp", X4[o_ * 128:(o_ + 1) * 128, :], yb[sl][:, :], ["y%d" % sl], ["X4"])
                trk.op("act", lambda a: a.activation(out=x4b[sl][:, :], in_=yb[sl][:, :], func=AF.Copy), ["y%d" % sl], ["x4b%d" % sl])
                transposes(trk, [(psT2[:, kc * 128:(kc + 1) * 128], x4b[sl][:, kc * 128:(kc + 1) * 128]) for kc in range(8)],
                           identb[:, :], ["x4b%d" % sl, "identb"], ["psT2"])
                trk.op("act", lambda a: a.activation(out=x4T[sl][:, :, :], in_=psT2[:, :].rearrange("p (k c) -> p k c", k=8), func=AF.Copy),
                       ["psT2"], ["x4T%d" % sl])
                trk.dma("sp", X4T[:, :, o_ * 128:(o_ + 1) * 128], x4T[sl][:, :, :], ["x4T%d" % sl], ["X4T"])
                transposes(trk, [(psW[:, kc // 4, (kc % 4) * 128:(kc % 4 + 1) * 128], yb[sl][:, kc * 128:(kc + 1) * 128]) for kc in range(8)],
                           identf[:, :], ["y%d" % sl, "identf"], ["psW"])
                trk.op("act", lambda a: a.activation(out=x4T32[:, :, :].rearrange("p (a b) c -> p a b c", a=2),
                                                     in_=psW[:, :, :].rearrange("p a (b c) -> p a b c", b=4), func=AF.Copy),
                       ["psW"], ["x4T32"])
                mm(trk, psL[:, 0:8], [(x4T32[:, kc, :], wr[:, kc, :]) for kc in range(8)], ["x4T32", "wr"], ["psL"])
                trk.op("dve", lambda v: v.tensor_copy(out=lg[:, :], in_=psL[:, 0:8]), ["psL"], ["lg"])
                trk.op("dve", lambda v: v.max(out=m8[:, :], in_=lg[:, :]), ["lg"], ["m8r"])
                trk.op("dve", lambda v: v.tensor_scalar(out=d8[:, :], in0=lg[:, :], scalar1=m8[:, 0:1], scalar2=None, op0=ALU.subtract),
                       ["lg", "m8r"], ["d8"])
                trk.op("act", lambda a: a.activation(out=e8[:, :], in_=d8[:, :], func=AF.Exp), ["d8"], ["e8"])
                trk.op("dve", lambda v: v.tensor_scalar(out=msk[:, :], in0=lg[:, :], scalar1=m8[:, 1:2], scalar2=None, op0=ALU.is_ge),
                       ["lg", "m8r"], ["msk"])
                trk.op("dve", lambda v: v.tensor_tensor(out=e8[:, :], in0=e8[:, :], in1=msk[:, :], op=ALU.mult), ["e8", "msk"], ["e8"])
                trk.op("dve", lambda v: v.tensor_reduce(out=ss[:, 0:1], in_=e8[:, :], axis=AX.X, op=ALU.add), ["e8"], ["ss0"])
                trk.op("dve", lambda v: v.reciprocal(out=ss[:, 1:2], in_=ss[:, 0:1]), ["ss0"], ["ss1"])
                trk.op("dve", lambda v: v.tensor_scalar(out=gtl[sl][:, :], in0=e8[:, :], scalar1=ss[:, 1:2], scalar2=None, op0=ALU.mult),
                       ["e8", "ss1"], ["gtl%d" % sl])
                trk.dma("sp", GATES[:, o_, :], gtl[sl][:, :], ["gtl%d" % sl], ["GATES"])

            for ti in range(1, NT):
                attention1(ti)
                if ti >= 2:
                    tail1(ti - 1)
            tail1(NT - 1)
            done()
    if stop_after == 5:
        return nc, es, trk

    experts = [(w_exp_gate[0, e], w_exp_up[0, e], w_exp_down[0, e]) for e in range(8)]
    ffn_layer(1, X4T, X4, p1, [(0, 16), (16, 16)], experts, True, out_d, None)
    return nc, es, trk


def _consts():
    d = np.arange(64)
    inv = np.zeros(64, np.float64)
    i = d % 8
    invf = 1.0 / (500000.0 ** (np.arange(0, 16, 2, dtype=np.float32) / np.float32(16)))
    inv[:16] = invf.astype(np.float64)[i[:16]]
    sgn = np.zeros(64); sgn[:8] = -1.0; sgn[8:16] = 1.0
    c64 = np.zeros((64, 4), np.float32)
    c64[:, 0] = (inv / (2 * np.pi)).astype(np.float32)
    c64[:, 1] = (sgn * 2 * np.pi).astype(np.float32)
    c64[:, 2] = sgn.astype(np.float32)
    pmat = np.zeros((64, 64), np.float32)
    for a in range(8):
        pmat[a + 8, a] = 1.0
        pmat[a, a + 8] = 1.0
    ident = np.eye(128, dtype=np.float32)
    blk = np.zeros((32, SEQ), np.float32)
    for n in range(32):
        blk[n, n * 256:(n + 1) * 256] = 1.0
    k = np.arange(128)[:, None]; q = np.arange(128)[None, :]
    tri = (q >= k).astype(np.float32)
    return c64, pmat, ident, blk, tri, (1.0 - tri).astype(np.float32)


def _core_inputs(inputs, c, consts, stop_after=None):
    b, h = c // 2, c % 2
    c64, pmat, ident, blk, tri, tric = consts
    x = inputs["x"][b]
    posb = inputs["positions"][b]
    lo = slice(0, HALF)
    own = slice(h * HALF, (h + 1) * HALF)
    xin = np.concatenate([x[lo], x[own]], axis=0)
    pos = np.concatenate([posb[lo], posb[own]])[None, :].astype(np.int32)
    p0e = np.concatenate([inputs["p"][0, b][lo], inputs["p"][0, b][own]], axis=0)[SEQ - NTOK:]
    p1 = inputs["p"][1, b][own]
    gbias = np.zeros((17, 32), np.float32)
    cb = np.zeros((17, 32), np.float32)
    for jj in range(17):
        J = 15 + jj
        for n in range(32):
            valid = (n < J) and (n >= 16 or h == 1)
            gbias[jj, n] = 0.0 if valid else -1e9
            cb[jj, n] = NEG if valid else 3 * NEG
        gbias[jj, J] = -2e9
        cb[jj, J] = 0.0
    m = {
        "xin": np.ascontiguousarray(xin), "pos": pos, "p0": np.ascontiguousarray(p0e), "p1": np.ascontiguousarray(p1),
        "cst64": c64, "pmat": pmat, "ident": ident, "blkind": blk, "tri": tri, "tric": tric,
        "gbias": gbias.reshape(1, -1), "cb": cb.reshape(1, -1), "fv": np.full((128, 1), float(h), np.float32),
    }
    for k in ("w_qkv", "w_o", "ln_mix_g", "ln_mix_b", "ln_ffn_g", "ln_ffn_b", "sinks", "w_ffn_gate", "w_ffn_up",
              "w_ffn_down", "w_router", "w_exp_gate", "w_exp_up", "w_exp_down", "w_ple_proj", "w_ple_gate"):
        if k.startswith("w_exp") and not (stop_after is None or stop_after >= 5):
            continue
        m[k] = np.ascontiguousarray(inputs[k], dtype=np.float32)
    return m


def run(inputs, stop_after=None, debug=False, cores=8, trace=False):
    inputs = {k: np.asarray(v) for k, v in inputs.items()}
    consts = _consts()
    nc, es, trk = build_program(stop_after=stop_after, debug=debug)
    in_maps = [_core_inputs(inputs, c, consts, stop_after) for c in range(cores)]
    res = run_bass_kernel_spmd(nc, in_maps, core_ids=list(range(cores)), trace=trace)
    es.close()
    return res


def kernel(**inputs):
    res = run(inputs)
    out = np.zeros((4, SEQ, D), np.float32)
    for c in range(8):
        b, h = c // 2, c % 2
        out[b, h * HALF:(h + 1) * HALF] = res.results[c]["out"]
    return out
```

```python
from contextlib import ExitStack
import os
import numpy as np
import concourse.bass as bass
import concourse.mybir as mybir
from concourse.bass_utils import run_bass_kernel_spmd

F32 = mybir.dt.float32
BF16 = mybir.dt.bfloat16
I32 = mybir.dt.int32
AF = mybir.ActivationFunctionType
ALU = mybir.AluOpType
AX = mybir.AxisListType

D = 1024
SEQ = 8192
HALF = 4096
NT = 33
NTOK = NT * 128
DFF = 3584
NFB = 7
ALPHA = 4.0 ** 0.25
EPS = 1e-5
TWO_PI = 6.283185307179586
NEG = -30000.0


class Trk:
    def __init__(self, nc, es):
        self.nc = nc
        self.es = es
        self.eng = {"pe": nc.tensor, "act": nc.scalar, "dve": nc.vector, "pool": nc.gpsimd, "sp": nc.sync}
        self.sem = {}
        self.cnt = {}
        self.seen = {e: {} for e in self.eng}
        self.lw = {}
        self.rd = {}

    def _sem(self, k):
        if k not in self.sem:
            self.sem[k] = self.es.enter_context(self.nc.semaphore("s%d" % len(self.sem)))
            self.cnt[k] = 0
        return self.sem[k]

    def _waits(self, e, reads, writes, own_sk=None):
        need = {}

        def add(sk, v, raw):
            if sk == e and not raw:
                return
            if sk == own_sk and not raw:
                return
            if v > need.get(sk, 0):
                need[sk] = v

        for k in reads:
            if k in self.lw:
                add(self.lw[k][0], self.lw[k][1], True)
        for k in writes:
            if k in self.lw:
                add(self.lw[k][0], self.lw[k][1], False)
            for sk, v in self.rd.get(k, {}).items():
                add(sk, v, False)
        for sk, v in need.items():
            if self.seen[e].get(sk, 0) < v:
                self.eng[e].wait_ge(self.sem[sk], v)
                self.seen[e][sk] = v

    def _record(self, sk, v, reads, writes):
        for k in reads:
            d = self.rd.setdefault(k, {})
            if d.get(sk, 0) < v:
                d[sk] = v
        for k in writes:
            self.lw[k] = (sk, v)
            self.rd[k] = {}

    def op(self, e, fn, reads=(), writes=()):
        self._sem(e)
        extra = [k for k in reads if isinstance(k, str) and k.startswith("ps") and k not in writes]
        if extra:
            writes = list(writes) + extra
        self._waits(e, reads, writes)
        ins = fn(self.eng[e])
        ins.then_inc(self.sem[e], 1)
        self.cnt[e] += 1
        self._record(e, self.cnt[e], reads, writes)

    def dma(self, e, out, in_, reads=(), writes=(), sk=None):
        if sk is None:
            w0 = writes[0]
            if isinstance(w0, str) and (w0[0].isupper() or w0.startswith("dbg") or w0 == "out"):
                sk = ("st", reads[0])
            else:
                sk = ("d", w0)
        self._sem(sk)
        self._waits(e, reads, writes, own_sk=sk)
        ins = self.eng[e].dma_start(out=out, in_=in_)
        ins.then_inc(self.sem[sk], 16)
        self.cnt[sk] += 16
        self._record(sk, self.cnt[sk], reads, writes)

    def barrier(self):
        for e in self.eng:
            for sk, v in self.cnt.items():
                if v > 0 and self.seen[e].get(sk, 0) < v:
                    self.eng[e].wait_ge(self.sem[sk], v)
                    self.seen[e][sk] = v
        self.lw = {}
        self.rd = {}

    def final_wait(self, e, key):
        sk = ("d", key)
        self.eng[e].wait_ge(self.sem[sk], self.cnt[sk])


def mm(trk, out, pairs, reads, writes):
    def fn(pe):
        n = len(pairs)
        ins = None
        for i, (l, r) in enumerate(pairs):
            ins = pe.matmul(out, l, r, start=(i == 0), stop=(i == n - 1))
        return ins
    trk.op("pe", fn, reads, writes)


def transposes(trk, outs_ins, ident, reads, writes):
    def fn(pe):
        ins = None
        for o, i in outs_ins:
            ins = pe.transpose(o, i, ident)
        return ins
    trk.op("pe", fn, reads, writes)


def layer_norm(trk, P, y, yk, g_sb, b_sb, out, outk, slot):
    st6 = P["st6"][slot]
    mv = P["mv"][slot]
    k6 = "st6_%d" % slot
    kmv = "mv_%d" % slot
    krs = "rs_%d" % slot
    trk.op("dve", lambda v: v.bn_stats(out=st6[:, 0:6], in_=y[:, 0:512]), [yk], [k6 + "a"])
    trk.op("dve", lambda v: v.bn_stats(out=st6[:, 6:12], in_=y[:, 512:1024]), [yk], [k6 + "b"])
    trk.op("dve", lambda v: v.bn_aggr(out=mv[:, 0:2], in_=st6[:, 0:12]), [k6 + "a", k6 + "b"], [kmv])
    trk.op("act", lambda a: a.activation(out=mv[:, 2:3], in_=mv[:, 1:2], func=AF.Sqrt, bias=EPS), [kmv], [krs + "s"])
    trk.op("dve", lambda v: v.reciprocal(out=mv[:, 3:4], in_=mv[:, 2:3]), [krs + "s"], [krs])
    trk.op("dve", lambda v: v.tensor_scalar(out=out[:, :], in0=y[:, :], scalar1=mv[:, 0:1], scalar2=mv[:, 3:4],
                                            op0=ALU.subtract, op1=ALU.mult), [yk, kmv, krs], [outk])
    trk.op("dve", lambda v: v.tensor_tensor(out=out[:, :], in0=out[:, :], in1=g_sb[:, :], op=ALU.mult), [outk, "lng"], [outk])
    trk.op("dve", lambda v: v.tensor_tensor(out=out[:, :], in0=out[:, :], in1=b_sb[:, :], op=ALU.add), [outk, "lnb"], [outk])


def build_program(stop_after=None, debug=False):
    nc = bass.Bass("TRN2", target_bir_lowering=False)
    es = ExitStack()
    trk = Trk(nc, es)

    def din(name, shape, dt=F32):
        return nc.dram_tensor(name, list(shape), dt, kind="ExternalInput").ap()

    def dscr(name, shape, dt):
        kind = "ExternalOutput" if debug else "Internal"
        return nc.dram_tensor(name, list(shape), dt, kind=kind).ap()

    xin = din("xin", [SEQ, D])
    pos = din("pos", [1, SEQ], I32)
    p0 = din("p0", [NTOK, 256])
    p1 = din("p1", [HALF, 256])
    cst64 = din("cst64", [64, 4])
    pmat_d = din("pmat", [64, 64])
    ident_d = din("ident", [128, 128])
    blkind = din("blkind", [32, SEQ])
    tri_d = din("tri", [128, 128])
    tric_d = din("tric", [128, 128])
    gbias_d = din("gbias", [1, 17 * 32])
    cb_d = din("cb", [1, 17 * 32])
    fv_d = din("fv", [128, 1])
    w_qkv = din("w_qkv", [2, D, 1536])
    w_o = din("w_o", [2, D, D])
    ln_mix_g = din("ln_mix_g", [2, D])
    ln_mix_b = din("ln_mix_b", [2, D])
    ln_ffn_g = din("ln_ffn_g", [2, D])
    ln_ffn_b = din("ln_ffn_b", [2, D])
    sinks = din("sinks", [1, 16])
    w_ffn_gate = din("w_ffn_gate", [1, D, DFF])
    w_ffn_up = din("w_ffn_up", [1, D, DFF])
    w_ffn_down = din("w_ffn_down", [1, DFF, D])
    w_router = din("w_router", [1, D, 8])
    if stop_after is None or stop_after >= 5:
        w_exp_gate = din("w_exp_gate", [1, 8, D, DFF])
        w_exp_up = din("w_exp_up", [1, 8, D, DFF])
        w_exp_down = din("w_exp_down", [1, 8, DFF, D])
    w_ple_proj = din("w_ple_proj", [2, 256, D])
    w_ple_gate = din("w_ple_gate", [2, D, D])
    out_d = nc.dram_tensor("out", [HALF, D], F32, kind="ExternalOutput").ap()

    CT = dscr("CT", [64, SEQ], F32)
    ST = dscr("ST", [64, SEQ], F32)
    QA = dscr("QA", [96, NT, 16 * 128], BF16)
    X1 = dscr("X1", [NTOK, D], F32)
    X1T = dscr("X1T", [128, 8, NTOK], BF16)
    X3 = dscr("X3", [NTOK, D], F32)
    X3T = dscr("X3T", [128, 8, NTOK], BF16)
    Q1 = dscr("Q1", [64, 32, 16 * 128], BF16)
    X4 = dscr("X4", [HALF, D], F32)
    X4T = dscr("X4T", [128, 8, HALF], BF16)
    GATES = dscr("GATES", [128, 32, 8], F32)

    uid = [0]

    def sb(stk, name, shape, dt):
        uid[0] += 1
        return stk.enter_context(nc.sbuf_tensor("s%d_%s" % (uid[0], name), list(shape), dt))

    def ps(stk, name, shape, dt=F32):
        uid[0] += 1
        return stk.enter_context(nc.psum_tensor("p%d_%s" % (uid[0], name), list(shape), dt))

    def done():
        trk.barrier()

    c64 = sb(es, "c64", [64, 4], F32)
    pm = sb(es, "pm", [64, 64], BF16)
    identb = sb(es, "identb", [128, 128], BF16)
    identf = sb(es, "identf", [128, 128], F32)
    trib = sb(es, "trib", [128, 128], BF16)
    tricb = sb(es, "tricb", [128, 128], BF16)
    tricfv = sb(es, "tricfv", [128, 128], BF16)
    fv = sb(es, "fv", [128, 1], F32)
    trk.dma("sp", c64[:, :], cst64[:, :], [], ["c64"])
    trk.dma("pool", pm[:, :], pmat_d[:, :], [], ["pm"])
    trk.dma("pool", identb[:, :], ident_d[:, :], [], ["identb"])
    trk.dma("sp", identf[:, :], ident_d[:, :], [], ["identf"])
    trk.dma("pool", trib[:, :], tri_d[:, :], [], ["trib"])
    trk.dma("pool", tricb[:, :], tric_d[:, :], [], ["tricb"])
    trk.dma("sp", fv[:, :], fv_d[:, :], [], ["fv"])
    trk.op("dve", lambda v: v.tensor_scalar(out=tricfv[:, :], in0=tricb[:, :], scalar1=fv[:, 0:1], scalar2=None,
                                            op0=ALU.mult), ["tricb", "fv"], ["tricfv"])

    if stop_after == -1:
        done()
        return nc, es, trk
    with ExitStack() as ph:
        CW = 2048
        pi = [sb(ph, "r_pi%d" % i, [64, CW], I32) for i in range(2)]
        u = [sb(ph, "r_u%d" % i, [64, CW], F32) for i in range(2)]
        ni = [sb(ph, "r_ni%d" % i, [64, CW], I32) for i in range(2)]
        nf = [sb(ph, "r_nf%d" % i, [64, CW], F32) for i in range(2)]
        to = [sb(ph, "r_to%d" % i, [64, CW], F32) for i in range(2)]
        it = 0
        for c in range(SEQ // CW):
            cols = slice(c * CW, (c + 1) * CW)
            s = c % 2
            trk.dma("sp", pi[s][:, :], pos[0:1, cols].partition_broadcast(64), [], ["r_pi%d" % s])
            trk.op("dve", lambda v: v.tensor_copy(out=u[s][:, :], in_=pi[s][:, :]), ["r_pi%d" % s], ["r_u%d" % s])
            trk.op("dve", lambda v: v.tensor_scalar(out=u[s][:, :], in0=u[s][:, :], scalar1=c64[:, 0:1], scalar2=None,
                                                    op0=ALU.mult), ["r_u%d" % s, "c64"], ["r_u%d" % s])
            for which in range(2):
                k = it % 2
                it += 1
                if which == 1:
                    trk.op("dve", lambda v: v.tensor_scalar(out=u[s][:, :], in0=u[s][:, :], scalar1=0.25, scalar2=None,
                                                            op0=ALU.add), ["r_u%d" % s], ["r_u%d" % s])
                trk.op("dve", lambda v: v.tensor_copy(out=ni[k][:, :], in_=u[s][:, :]), ["r_u%d" % s], ["r_ni%d" % k])
                trk.op("dve", lambda v: v.tensor_copy(out=nf[k][:, :], in_=ni[k][:, :]), ["r_ni%d" % k], ["r_nf%d" % k])
                trk.op("dve", lambda v: v.tensor_tensor(out=nf[k][:, :], in0=u[s][:, :], in1=nf[k][:, :], op=ALU.subtract),
                       ["r_u%d" % s, "r_nf%d" % k], ["r_nf%d" % k])
                PH0 = int(os.environ.get("PH0", "9"))
                if PH0 == 1:
                    trk.dma("sp", (ST if which == 0 else CT)[:, cols], nf[k][:, :], ["r_nf%d" % k], ["ST" if which == 0 else "CT"])
                    continue
                if PH0 == 2:
                    trk.op("act", lambda a: a.activation(out=to[k][:, :], in_=nf[k][:, :], func=AF.Sin, scale=TWO_PI),
                           ["r_nf%d" % k], ["r_to%d" % k])
                    trk.dma("sp", (ST if which == 0 else CT)[:, cols], to[k][:, :], ["r_to%d" % k], ["ST" if which == 0 else "CT"])
                    continue
                if which == 0:
                    trk.op("dve", lambda v: v.tensor_scalar(out=nf[k][:, :], in0=nf[k][:, :], scalar1=c64[:, 2:3], scalar2=None,
                                                            op0=ALU.mult), ["r_nf%d" % k, "c64"], ["r_nf%d" % k])
                    trk.op("act", lambda a: a.activation(out=to[k][:, :], in_=nf[k][:, :], func=AF.Sin, scale=TWO_PI),
                           ["r_nf%d" % k], ["r_to%d" % k])
                    trk.dma("sp", ST[:, cols], to[k][:, :], ["r_to%d" % k], ["ST"])
                else:
                    trk.op("act", lambda a: a.activation(out=to[k][:, :], in_=nf[k][:, :], func=AF.Sin, scale=TWO_PI),
                           ["r_nf%d" % k], ["r_to%d" % k])
                    trk.dma("sp", CT[:, cols], to[k][:, :], ["r_to%d" % k], ["CT"])
        done()
    if stop_after == 0:
        return nc, es, trk

    def rope_head(P, psrc, psk, ncols, scale, ct_ap, st_ap, ctk, out_ap, outk, slot):
        t1 = P["t1"][slot]
        t2 = P["t2"][slot]
        krb = P["krb"][slot]
        psP = P["psP"][slot]
        k1, k2, kk, kp = "t1_%d" % slot, "t2_%d" % slot, "krb_%d" % slot, "psP_%d" % slot
        trk.op("dve", lambda v: v.scalar_tensor_tensor(out=t1[:, 0:ncols], in0=psrc, scalar=scale, in1=ct_ap,
                                                       op0=ALU.mult, op1=ALU.mult), [psk, ctk], [k1])
        trk.op("act", lambda a: a.activation(out=krb[:, 0:ncols], in_=psrc, func=AF.Copy, scale=scale), [psk], [kk])
        mm(trk, psP[:, 0:ncols], [(pm[:, :], krb[:, 0:ncols])], [kk, "pm"], [kp])
        trk.op("dve", lambda v: v.tensor_tensor(out=t2[:, 0:ncols], in0=psP[:, 0:ncols], in1=st_ap, op=ALU.mult),
               [kp, ctk], [k2])
        trk.op("dve", out_ap_writer(out_ap, t1, t2, ncols), [k1, k2], [outk])

    def out_ap_writer(out_ap, t1, t2, ncols):
        nd = len(out_ap.shape)
        if nd == 2:
            return lambda v: v.tensor_tensor(out=out_ap, in0=t1[:, 0:ncols], in1=t2[:, 0:ncols], op=ALU.add)
        nsub = out_ap.shape[1]
        a = t1[:, 0:ncols].rearrange("p (s c) -> p s c", s=nsub)
        b = t2[:, 0:ncols].rearrange("p (s c) -> p s c", s=nsub)
        return lambda v: v.tensor_tensor(out=out_ap, in0=a, in1=b, op=ALU.add)

    def load_w(eng, dst, src, key, nsplit=1):
        v = src.rearrange("(k p) n -> p k n", p=128)
        kc = v.shape[1]
        step = kc // nsplit
        for i in range(nsplit):
            trk.dma(eng, dst[:, i * step:(i + 1) * step, :], v[:, i * step:(i + 1) * step, :], [], [key], sk=("d", key))

    rp_cnt = [0]

    def rope_pipeline(items, wq, rhs_fn, xkeys, ncl, c_ap, s_ap, ckey, skey, psKs, psPs, t1s, t2s, krbs):
        n = len(items)
        slots = []
        for _ in range(n):
            slots.append(rp_cnt[0])
            rp_cnt[0] += 1

        def MM(i):
            c0 = items[i][0]
            pk, pkk = psKs[slots[i] % len(psKs)]
            mm(trk, pk[:, 0:ncl], [(wq[:, kc, c0:c0 + 64], rhs_fn(kc)) for kc in range(8)], xkeys + ["wqkv"], [pkk])

        def R1(i):
            scale = items[i][1]
            pk, pkk = psKs[slots[i] % len(psKs)]
            t1, t1k = t1s[slots[i] % len(t1s)]
            krb, krk = krbs[slots[i] % len(krbs)]
            trk.op("dve", lambda v: v.scalar_tensor_tensor(out=t1[:, 0:ncl], in0=pk[:, 0:ncl], scalar=scale, in1=c_ap,
                                                           op0=ALU.mult, op1=ALU.mult), [pkk, ckey], [t1k])
            trk.op("act", lambda a: a.activation(out=krb[:, 0:ncl], in_=pk[:, 0:ncl], func=AF.Copy, scale=scale), [pkk], [krk])

        def PM(i):
            krb, krk = krbs[slots[i] % len(krbs)]
            pp, ppk = psPs[slots[i] % len(psPs)]
            mm(trk, pp[:, 0:ncl], [(pm[:, :], krb[:, 0:ncl])], [krk, "pm"], [ppk])

        def R2(i):
            out_ap, outk = items[i][2], items[i][3]
            pp, ppk = psPs[slots[i] % len(psPs)]
            t1, t1k = t1s[slots[i] % len(t1s)]
            t2, t2k = t2s[slots[i] % len(t2s)]
            trk.op("dve", lambda v: v.tensor_tensor(out=t2[:, 0:ncl], in0=pp[:, 0:ncl], in1=s_ap, op=ALU.mult), [ppk, skey], [t2k])
            if len(out_ap.shape) == 2:
                trk.op("dve", lambda v: v.tensor_tensor(out=out_ap, in0=t1[:, 0:ncl], in1=t2[:, 0:ncl], op=ALU.add), [t1k, t2k], [outk])
            else:
                trk.op("dve", lambda v: v.tensor_tensor(out=out_ap, in0=t1[:, 0:ncl].rearrange("p (s c) -> p s c", c=128),
                                                        in1=t2[:, 0:ncl].rearrange("p (s c) -> p s c", c=128), op=ALU.add), [t1k, t2k], [outk])

        MM(0)
        for i in range(n):
            R1(i)
            if i + 1 < n:
                MM(i + 1)
            PM(i)
            R2(i)

    with ExitStack() as kv:
        kaug = sb(kv, "kaug", [96, 4, SEQ], BF16)
        vaug = sb(kv, "vaug", [128, 64, 4, 128], BF16)
        with ExitStack() as ph:
            wqkv = sb(ph, "wqkv", [128, 8, 1536], BF16)
            load_w("pool", wqkv, w_qkv[0], "wqkv", nsplit=4)
            for kvh in range(4):
                trk.dma("pool", kaug[64:96, kvh, :], blkind[:, :], [], ["kaug_ind"], sk=("d", "kaug_ind"))
            trk.op("pool", lambda g: g.memset(vaug[:, :, :, :], 1.0), [], ["vaug_init"])
            kms = sb(ph, "kms", [64, 4, 32], F32)
            kmb = sb(ph, "kmb", [64, 4, 32], BF16)
            trk.op("pool", lambda g: g.memset(kms[:, :, :], 0.0), [], ["kms"])
            trk.op("pool", lambda g: g.memset(kmb[:, :, :], 0.0), [], ["kmb"])
            gbias_sb = sb(ph, "gbias_sb", [128, 17, 32], F32)
            cb_sb = sb(ph, "cb_sb", [128, 17, 32], F32)
            trk.dma("sp", gbias_sb[:, :, :].rearrange("p a b -> p (a b)"), gbias_d.partition_broadcast(128), [], ["gbias"])
            trk.dma("sp", cb_sb[:, :, :].rearrange("p a b -> p (a b)"), cb_d.partition_broadcast(128), [], ["cb"])
            xb = [sb(ph, "xb%d" % i, [128, D], BF16) for i in range(2)]
            XT = [sb(ph, "XT%d" % i, [128, 8, 512], BF16) for i in range(1)] * 2
            ctt = [sb(ph, "ct%d" % i, [64, 512], F32) for i in range(1)] * 2
            stt = [sb(ph, "st%d" % i, [64, 512], F32) for i in range(1)] * 2
            P = {
                "t1": [sb(ph, "t1_%d" % i, [64, 512], F32) for i in range(1)] * 2,
                "t2": [sb(ph, "t2_%d" % i, [64, 512], F32) for i in range(1)] * 2,
                "krb": [sb(ph, "krb_%d" % i, [64, 512], BF16) for i in range(2)],
                "psP": [ps(ph, "psP_%d" % i, [64, 512]) for i in range(2)],
            }
            qa = [sb(ph, "qa%d" % i, [96, 4, 16, 128], BF16) for i in range(1)] * 2
            gb = sb(ph, "gb", [128, 16, 32], F32)
            m8 = sb(ph, "m8", [128, 16, 8], F32)
            sel = sb(ph, "sel", [128, 16, 32], F32)
            stage = sb(ph, "stage", [128, 16, 128], BF16)
            trk.op("pool", lambda g: g.memset(stage[:, :, :], 0.0), [], ["stage"])
            psT = [ps(ph, "psT%d" % i, [128, 1024], BF16) for i in range(1)]
            psK = [ps(ph, "psK%d" % i, [64, 512]) for i in range(2)]
            psV = ps(ph, "psV", [128, 512])
            psG = ps(ph, "psG", [128, 512])
            psM = ps(ph, "psM", [128, 8, 128], BF16)
            xi = 0
            ki = 0
            PHA = int(os.environ.get("PHA", "99"))
            for g in range(16 if PHA > 5 else (1 if PHA > 1 else 0)):
                gs = 0
                gcols = slice(g * 512, (g + 1) * 512)
                trk.dma("sp", ctt[gs][:, :], CT[:, gcols], ["CT"], ["ct0"])
                trk.dma("sp", stt[gs][:, :], ST[:, gcols], ["ST"], ["st0"])
                ctk = "ctst%d" % gs
                for sub in range(4):
                    sl = xi % 2
                    tp = 0
                    xi += 1
                    r0 = g * 512 + sub * 128
                    trk.dma("pool", xb[sl][:, :], xin[r0:r0 + 128, :], [], ["xb%d" % sl])
                    transposes(trk, [(psT[tp][:, kc * 128:(kc + 1) * 128], xb[sl][:, kc * 128:(kc + 1) * 128]) for kc in range(8)],
                               identb[:, :], ["xb%d" % sl, "identb"], ["psT%d" % tp])
                    trk.op("act", lambda a: a.activation(out=XT[gs][:, :, sub * 128:(sub + 1) * 128],
                                                         in_=psT[tp][:, :].rearrange("p (k c) -> p k c", k=8), func=AF.Copy),
                           ["psT%d" % tp], ["XT%d_%d" % (gs, sub)])
                xtk = ["XT%d_%d" % (gs, s_) for s_ in range(4)]
                bufsA = dict(psKs=[(psK[0], "psK0"), (psK[1], "psK1")], psPs=[(P["psP"][0], "psP_0"), (P["psP"][1], "psP_1")],
                             t1s=[(P["t1"][0], "t1_0")], t2s=[(P["t2"][0], "t2_0")], krbs=[(P["krb"][0], "krb_0"), (P["krb"][1], "krb_1")])
                if PHA >= 3:
                    rope_pipeline([(1024 + kvh * 64, 1.0, kaug[0:64, kvh, gcols], "kaug_%d_%d" % (g, kvh)) for kvh in range(4)],
                                  wqkv, lambda kc: XT[gs][:, kc, :], xtk, 512, ctt[gs][:, :], stt[gs][:, :], "ct0", "st0", **bufsA)
                for sub in range(4 if PHA >= 4 else 0):
                    mm(trk, psV[:, 0:256], [(XT[gs][:, kc, sub * 128:(sub + 1) * 128], wqkv[:, kc, 1280:1536]) for kc in range(8)],
                       xtk + ["wqkv"], ["psV"])
                    trk.op("act", lambda a: a.activation(out=vaug[:, g * 4 + sub, :, 0:64],
                                                         in_=psV[:, 0:256].rearrange("p (h d) -> p h d", h=4), func=AF.Copy),
                           ["psV", "vaug_init"], ["vaug_%d" % (g * 4 + sub)])
                if PHA < 5:
                    continue
                trk.op("dve", lambda v: v.tensor_reduce(out=kms[:, :, 2 * g:2 * g + 2],
                                                        in_=kaug[0:64, :, gcols].rearrange("p h (b c) -> p h b c", b=2),
                                                        axis=AX.X, op=ALU.add),
                       ["kaug_%d_%d" % (g, k_) for k_ in range(4)] + ["kms"], ["kms"])
                trk.op("dve", lambda v: v.tensor_copy(out=kmb[:, :, :], in_=kms[:, :, :]), ["kms"], ["kmb"])
                if g < 7 or PHA < 7:
                    continue
                subs = [3] if g == 7 else [0, 1, 2, 3]
                c0q = subs[0] * 128
                ncols = len(subs) * 128
                qs = 0
                rope_pipeline([(h * 64, 0.125, qa[qs][0:64, subs[0]:subs[-1] + 1, h, :], "qa%d_q" % qs) for h in range(16)],
                              wqkv, lambda kc: XT[gs][:, kc, c0q:512], xtk, ncols, ctt[gs][:, c0q:512], stt[gs][:, c0q:512],
                              "ct0", "st0", **bufsA)
                for sub in (subs if PHA >= 8 else []):
                    et = g * 4 + sub
                    jj = et // 2 - 15
                    def gate_mm(pe):
                        ins = None
                        for h in range(16):
                            ins = pe.matmul(psG[:, h * 32:(h + 1) * 32], qa[qs][0:64, sub, h, :], kmb[0:64, h // 4, :],
                                            start=True, stop=True)
                        return ins
                    trk.op("pe", gate_mm, ["qa%d_q" % qs, "kmb"], ["psG"])
                    trk.op("dve", lambda v: v.tensor_tensor(out=gb[:, :, :], in0=psG[:, :].rearrange("p (h n) -> p h n", h=16),
                                                            in1=gbias_sb[:, jj, :].unsqueeze(1).broadcast_to([128, 16, 32]), op=ALU.add),
                           ["psG", "gbias"], ["gb"])
                    for h in range(16):
                        trk.op("dve", lambda v: v.max(out=m8[:, h, :], in_=gb[:, h, :]), ["gb"], ["m8_%d" % h])
                    for h in range(16):
                        trk.op("dve", lambda v: v.tensor_scalar(out=sel[:, h, :], in0=gb[:, h, :], scalar1=m8[:, h, 2:3], scalar2=-NEG,
                                                                op0=ALU.is_ge, op1=ALU.mult), ["gb", "m8_%d" % h], ["sel_%d" % h])
                    trk.op("dve", lambda v: v.tensor_tensor(out=stage[:, :, 64:96], in0=sel[:, :, :],
                                                            in1=cb_sb[:, jj, :].unsqueeze(1).broadcast_to([128, 16, 32]), op=ALU.add),
                           ["sel_%d" % h for h in range(16)] + ["cb", "stage"], ["stage"])
                    for rnd in range(2):
                        transposes(trk, [(psM[:, hh, :], stage[:, rnd * 8 + hh, :]) for hh in range(8)], identb[:, :],
                                   ["stage", "identb"], ["psM"])
                        trk.op("act", lambda a: a.activation(out=qa[qs][64:96, sub, rnd * 8:(rnd + 1) * 8, :], in_=psM[64:96, :, :], func=AF.Copy),
                               ["psM"], ["qa%d_m%d_%d" % (qs, sub, rnd)])
                qkeys = ["qa%d_q" % qs] + (["qa%d_m%d_%d" % (qs, s_, r_) for s_ in subs for r_ in range(2)] if PHA >= 8 else [])
                if g == 7:
                    trk.dma("sp", QA[:, 0, :], qa[qs][:, 3, :, :].rearrange("p h c -> p (h c)"), qkeys, ["QA"])
                else:
                    t0 = 1 + 4 * (g - 8)
                    trk.dma("sp", QA[:, t0:t0 + 4, :], qa[qs][:, :, :, :].rearrange("p s h c -> p s (h c)"), qkeys, ["QA"])
            done()
            if debug:
                dk = nc.dram_tensor("dbg_kaug", [96, 4, SEQ], BF16, kind="ExternalOutput").ap()
                dv_ = nc.dram_tensor("dbg_vaug", [128, 64, 4, 128], BF16, kind="ExternalOutput").ap()
                trk.dma("sp", dk[:, :, :], kaug[:, :, :], [], ["dbg_k"])
                trk.dma("sp", dv_[:, :, :, :], vaug[:, :, :, :], [], ["dbg_v"])
                done()
        if stop_after == 1:
            return nc, es, trk
        with ExitStack() as ph:
            wo = sb(ph, "wo", [128, 8, D], BF16)
            load_w("pool", wo, w_o[0], "wo", nsplit=2)
            lng = sb(ph, "lng", [128, D], F32)
            lnb = sb(ph, "lnb", [128, D], F32)
            trk.dma("sp", lng[:, :], ln_mix_g[0:1, :].partition_broadcast(128), [], ["lng"])
            trk.dma("sp", lnb[:, :], ln_mix_b[0:1, :].partition_broadcast(128), [], ["lnb"])
            qt = [sb(ph, "qt%d" % i, [96, 2048], BF16) for i in range(2)]
            PT = [sb(ph, "PT%d" % i, [128, 512], BF16) for i in range(3)]
            rden = [sb(ph, "rden%d" % i, [128, 512], F32) for i in range(2)]
            aT = [sb(ph, "aT%d" % i, [128, 8, 128], BF16) for i in range(2)]
            xf = [sb(ph, "xf%d" % i, [128, D], F32) for i in range(2)]
            yb = [sb(ph, "y%d" % i, [128, D], F32) for i in range(2)]
            x1b = [sb(ph, "x1b%d" % i, [128, D], BF16) for i in range(2)]
            x1T = [sb(ph, "x1T%d" % i, [128, 8, 128], BF16) for i in range(2)]
            LNP = {"st6": [sb(ph, "st6_%d" % i, [128, 12], F32) for i in range(2)],
                   "mv": [sb(ph, "mv_%d" % i, [128, 4], F32) for i in range(2)]}
            psS = [ps(ph, "psS%d" % i, [128, 512]) for i in range(3)]
            psO = [ps(ph, "psO%d" % i, [128, 512]) for i in range(2)]
            psW = ps(ph, "psW", [128, 2, 512])
            psT2 = ps(ph, "psT2", [128, 1024], BF16)
            cnt = {"s": 0, "p": 0, "o": 0}
            if debug:
                dbg_aT = nc.dram_tensor("dbg_aT", [NT, 128, 8, 128], BF16, kind="ExternalOutput").ap()
                dbg_rd = nc.dram_tensor("dbg_rd", [NT, 4, 128, 512], F32, kind="ExternalOutput").ap()

            def attention(ti):
                sl = ti % 2
                et = 31 + ti
                J, s_ = et // 2, et % 2
                trk.dma("sp", qt[sl][:, :], QA[:, ti, :], ["QA"], ["qt%d" % sl])
                r0 = 3968 + 128 * ti
                trk.dma("sp", xf[sl][:, :], xin[r0:r0 + 128, :], [], ["xf%d" % sl])
                ktiles = [(kt, False) for kt in range(2 * J)]
                if s_ == 0:
                    ktiles.append((2 * J, True))
                else:
                    ktiles.append((2 * J, False))
                    ktiles.append((2 * J + 1, True))
                steps = []
                for kvh in range(4):
                    so = cnt["o"] % 2
                    cnt["o"] += 1
                    n = len(ktiles)
                    for idx, (kt, diag) in enumerate(ktiles):
                        ss = cnt["s"] % 3
                        cnt["s"] += 1
                        pp = cnt["p"] % 3
                        cnt["p"] += 1
                        steps.append((kvh, so, idx, n, kt, diag, ss, pp))

                def S(j):
                    kvh, so, idx, n, kt, diag, ss, pp = steps[j]
                    mm(trk, psS[ss][:, :], [(kaug[0:96, kvh, kt * 128:(kt + 1) * 128], qt[sl][0:96, kvh * 512:(kvh + 1) * 512])],
                       ["qt%d" % sl], ["psS%d" % ss])

                def E(j):
                    kvh, so, idx, n, kt, diag, ss, pp = steps[j]
                    trk.op("act", lambda a: a.activation(out=PT[pp][:, :], in_=psS[ss][:, :], func=AF.Exp),
                           ["psS%d" % ss], ["PT%d" % pp])
                    if diag:
                        trk.op("dve", lambda v: v.tensor_tensor(out=PT[pp][:, :].rearrange("p (h q) -> p h q", h=4),
                                                                in0=PT[pp][:, :].rearrange("p (h q) -> p h q", h=4),
                                                                in1=trib[:, :].unsqueeze(1).broadcast_to([128, 4, 128]), op=ALU.mult),
                               ["PT%d" % pp, "trib"], ["PT%d" % pp])

                def PV(j):
                    kvh, so, idx, n, kt, diag, ss, pp = steps[j]
                    def pv(pe):
                        return pe.matmul(psO[so][:, :], vaug[:, kt, kvh, :], PT[pp][:, :], start=(idx == 0), stop=(idx == n - 1))
                    trk.op("pe", pv, ["PT%d" % pp], ["psO%d" % so])
                    if idx != n - 1:
                        return
                    rs = so
                    trk.op("dve", lambda v: v.reciprocal(out=rden[rs][64:128, :], in_=psO[so][64:128, :]), ["psO%d" % so], ["rden%d" % rs])
                    if debug:
                        trk.dma("sp", dbg_rd[ti, kvh, 64:128, :], rden[rs][64:128, :], ["rden%d" % rs], ["dbg_rd"])
                    for i in range(4):
                        pb = (i % 2) * 64
                        trk.op("dve", lambda v: v.tensor_tensor(out=aT[sl][pb:pb + 64, kvh * 2 + i // 2, :],
                                                                in0=psO[so][0:64, i * 128:(i + 1) * 128],
                                                                in1=rden[rs][64:128, i * 128:(i + 1) * 128], op=ALU.mult),
                               ["psO%d" % so, "rden%d" % rs], ["aT%d_%d_%d" % (sl, kvh, i)])

                N = len(steps)
                S(0)
                if N > 1:
                    S(1)
                for j in range(N):
                    E(j)
                    if j + 2 < N:
                        S(j + 2)
                    PV(j)


            def tail(ti):
                sl = ti % 2
                akeys = ["aT%d_%d_%d" % (sl, k_, i_) for k_ in range(4) for i_ in range(4)]
                if debug:
                    trk.dma("sp", dbg_aT[ti], aT[sl][:, :, :], akeys, ["dbg_aT"])
                for half in range(2):
                    mm(trk, psW[:, half, :], [(aT[sl][:, c, :], wo[:, c, half * 512:(half + 1) * 512]) for c in range(8)],
                       akeys + ["wo"], ["psW"])
                for half in range(2):
                    trk.op("dve", lambda v: v.scalar_tensor_tensor(out=yb[sl][:, half * 512:(half + 1) * 512],
                                                                   in0=xf[sl][:, half * 512:(half + 1) * 512], scalar=ALPHA,
                                                                   in1=psW[:, half, :], op0=ALU.mult, op1=ALU.add),
                           ["xf%d" % sl, "psW"], ["y%d" % sl])
                layer_norm(trk, LNP, yb[sl], "y%d" % sl, lng, lnb, yb[sl], "y%d" % sl, sl)
                trk.dma("sp", X1[ti * 128:(ti + 1) * 128, :], yb[sl][:, :], ["y%d" % sl], ["X1"])
                trk.op("act", lambda a: a.activation(out=x1b[sl][:, :], in_=yb[sl][:, :], func=AF.Copy), ["y%d" % sl], ["x1b%d" % sl])
                transposes(trk, [(psT2[:, kc * 128:(kc + 1) * 128], x1b[sl][:, kc * 128:(kc + 1) * 128]) for kc in range(8)],
                           identb[:, :], ["x1b%d" % sl, "identb"], ["psT2"])
                trk.op("act", lambda a: a.activation(out=x1T[sl][:, :, :], in_=psT2[:, :].rearrange("p (k c) -> p k c", k=8), func=AF.Copy),
                       ["psT2"], ["x1T%d" % sl])
                trk.dma("sp", X1T[:, :, ti * 128:(ti + 1) * 128], x1T[sl][:, :, :], ["x1T%d" % sl], ["X1T"])

            NTB = int(os.environ.get("NTB", str(NT)))
            for ti in range(NTB):
                attention(ti)
                if ti >= 1:
                    tail(ti - 1)
            tail(NTB - 1)
            done()
    if stop_after == 2:
        return nc, es, trk

    def ffn_layer(layer, XTd, Xd, pd, chunks, experts, use_gates, Yd, YTd):
        for (t0, nt) in chunks:
            with ExitStack() as ck:
                ncol = nt * 128
                XTc = sb(ck, "XTc", [128, 8, ncol], BF16)
                acc = sb(ck, "acc", [128, nt, D], F32)
                for kc in range(8):
                    trk.dma("sp", XTc[:, kc, :], XTd[:, kc, t0 * 128:t0 * 128 + ncol], [], ["XTc"], sk=("d", "XTc"))
                for t in range(nt):
                    r0 = (t0 + t) * 128
                    trk.dma("sp", acc[:, t, :], Xd[r0:r0 + 128, :], [], ["accld"], sk=("d", "accld"))
                for t in range(nt):
                    trk.op("act", lambda a: a.mul(out=acc[:, t, :], in_=acc[:, t, :], mul=ALPHA), ["accld"], ["acc%d" % t])
                if use_gates:
                    gt = sb(ck, "gt", [128, nt, 8], F32)
                    trk.dma("sp", gt[:, :, :], GATES[:, t0:t0 + nt, :], [], ["gt"])
                with ExitStack() as ph:
                    wg = [sb(ph, "wg%d" % i, [128, 8, 512], BF16) for i in range(2)]
                    wu = [sb(ph, "wu%d" % i, [128, 8, 512], BF16) for i in range(2)]
                    wd = [sb(ph, "wd%d" % i, [128, 4, D], BF16) for i in range(2)]
                    sg = [sb(ph, "sg%d" % i, [128, 512], F32) for i in range(2)]
                    hh = [sb(ph, "hh%d" % i, [128, 4, 512], BF16) for i in range(2)]
                    psG = [ps(ph, "psG%d" % i, [128, 512]) for i in range(2)]
                    psU = [ps(ph, "psU%d" % i, [128, 512]) for i in range(2)]
                    psY = [ps(ph, "psY%d" % i, [128, 2, 512]) for i in range(2)]
                    steps = [(e, fb) for e in range(len(experts)) for fb in range(NFB)]

                    def load(i):
                        e, fb = steps[i]
                        sl = i % 2
                        wg_ap, wu_ap, wd_ap = experts[e]
                        fs = slice(fb * 512, (fb + 1) * 512)
                        trk.dma("pool", wg[sl][:, :, :], wg_ap.rearrange("(k p) n -> p k n", p=128)[:, :, fs], [], ["wg%d" % sl])
                        trk.dma("pool", wu[sl][:, :, :], wu_ap.rearrange("(k p) n -> p k n", p=128)[:, :, fs], [], ["wu%d" % sl])
                        trk.dma("pool", wd[sl][:, :, :], wd_ap[fs, :].rearrange("(c p) n -> p c n", p=128), [], ["wd%d" % sl])

                    groups = []
                    tt = 0
                    while tt < nt:
                        gn = min(4, nt - tt)
                        groups.append((tt, gn))
                        tt += gn
                    c1 = 0
                    c2 = 0
                    c3 = 0
                    load(0)
                    for i, (e, fb) in enumerate(steps):
                        if i + 1 < len(steps):
                            load(i + 1)
                        sl = i % 2
                        for (g0, gn) in groups:
                            cols = slice(g0 * 128, (g0 + gn) * 128)
                            ncl = gn * 128
                            hs = c2 % 2
                            c2 += 1
                            for fc in range(4):
                                a_ = c1 % 2
                                c1 += 1
                                fsl = slice(fc * 128, (fc + 1) * 128)
                                mm(trk, psG[a_][:, 0:ncl], [(wg[sl][:, kc, fsl], XTc[:, kc, cols]) for kc in range(8)],
                                   ["wg%d" % sl, "XTc"], ["psG%d" % a_])
                                mm(trk, psU[a_][:, 0:ncl], [(wu[sl][:, kc, fsl], XTc[:, kc, cols]) for kc in range(8)],
                                   ["wu%d" % sl, "XTc"], ["psU%d" % a_])
                                trk.op("act", lambda a: a.activation(out=sg[a_][:, 0:ncl], in_=psG[a_][:, 0:ncl], func=AF.Silu),
                                       ["psG%d" % a_], ["sg%d" % a_])
                                trk.op("dve", lambda v: v.tensor_tensor(out=hh[hs][:, fc, 0:ncl], in0=sg[a_][:, 0:ncl], in1=psU[a_][:, 0:ncl],
                                                                        op=ALU.mult), ["sg%d" % a_, "psU%d" % a_], ["hh%d_%d" % (hs, fc)])
                            hkeys = ["hh%d_%d" % (hs, fc) for fc in range(4)]
                            for tl in range(gn):
                                t = g0 + tl
                                ys = c3 % 2
                                c3 += 1
                                for half in range(2):
                                    mm(trk, psY[ys][:, half, :], [(hh[hs][:, fc, tl * 128:(tl + 1) * 128], wd[sl][:, fc, half * 512:(half + 1) * 512])
                                                                 for fc in range(4)], hkeys + ["wd%d" % sl], ["psY%d" % ys])
                                for half in range(2):
                                    hsl = slice(half * 512, (half + 1) * 512)
                                    if use_gates:
                                        trk.op("dve", lambda v: v.scalar_tensor_tensor(out=acc[:, t, hsl], in0=psY[ys][:, half, :],
                                                                                       scalar=gt[:, t, e:e + 1], in1=acc[:, t, hsl],
                                                                                       op0=ALU.mult, op1=ALU.add),
                                               ["psY%d" % ys, "acc%d" % t, "gt"], ["acc%d" % t])
                                    else:
                                        trk.op("dve", lambda v: v.tensor_tensor(out=acc[:, t, hsl], in0=psY[ys][:, half, :], in1=acc[:, t, hsl],
                                                                                op=ALU.add), ["psY%d" % ys, "acc%d" % t], ["acc%d" % t])
                    done()
                with ExitStack() as ph:
                    wpg = sb(ph, "wpg", [128, 8, D], BF16)
                    wpp = sb(ph, "wpp", [128, 2, D], BF16)
                    load_w("pool", wpg, w_ple_gate[layer], "wpg", nsplit=2)
                    load_w("pool", wpp, w_ple_proj[layer], "wpp")
                    lng = sb(ph, "lng", [128, D], F32)
                    lnb = sb(ph, "lnb", [128, D], F32)
                    trk.dma("sp", lng[:, :], ln_ffn_g[layer:layer + 1, :].partition_broadcast(128), [], ["lng"])
                    trk.dma("sp", lnb[:, :], ln_ffn_b[layer:layer + 1, :].partition_broadcast(128), [], ["lnb"])
                    x2b = [sb(ph, "x2b%d" % i, [128, D], BF16) for i in range(2)]
                    x2T = [sb(ph, "x2T%d" % i, [128, 8, 128], BF16) for i in range(2)]
                    pb = [sb(ph, "pb%d" % i, [128, 256], BF16) for i in range(2)]
                    pT = [sb(ph, "pT%d" % i, [128, 2, 128], BF16) for i in range(2)]
                    sig = [sb(ph, "sig%d" % i, [128, 2, 512], F32) for i in range(2)]
                    ob = [sb(ph, "ob%d" % i, [128, D], BF16) for i in range(2)]
                    oT = [sb(ph, "oT%d" % i, [128, 8, 128], BF16) for i in range(2)]
                    LNP = {"st6": [sb(ph, "st6_%d" % i, [128, 12], F32) for i in range(2)],
                           "mv": [sb(ph, "mv_%d" % i, [128, 4], F32) for i in range(2)]}
                    psT = ps(ph, "psT", [128, 1024], BF16)
                    psA = ps(ph, "psA", [128, 2, 512])
                    psB = ps(ph, "psB", [128, 2, 512])
                    psTp = ps(ph, "psTp", [128, 1024], BF16)
                    psT3 = ps(ph, "psT3", [128, 1024], BF16)
                    for t in range(nt):
                        sl = t % 2
                        r0 = (t0 + t) * 128
                        ak = "acc%d" % t
                        x2 = acc[:, t, :]
                        trk.dma("pool", pb[sl][:, :], pd[r0:r0 + 128, :], [], ["pb%d" % sl])
                        layer_norm(trk, LNP, x2, ak, lng, lnb, x2, ak, sl)
                        trk.op("act", lambda a: a.activation(out=x2b[sl][:, :], in_=x2, func=AF.Copy), [ak], ["x2b%d" % sl])
                        transposes(trk, [(psT[:, kc * 128:(kc + 1) * 128], x2b[sl][:, kc * 128:(kc + 1) * 128]) for kc in range(8)],
                                   identb[:, :], ["x2b%d" % sl, "identb"], ["psT"])
                        trk.op("act", lambda a: a.activation(out=x2T[sl][:, :, :], in_=psT[:, :].rearrange("p (k c) -> p k c", k=8), func=AF.Copy),
                               ["psT"], ["x2T%d" % sl])
                        for half in range(2):
                            mm(trk, psA[:, half, :], [(x2T[sl][:, kc, :], wpg[:, kc, half * 512:(half + 1) * 512]) for kc in range(8)],
                               ["x2T%d" % sl, "wpg"], ["psA"])
                        trk.op("act", lambda a: a.activation(out=sig[sl][:, :, :], in_=psA[:, :, :], func=AF.Sigmoid), ["psA"], ["sig%d" % sl])
                        transposes(trk, [(psTp[:, c * 128:(c + 1) * 128], pb[sl][:, c * 128:(c + 1) * 128]) for c in range(2)],
                                   identb[:, :], ["pb%d" % sl, "identb"], ["psTp"])
                        trk.op("act", lambda a: a.activation(out=pT[sl][:, :, :], in_=psTp[:, 0:256].rearrange("p (k c) -> p k c", k=2), func=AF.Copy),
                               ["psTp"], ["pT%d" % sl])
                        for half in range(2):
                            mm(trk, psB[:, half, :], [(pT[sl][:, c, :], wpp[:, c, half * 512:(half + 1) * 512]) for c in range(2)],
                               ["pT%d" % sl, "wpp"], ["psB"])
                        trk.op("dve", lambda v: v.tensor_tensor(out=sig[sl][:, :, :], in0=psB[:, :, :], in1=sig[sl][:, :, :], op=ALU.mult),
                               ["psB", "sig%d" % sl], ["sig%d" % sl])
                        trk.op("dve", lambda v: v.tensor_tensor(out=x2, in0=x2, in1=sig[sl][:, :, :].rearrange("p a b -> p (a b)"), op=ALU.add),
                               [ak, "sig%d" % sl], [ak])
                        okey = "out" if Yd is out_d else "Yd%d" % layer
                        trk.dma("sp", Yd[r0:r0 + 128, :], x2, [ak], [okey])
                        if YTd is not None:
                            trk.op("act", lambda a: a.activation(out=ob[sl][:, :], in_=x2, func=AF.Copy), [ak], ["ob%d" % sl])
                            transposes(trk, [(psT3[:, kc * 128:(kc + 1) * 128], ob[sl][:, kc * 128:(kc + 1) * 128]) for kc in range(8)],
                                       identb[:, :], ["ob%d" % sl, "identb"], ["psT3"])
                            trk.op("act", lambda a: a.activation(out=oT[sl][:, :, :], in_=psT3[:, :].rearrange("p (k c) -> p k c", k=8), func=AF.Copy),
                                   ["psT3"], ["oT%d" % sl])
                            trk.dma("sp", YTd[:, :, r0:r0 + 128], oT[sl][:, :, :], ["oT%d" % sl], ["YTd%d" % layer])
                    done()

    ffn_layer(0, X1T, X1, p0, [(0, 17), (17, 16)], [(w_ffn_gate[0], w_ffn_up[0], w_ffn_down[0])], False, X3, X3T)
    if stop_after == 3:
        return nc, es, trk

    with ExitStack() as kv1:
        k1 = sb(kv1, "k1", [64, 4, NTOK], BF16)
        v1 = sb(kv1, "v1", [128, NT, 4, 128], BF16)
        with ExitStack() as ph:
            wqkv = sb(ph, "wqkv1", [128, 8, 1536], BF16)
            load_w("pool", wqkv, w_qkv[1], "wqkv", nsplit=4)
            trk.op("pool", lambda g: g.memset(v1[:, :, :, :], 1.0), [], ["v1_init"])
            XT = [sb(ph, "XTd%d" % i, [128, 8, 512], BF16) for i in range(2)]
            ctt = [sb(ph, "ctd%d" % i, [64, 512], F32) for i in range(2)]
            stt = [sb(ph, "std%d" % i, [64, 512], F32) for i in range(2)]
            t1s = [sb(ph, "t1d%d" % i, [64, 512], F32) for i in range(2)]
            t2s = [sb(ph, "t2d%d" % i, [64, 512], F32) for i in range(2)]
            krbs = [sb(ph, "krbd%d" % i, [64, 512], BF16) for i in range(2)]
            qg = [sb(ph, "qg%d" % i, [64, 4, 16, 128], BF16) for i in range(2)]
            psK = [ps(ph, "psK%d" % i, [64, 512]) for i in range(2)]
            psPp = [ps(ph, "psP%d" % i, [64, 512]) for i in range(2)]
            psV = [ps(ph, "psV%d" % i, [128, 512]) for i in range(2)]
            ki = 0
            vi = 0
            groups = [(0, 1)] + [(1 + 4 * i, 4) for i in range(8)]
            for gi, (t0, gn) in enumerate(groups):
                gs = gi % 2
                ncl = gn * 128
                cols = slice(t0 * 128, t0 * 128 + ncl)
                ecols = slice(3968 + t0 * 128, 3968 + t0 * 128 + ncl)
                for kc in range(8):
                    trk.dma("sp", XT[gs][:, kc, 0:ncl], X3T[:, kc, cols], [], ["XT%d" % gs], sk=("d", "XTd%d" % gs))
                trk.dma("sp", ctt[gs][:, 0:ncl], CT[:, ecols], [], ["ct%d" % gs])
                trk.dma("sp", stt[gs][:, 0:ncl], ST[:, ecols], [], ["st%d" % gs])

                def proj_rope(c0, scale, out_ap, outk):
                    nonlocal ki
                    s2 = ki % 2
                    ki += 1
                    t1 = t1s[s2]; t2 = t2s[s2]; krb = krbs[s2]; psP = psPp[s2]
                    mm(trk, psK[s2][:, 0:ncl], [(wqkv[:, kc, c0:c0 + 64], XT[gs][:, kc, 0:ncl]) for kc in range(8)],
                       ["XT%d" % gs, "wqkv"], ["psK%d" % s2])
                    trk.op("dve", lambda v: v.scalar_tensor_tensor(out=t1[:, 0:ncl], in0=psK[s2][:, 0:ncl], scalar=scale,
                                                                   in1=ctt[gs][:, 0:ncl], op0=ALU.mult, op1=ALU.mult),
                           ["psK%d" % s2, "ct%d" % gs], ["t1_%d" % s2])
                    trk.op("act", lambda a: a.activation(out=krb[:, 0:ncl], in_=psK[s2][:, 0:ncl], func=AF.Copy, scale=scale),
                           ["psK%d" % s2], ["krb_%d" % s2])
                    mm(trk, psP[:, 0:ncl], [(pm[:, :], krb[:, 0:ncl])], ["krb_%d" % s2, "pm"], ["psP%d" % s2])
                    trk.op("dve", lambda v: v.tensor_tensor(out=t2[:, 0:ncl], in0=psP[:, 0:ncl], in1=stt[gs][:, 0:ncl], op=ALU.mult),
                           ["psP%d" % s2, "st%d" % gs], ["t2_%d" % s2])
                    if len(out_ap.shape) == 2:
                        trk.op("dve", lambda v: v.tensor_tensor(out=out_ap, in0=t1[:, 0:ncl], in1=t2[:, 0:ncl], op=ALU.add),
                               ["t1_%d" % s2, "t2_%d" % s2], [outk])
                    else:
                        trk.op("dve", lambda v: v.tensor_tensor(out=out_ap, in0=t1[:, 0:ncl].rearrange("p (s c) -> p s c", c=128),
                                                                in1=t2[:, 0:ncl].rearrange("p (s c) -> p s c", c=128), op=ALU.add),
                               ["t1_%d" % s2, "t2_%d" % s2], [outk])

                bufsD = dict(psKs=[(psK[0], "psK0"), (psK[1], "psK1")], psPs=[(psPp[0], "psP0"), (psPp[1], "psP1")],
                             t1s=[(t1s[0], "t1_0"), (t1s[1], "t1_1")], t2s=[(t2s[0], "t2_0"), (t2s[1], "t2_1")],
                             krbs=[(krbs[0], "krb_0"), (krbs[1], "krb_1")])
                rope_pipeline([(1024 + kvh * 64, 1.0, k1[0:64, kvh, cols], "k1_%d_%d" % (gi, kvh)) for kvh in range(4)],
                              wqkv, lambda kc: XT[gs][:, kc, 0:ncl], ["XT%d" % gs], ncl, ctt[gs][:, 0:ncl], stt[gs][:, 0:ncl],
                              "ct%d" % gs, "st%d" % gs, **bufsD)
                for tl in range(gn):
                    vs = vi % 2
                    vi += 1
                    mm(trk, psV[vs][:, 0:256], [(XT[gs][:, kc, tl * 128:(tl + 1) * 128], wqkv[:, kc, 1280:1536]) for kc in range(8)],
                       ["XT%d" % gs, "wqkv"], ["psV%d" % vs])
                    trk.op("act", lambda a: a.activation(out=v1[:, t0 + tl, :, 0:64], in_=psV[vs][:, 0:256].rearrange("p (h d) -> p h d", h=4),
                                                         func=AF.Copy), ["psV%d" % vs, "v1_init"], ["v1_%d" % (t0 + tl)])
                if t0 == 0:
                    continue
                qs = gi % 2
                rope_pipeline([(h * 64, 0.125, qg[qs][0:64, :, h, :], "qg%d" % qs) for h in range(16)],
                              wqkv, lambda kc: XT[gs][:, kc, 0:ncl], ["XT%d" % gs], ncl, ctt[gs][:, 0:ncl], stt[gs][:, 0:ncl],
                              "ct%d" % gs, "st%d" % gs, **bufsD)
                trk.dma("sp", Q1[:, t0 - 1:t0 + 3, :], qg[qs][:, :, :, :].rearrange("p s h c -> p s (h c)"), ["qg%d" % qs], ["Q1"])
            done()
        if stop_after == 4:
            return nc, es, trk
        with ExitStack() as ph:
            wo = sb(ph, "wo1", [128, 8, D], BF16)
            load_w("pool", wo, w_o[1], "wo", nsplit=2)
            lng = sb(ph, "lng1", [128, D], F32)
            lnb = sb(ph, "lnb1", [128, D], F32)
            trk.dma("sp", lng[:, :], ln_mix_g[1:2, :].partition_broadcast(128), [], ["lng"])
            trk.dma("sp", lnb[:, :], ln_mix_b[1:2, :].partition_broadcast(128), [], ["lnb"])
            wr = sb(ph, "wr", [128, 8, 8], F32)
            trk.dma("sp", wr[:, :, :], w_router[0].rearrange("(k p) n -> p k n", p=128), [], ["wr"])
            esr = sb(ph, "esr", [128, 16], F32)
            esink = sb(ph, "esink", [128, 16], F32)
            trk.dma("sp", esr[:, :], sinks[0:1, :].partition_broadcast(128), [], ["esr"])
            trk.op("act", lambda a: a.activation(out=esink[:, :], in_=esr[:, :], func=AF.Exp), ["esr"], ["esink"])
            qt = [sb(ph, "qt1_%d" % i, [64, 2048], BF16) for i in range(2)]
            PT = [sb(ph, "PT1_%d" % i, [128, 512], BF16) for i in range(4)]
            den = [sb(ph, "den%d" % i, [128, 512], F32) for i in range(2)]
            rden = [sb(ph, "rden1_%d" % i, [128, 512], F32) for i in range(2)]
            aT = [sb(ph, "aT1_%d" % i, [128, 8, 128], BF16) for i in range(2)]
            xf = [sb(ph, "xf1_%d" % i, [128, D], F32) for i in range(2)]
            yb = [sb(ph, "y1_%d" % i, [128, D], F32) for i in range(2)]
            x4b = [sb(ph, "x4b%d" % i, [128, D], BF16) for i in range(2)]
            x4T = [sb(ph, "x4T%d" % i, [128, 8, 128], BF16) for i in range(2)]
            x4T32 = sb(ph, "x4T32", [128, 8, 128], F32)
            lg = sb(ph, "lg", [128, 8], F32)
            m8 = sb(ph, "m8r", [128, 8], F32)
            d8 = sb(ph, "d8", [128, 8], F32)
            e8 = sb(ph, "e8", [128, 8], F32)
            msk = sb(ph, "msk", [128, 8], F32)
            ss = sb(ph, "ss", [128, 2], F32)
            gtl = [sb(ph, "gtl%d" % i, [128, 8], F32) for i in range(2)]
            LNP = {"st6": [sb(ph, "st6d_%d" % i, [128, 12], F32) for i in range(2)],
                   "mv": [sb(ph, "mvd_%d" % i, [128, 4], F32) for i in range(2)]}
            psS = [ps(ph, "psS%d" % i, [128, 512]) for i in range(2)]
            psO = [ps(ph, "psO%d" % i, [128, 512]) for i in range(2)]
            psW = ps(ph, "psW", [128, 2, 512])
            psT2 = ps(ph, "psT2", [128, 1024], BF16)
            psL = ps(ph, "psL", [128, 512])
            cnt = {"s": 0, "p": 0, "o": 0}

            def attention1(ti):
                sl = ti % 2
                o_ = ti - 1
                trk.dma("sp", qt[sl][:, :], Q1[:, o_, :], [], ["qt%d" % sl])
                trk.dma("sp", xf[sl][:, :], X3[ti * 128:(ti + 1) * 128, :], [], ["xf%d" % sl])
                steps = []
                for kvh in range(4):
                    so = cnt["o"] % 2
                    cnt["o"] += 1
                    parts = [(ti - 1, tricfv if ti == 1 else tricb, "tricfv" if ti == 1 else "tricb"), (ti, trib, "trib")]
                    for idx, (kt, mask, mkey) in enumerate(parts):
                        ss_ = cnt["s"] % 2
                        cnt["s"] += 1
                        pp = cnt["p"] % 4
                        cnt["p"] += 1
                        steps.append((kvh, so, idx, kt, mask, mkey, ss_, pp))

                def S(j):
                    kvh, so, idx, kt, mask, mkey, ss_, pp = steps[j]
                    mm(trk, psS[ss_][:, :], [(k1[0:64, kvh, kt * 128:(kt + 1) * 128], qt[sl][0:64, kvh * 512:(kvh + 1) * 512])],
                       ["qt%d" % sl], ["psS%d" % ss_])

                def E(j):
                    kvh, so, idx, kt, mask, mkey, ss_, pp = steps[j]
                    trk.op("act", lambda a: a.activation(out=PT[pp][:, :], in_=psS[ss_][:, :], func=AF.Exp), ["psS%d" % ss_], ["PT%d" % pp])
                    trk.op("dve", lambda v: v.tensor_tensor(out=PT[pp][:, :].rearrange("p (h q) -> p h q", h=4),
                                                            in0=PT[pp][:, :].rearrange("p (h q) -> p h q", h=4),
                                                            in1=mask[:, :].unsqueeze(1).broadcast_to([128, 4, 128]), op=ALU.mult),
                           ["PT%d" % pp, mkey], ["PT%d" % pp])

                def PV(j):
                    kvh, so, idx, kt, mask, mkey, ss_, pp = steps[j]
                    def pv(pe):
                        return pe.matmul(psO[so][:, :], v1[:, kt, kvh, :], PT[pp][:, :], start=(idx == 0), stop=(idx == 1))
                    trk.op("pe", pv, ["PT%d" % pp], ["psO%d" % so])
                    if idx != 1:
                        return
                    rs = so
                    for i in range(4):
                        h = kvh * 4 + i
                        trk.op("dve", lambda v: v.tensor_scalar(out=den[rs][64:128, i * 128:(i + 1) * 128], in0=psO[so][64:128, i * 128:(i + 1) * 128],
                                                                scalar1=esink[64:128, h:h + 1], scalar2=None, op0=ALU.add),
                               ["psO%d" % so, "esink"], ["den%d_%d" % (rs, i)])
                    trk.op("dve", lambda v: v.reciprocal(out=rden[rs][64:128, :], in_=den[rs][64:128, :]),
                           ["den%d_%d" % (rs, i) for i in range(4)], ["rden%d" % rs])
                    for i in range(4):
                        pb_ = (i % 2) * 64
                        trk.op("dve", lambda v: v.tensor_tensor(out=aT[sl][pb_:pb_ + 64, kvh * 2 + i // 2, :],
                                                                in0=psO[so][0:64, i * 128:(i + 1) * 128],
                                                                in1=rden[rs][64:128, i * 128:(i + 1) * 128], op=ALU.mult),
                               ["psO%d" % so, "rden%d" % rs], ["aT%d_%d_%d" % (sl, kvh, i)])

                N = len(steps)
                S(0)
                S(1)
                for j in range(N):
                    E(j)
                    if j + 2 < N:
                        S(j + 2)
                    PV(j)

            def tail1(ti):
                sl = ti % 2
                o_ = ti - 1
                akeys = ["aT%d_%d_%d" % (sl, k_, i_) for k_ in range(4) for i_ in range(4)]
                for half in range(2):
                    mm(trk, psW[:, half, :], [(aT[sl][:, c, :], wo[:, c, half * 512:(half + 1) * 512]) for c in range(8)],
                       akeys + ["wo"], ["psW"])
                for half in range(2):
                    trk.op("dve", lambda v: v.scalar_tensor_tensor(out=yb[sl][:, half * 512:(half + 1) * 512],
                                                                   in0=xf[sl][:, half * 512:(half + 1) * 512], scalar=ALPHA,
                                                                   in1=psW[:, half, :], op0=ALU.mult, op1=ALU.add),
                           ["xf%d" % sl, "psW"], ["y%d" % sl])
                layer_norm(trk, LNP, yb[sl], "y%d" % sl, lng, lnb, yb[sl], "y%d" % sl, sl)
                trk.dma("sp", X4[o_ * 128:(o_ + 1) * 128, :], yb[sl][:, :], ["y%d" % sl], ["X4"])
                trk.op("act", lambda a: a.activation(out=x4b[sl][:, :], in_=yb[sl][:, :], func=AF.Copy), ["y%d" % sl], ["x4b%d" % sl])
                transposes(trk, [(psT2[:, kc * 128:(kc + 1) * 128], x4b[sl][:, kc * 128:(kc + 1) * 128]) for kc in range(8)],
                           identb[:, :], ["x4b%d" % sl, "identb"], ["psT2"])
                trk.op("act", lambda a: a.activation(out=x4T[sl][:, :, :], in_=psT2[:, :].rearrange("p (k c) -> p k c", k=8), func=AF.Copy),
                       ["psT2"], ["x4T%d" % sl])
                trk.dma("sp", X4T[:, :, o_ * 128:(o_ + 1) * 128], x4T[sl][:, :, :], ["x4T%d" % sl], ["X4T"])
                transposes(trk, [(psW[:, kc // 4, (kc % 4) * 128:(kc % 4 + 1) * 128], yb[sl][:, kc * 128:(kc + 1) * 128]) for kc in range(8)],
                           identf[:, :], ["y%d" % sl, "identf"], ["psW"])
                trk.op("act", lambda a: a.activation(out=x4T32[:, :, :].rearrange("p (a b) c -> p a b c", a=2),
                                                     in_=psW[:, :, :].rearrange("p a (b c) -> p a b c", b=4), func=AF.Copy),
                       ["psW"], ["x4T32"])
                mm(trk, psL[:, 0:8], [(x4T32[:, kc, :], wr[:, kc, :]) for kc in range(8)], ["x4T32", "wr"], ["psL"])
                trk.op("dve", lambda v: v.tensor_copy(out=lg[:, :], in_=psL[:, 0:8]), ["psL"], ["lg"])
                trk.op("dve", lambda v: v.max(out=m8[:, :], in_=lg[:, :]), ["lg"], ["m8r"])
                trk.op("dve", lambda v: v.tensor_scalar(out=d8[:, :], in0=lg[:, :], scalar1=m8[:, 0:1], scalar2=None, op0=ALU.subtract),
                       ["lg", "m8r"], ["d8"])
                trk.op("act", lambda a: a.activation(out=e8[:, :], in_=d8[:, :], func=AF.Exp), ["d8"], ["e8"])
                trk.op("dve", lambda v: v.tensor_scalar(out=msk[:, :], in0=lg[:, :], scalar1=m8[:, 1:2], scalar2=None, op0=ALU.is_ge),
                       ["lg", "m8r"], ["msk"])
                trk.op("dve", lambda v: v.tensor_tensor(out=e8[:, :], in0=e8[:, :], in1=msk[:, :], op=ALU.mult), ["e8", "msk"], ["e8"])
                trk.op("dve", lambda v: v.tensor_reduce(out=ss[:, 0:1], in_=e8[:, :], axis=AX.X, op=ALU.add), ["e8"], ["ss0"])
                trk.op("dve", lambda v: v.reciprocal(out=ss[:, 1:2], in_=ss[:, 0:1]), ["ss0"], ["ss1"])
                trk.op("dve", lambda v: v.tensor_scalar(out=gtl[sl][:, :], in0=e8[:, :], scalar1=ss[:, 1:2], scalar2=None, op0=ALU.mult),
                       ["e8", "ss1"], ["gtl%d" % sl])
                trk.dma("sp", GATES[:, o_, :], gtl[sl][:, :], ["gtl%d" % sl], ["GATES"])

            for ti in range(1, NT):
                attention1(ti)
                if ti >= 2:
                    tail1(ti - 1)
            tail1(NT - 1)
            done()
    if stop_after == 5:
        return nc, es, trk

    experts = [(w_exp_gate[0, e], w_exp_up[0, e], w_exp_down[0, e]) for e in range(8)]
    ffn_layer(1, X4T, X4, p1, [(0, 16), (16, 16)], experts, True, out_d, None)
    return nc, es, trk


def _consts():
    d = np.arange(64)
    inv = np.zeros(64, np.float64)
    i = d % 8
    invf = 1.0 / (500000.0 ** (np.arange(0, 16, 2, dtype=np.float32) / np.float32(16)))
    inv[:16] = invf.astype(np.float64)[i[:16]]
    sgn = np.zeros(64); sgn[:8] = -1.0; sgn[8:16] = 1.0
    c64 = np.zeros((64, 4), np.float32)
    c64[:, 0] = (inv / (2 * np.pi)).astype(np.float32)
    c64[:, 1] = (sgn * 2 * np.pi).astype(np.float32)
    c64[:, 2] = sgn.astype(np.float32)
    pmat = np.zeros((64, 64), np.float32)
    for a in range(8):
        pmat[a + 8, a] = 1.0
        pmat[a, a + 8] = 1.0
    ident = np.eye(128, dtype=np.float32)
    blk = np.zeros((32, SEQ), np.float32)
    for n in range(32):
        blk[n, n * 256:(n + 1) * 256] = 1.0
    k = np.arange(128)[:, None]; q = np.arange(128)[None, :]
    tri = (q >= k).astype(np.float32)
    return c64, pmat, ident, blk, tri, (1.0 - tri).astype(np.float32)


def _core_inputs(inputs, c, consts, stop_after=None):
    b, h = c // 2, c % 2
    c64, pmat, ident, blk, tri, tric = consts
    x = inputs["x"][b]
    posb = inputs["positions"][b]
    lo = slice(0, HALF)
    own = slice(h * HALF, (h + 1) * HALF)
    xin = np.concatenate([x[lo], x[own]], axis=0)
    pos = np.concatenate([posb[lo], posb[own]])[None, :].astype(np.int32)
    p0e = np.concatenate([inputs["p"][0, b][lo], inputs["p"][0, b][own]], axis=0)[SEQ - NTOK:]
    p1 = inputs["p"][1, b][own]
    gbias = np.zeros((17, 32), np.float32)
    cb = np.zeros((17, 32), np.float32)
    for jj in range(17):
        J = 15 + jj
        for n in range(32):
            valid = (n < J) and (n >= 16 or h == 1)
            gbias[jj, n] = 0.0 if valid else -1e9
            cb[jj, n] = NEG if valid else 3 * NEG
        gbias[jj, J] = -2e9
        cb[jj, J] = 0.0
    m = {
        "xin": np.ascontiguousarray(xin), "pos": pos, "p0": np.ascontiguousarray(p0e), "p1": np.ascontiguousarray(p1),
        "cst64": c64, "pmat": pmat, "ident": ident, "blkind": blk, "tri": tri, "tric": tric,
        "gbias": gbias.reshape(1, -1), "cb": cb.reshape(1, -1), "fv": np.full((128, 1), float(h), np.float32),
    }
    for k in ("w_qkv", "w_o", "ln_mix_g", "ln_mix_b", "ln_ffn_g", "ln_ffn_b", "sinks", "w_ffn_gate", "w_ffn_up",
              "w_ffn_down", "w_router", "w_exp_gate", "w_exp_up", "w_exp_down", "w_ple_proj", "w_ple_gate"):
        if k.startswith("w_exp") and not (stop_after is None or stop_after >= 5):
            continue
        m[k] = np.ascontiguousarray(inputs[k], dtype=np.float32)
    return m


def run(inputs, stop_after=None, debug=False, cores=8, trace=False):
    inputs = {k: np.asarray(v) for k, v in inputs.items()}
    consts = _consts()
    nc, es, trk = build_program(stop_after=stop_after, debug=debug)
    in_maps = [_core_inputs(inputs, c, consts, stop_after) for c in range(cores)]
    res = run_bass_kernel_spmd(nc, in_maps, core_ids=list(range(cores)), trace=trace)
    es.close()
    return res


def kernel(**inputs):
    res = run(inputs)
    out = np.zeros((4, SEQ, D), np.float32)
    for c in range(8):
        b, h = c // 2, c % 2
        out[b, h * HALF:(h + 1) * HALF] = res.results[c]["out"]
    return out
```

```python
from contextlib import ExitStack
import os
import numpy as np
import concourse.bass as bass
import concourse.mybir as mybir
from concourse.bass_utils import run_bass_kernel_spmd

F32 = mybir.dt.float32
BF16 = mybir.dt.bfloat16
I32 = mybir.dt.int32
AF = mybir.ActivationFunctionType
ALU = mybir.AluOpType
AX = mybir.AxisListType

D = 1024
SEQ = 8192
HALF = 4096
NT = 33
NTOK = NT * 128
DFF = 3584
NFB = 7
ALPHA = 4.0 ** 0.25
EPS = 1e-5
TWO_PI = 6.283185307179586
NEG = -30000.0


class Trk:
    def __init__(self, nc, es):
        self.nc = nc
        self.es = es
        self.eng = {"pe": nc.tensor, "act": nc.scalar, "dve": nc.vector, "pool": nc.gpsimd, "sp": nc.sync}
        self.sem = {}
        self.cnt = {}
        self.seen = {e: {} for e in self.eng}
        self.lw = {}
        self.rd = {}

    def _sem(self, k):
        if k not in self.sem:
            self.sem[k] = self.es.enter_context(self.nc.semaphore("s%d" % len(self.sem)))
            self.cnt[k] = 0
        return self.sem[k]

    def _waits(self, e, reads, writes, own_sk=None):
        need = {}

        def add(sk, v, raw):
            if sk == e and not raw:
                return
            if sk == own_sk and not raw:
                return
            if v > need.get(sk, 0):
                need[sk] = v

        for k in reads:
            if k in self.lw:
                add(self.lw[k][0], self.lw[k][1], True)
        for k in writes:
            if k in self.lw:
                add(self.lw[k][0], self.lw[k][1], False)
            for sk, v in self.rd.get(k, {}).items():
                add(sk, v, False)
        for sk, v in need.items():
            if self.seen[e].get(sk, 0) < v:
                self.eng[e].wait_ge(self.sem[sk], v)
                self.seen[e][sk] = v

    def _record(self, sk, v, reads, writes):
        for k in reads:
            d = self.rd.setdefault(k, {})
            if d.get(sk, 0) < v:
                d[sk] = v
        for k in writes:
            self.lw[k] = (sk, v)
            self.rd[k] = {}

    def op(self, e, fn, reads=(), writes=()):
        self._sem(e)
        extra = [k for k in reads if isinstance(k, str) and k.startswith("ps") and k not in writes]
        if extra:
            writes = list(writes) + extra
        self._waits(e, reads, writes)
        ins = fn(self.eng[e])
        ins.then_inc(self.sem[e], 1)
        self.cnt[e] += 1
        self._record(e, self.cnt[e], reads, writes)

    def dma(self, e, out, in_, reads=(), writes=(), sk=None):
        if sk is None:
            w0 = writes[0]
            if isinstance(w0, str) and (w0[0].isupper() or w0.startswith("dbg") or w0 == "out"):
                sk = ("st", reads[0])
            else:
                sk = ("d", w0)
        self._sem(sk)
        self._waits(e, reads, writes, own_sk=sk)
        ins = self.eng[e].dma_start(out=out, in_=in_)
        ins.then_inc(self.sem[sk], 16)
        self.cnt[sk] += 16
        self._record(sk, self.cnt[sk], reads, writes)

    def barrier(self):
        for e in self.eng:
            for sk, v in self.cnt.items():
                if v > 0 and self.seen[e].get(sk, 0) < v:
                    self.eng[e].wait_ge(self.sem[sk], v)
                    self.seen[e][sk] = v
        self.lw = {}
        self.rd = {}

    def final_wait(self, e, key):
        sk = ("d", key)
        self.eng[e].wait_ge(self.sem[sk], self.cnt[sk])


def mm(trk, out, pairs, reads, writes):
    def fn(pe):
        n = len(pairs)
        ins = None
        for i, (l, r) in enumerate(pairs):
            ins = pe.matmul(out, l, r, start=(i == 0), stop=(i == n - 1))
        return ins
    trk.op("pe", fn, reads, writes)


def transposes(trk, outs_ins, ident, reads, writes):
    def fn(pe):
        ins = None
        for o, i in outs_ins:
            ins = pe.transpose(o, i, ident)
        return ins
    trk.op("pe", fn, reads, writes)


def layer_norm(trk, P, y, yk, g_sb, b_sb, out, outk, slot):
    st6 = P["st6"][slot]
    mv = P["mv"][slot]
    k6 = "st6_%d" % slot
    kmv = "mv_%d" % slot
    krs = "rs_%d" % slot
    trk.op("dve", lambda v: v.bn_stats(out=st6[:, 0:6], in_=y[:, 0:512]), [yk], [k6 + "a"])
    trk.op("dve", lambda v: v.bn_stats(out=st6[:, 6:12], in_=y[:, 512:1024]), [yk], [k6 + "b"])
    trk.op("dve", lambda v: v.bn_aggr(out=mv[:, 0:2], in_=st6[:, 0:12]), [k6 + "a", k6 + "b"], [kmv])
    trk.op("act", lambda a: a.activation(out=mv[:, 2:3], in_=mv[:, 1:2], func=AF.Sqrt, bias=EPS), [kmv], [krs + "s"])
    trk.op("dve", lambda v: v.reciprocal(out=mv[:, 3:4], in_=mv[:, 2:3]), [krs + "s"], [krs])
    trk.op("dve", lambda v: v.tensor_scalar(out=out[:, :], in0=y[:, :], scalar1=mv[:, 0:1], scalar2=mv[:, 3:4],
                                            op0=ALU.subtract, op1=ALU.mult), [yk, kmv, krs], [outk])
    trk.op("dve", lambda v: v.tensor_tensor(out=out[:, :], in0=out[:, :], in1=g_sb[:, :], op=ALU.mult), [outk, "lng"], [outk])
    trk.op("dve", lambda v: v.tensor_tensor(out=out[:, :], in0=out[:, :], in1=b_sb[:, :], op=ALU.add), [outk, "lnb"], [outk])


def build_program(stop_after=None, debug=False):
    nc = bass.Bass("TRN2", target_bir_lowering=False)
    es = ExitStack()
    trk = Trk(nc, es)

    def din(name, shape, dt=F32):
        return nc.dram_tensor(name, list(shape), dt, kind="ExternalInput").ap()

    def dscr(name, shape, dt):
        kind = "ExternalOutput" if debug else "Internal"
        return nc.dram_tensor(name, list(shape), dt, kind=kind).ap()

    xin = din("xin", [SEQ, D])
    pos = din("pos", [1, SEQ], I32)
    p0 = din("p0", [NTOK, 256])
    p1 = din("p1", [HALF, 256])
    cst64 = din("cst64", [64, 4])
    pmat_d = din("pmat", [64, 64])
    ident_d = din("ident", [128, 128])
    blkind = din("blkind", [32, SEQ])
    tri_d = din("tri", [128, 128])
    tric_d = din("tric", [128, 128])
    gbias_d = din("gbias", [1, 17 * 32])
    cb_d = din("cb", [1, 17 * 32])
    fv_d = din("fv", [128, 1])
    w_qkv = din("w_qkv", [2, D, 1536])
    w_o = din("w_o", [2, D, D])
    ln_mix_g = din("ln_mix_g", [2, D])
    ln_mix_b = din("ln_mix_b", [2, D])
    ln_ffn_g = din("ln_ffn_g", [2, D])
    ln_ffn_b = din("ln_ffn_b", [2, D])
    sinks = din("sinks", [1, 16])
    w_ffn_gate = din("w_ffn_gate", [1, D, DFF])
    w_ffn_up = din("w_ffn_up", [1, D, DFF])
    w_ffn_down = din("w_ffn_down", [1, DFF, D])
    w_router = din("w_router", [1, D, 8])
    if stop_after is None or stop_after >= 5:
        w_exp_gate = din("w_exp_gate", [1, 8, D, DFF])
        w_exp_up = din("w_exp_up", [1, 8, D, DFF])
        w_exp_down = din("w_exp_down", [1, 8, DFF, D])
    w_ple_proj = din("w_ple_proj", [2, 256, D])
    w_ple_gate = din("w_ple_gate", [2, D, D])
    out_d = nc.dram_tensor("out", [HALF, D], F32, kind="ExternalOutput").ap()

    CT = dscr("CT", [64, SEQ], F32)
    ST = dscr("ST", [64, SEQ], F32)
    QA = dscr("QA", [96, NT, 16 * 128], BF16)
    X1 = dscr("X1", [NTOK, D], F32)
    X1T = dscr("X1T", [128, 8, NTOK], BF16)
    X3 = dscr("X3", [NTOK, D], F32)
    X3T = dscr("X3T", [128, 8, NTOK], BF16)
    Q1 = dscr("Q1", [64, 32, 16 * 128], BF16)
    X4 = dscr("X4", [HALF, D], F32)
    X4T = dscr("X4T", [128, 8, HALF], BF16)
    GATES = dscr("GATES", [128, 32, 8], F32)

    uid = [0]

    def sb(stk, name, shape, dt):
        uid[0] += 1
        return stk.enter_context(nc.sbuf_tensor("s%d_%s" % (uid[0], name), list(shape), dt))

    def ps(stk, name, shape, dt=F32):
        uid[0] += 1
        return stk.enter_context(nc.psum_tensor("p%d_%s" % (uid[0], name), list(shape), dt))

    def done():
        trk.barrier()

    c64 = sb(es, "c64", [64, 4], F32)
    pm = sb(es, "pm", [64, 64], BF16)
    identb = sb(es, "identb", [128, 128], BF16)
    identf = sb(es, "identf", [128, 128], F32)
    trib = sb(es, "trib", [128, 128], BF16)
    tricb = sb(es, "tricb", [128, 128], BF16)
    tricfv = sb(es, "tricfv", [128, 128], BF16)
    fv = sb(es, "fv", [128, 1], F32)
    trk.dma("sp", c64[:, :], cst64[:, :], [], ["c64"])
    trk.dma("pool", pm[:, :], pmat_d[:, :], [], ["pm"])
    trk.dma("pool", identb[:, :], ident_d[:, :], [], ["identb"])
    trk.dma("sp", identf[:, :], ident_d[:, :], [], ["identf"])
    trk.dma("pool", trib[:, :], tri_d[:, :], [], ["trib"])
    trk.dma("pool", tricb[:, :], tric_d[:, :], [], ["tricb"])
    trk.dma("sp", fv[:, :], fv_d[:, :], [], ["fv"])
    trk.op("dve", lambda v: v.tensor_scalar(out=tricfv[:, :], in0=tricb[:, :], scalar1=fv[:, 0:1], scalar2=None,
                                            op0=ALU.mult), ["tricb", "fv"], ["tricfv"])

    if stop_after == -1:
        done()
        return nc, es, trk
    with ExitStack() as ph:
        CW = 2048
        pi = [sb(ph, "r_pi%d" % i, [64, CW], I32) for i in range(2)]
        u = [sb(ph, "r_u%d" % i, [64, CW], F32) for i in range(2)]
        ni = [sb(ph, "r_ni%d" % i, [64, CW], I32) for i in range(2)]
        nf = [sb(ph, "r_nf%d" % i, [64, CW], F32) for i in range(2)]
        to = [sb(ph, "r_to%d" % i, [64, CW], F32) for i in range(2)]
        it = 0
        for c in range(SEQ // CW):
            cols = slice(c * CW, (c + 1) * CW)
            s = c % 2
            trk.dma("sp", pi[s][:, :], pos[0:1, cols].partition_broadcast(64), [], ["r_pi%d" % s])
            trk.op("dve", lambda v: v.tensor_copy(out=u[s][:, :], in_=pi[s][:, :]), ["r_pi%d" % s], ["r_u%d" % s])
            trk.op("dve", lambda v: v.tensor_scalar(out=u[s][:, :], in0=u[s][:, :], scalar1=c64[:, 0:1], scalar2=None,
                                                    op0=ALU.mult), ["r_u%d" % s, "c64"], ["r_u%d" % s])
            for which in range(2):
                k = it % 2
                it += 1
                if which == 1:
                    trk.op("dve", lambda v: v.tensor_scalar(out=u[s][:, :], in0=u[s][:, :], scalar1=0.25, scalar2=None,
                                                            op0=ALU.add), ["r_u%d" % s], ["r_u%d" % s])
                trk.op("dve", lambda v: v.tensor_copy(out=ni[k][:, :], in_=u[s][:, :]), ["r_u%d" % s], ["r_ni%d" % k])
                trk.op("dve", lambda v: v.tensor_copy(out=nf[k][:, :], in_=ni[k][:, :]), ["r_ni%d" % k], ["r_nf%d" % k])
                trk.op("dve", lambda v: v.tensor_tensor(out=nf[k][:, :], in0=u[s][:, :], in1=nf[k][:, :], op=ALU.subtract),
                       ["r_u%d" % s, "r_nf%d" % k], ["r_nf%d" % k])
                PH0 = int(os.environ.get("PH0", "9"))
                if PH0 == 1:
                    trk.dma("sp", (ST if which == 0 else CT)[:, cols], nf[k][:, :], ["r_nf%d" % k], ["ST" if which == 0 else "CT"])
                    continue
                if PH0 == 2:
                    trk.op("act", lambda a: a.activation(out=to[k][:, :], in_=nf[k][:, :], func=AF.Sin, scale=TWO_PI),
                           ["r_nf%d" % k], ["r_to%d" % k])
                    trk.dma("sp", (ST if which == 0 else CT)[:, cols], to[k][:, :], ["r_to%d" % k], ["ST" if which == 0 else "CT"])
                    continue
                if which == 0:
                    trk.op("dve", lambda v: v.tensor_scalar(out=nf[k][:, :], in0=nf[k][:, :], scalar1=c64[:, 2:3], scalar2=None,
                                                            op0=ALU.mult), ["r_nf%d" % k, "c64"], ["r_nf%d" % k])
                    trk.op("act", lambda a: a.activation(out=to[k][:, :], in_=nf[k][:, :], func=AF.Sin, scale=TWO_PI),
                           ["r_nf%d" % k], ["r_to%d" % k])
                    trk.dma("sp", ST[:, cols], to[k][:, :], ["r_to%d" % k], ["ST"])
                else:
                    trk.op("act", lambda a: a.activation(out=to[k][:, :], in_=nf[k][:, :], func=AF.Sin, scale=TWO_PI),
                           ["r_nf%d" % k], ["r_to%d" % k])
                    trk.dma("sp", CT[:, cols], to[k][:, :], ["r_to%d" % k], ["CT"])
        done()
    if stop_after == 0:
        return nc, es, trk

    def rope_head(P, psrc, psk, ncols, scale, ct_ap, st_ap, ctk, out_ap, outk, slot):
        t1 = P["t1"][slot]
        t2 = P["t2"][slot]
        krb = P["krb"][slot]
        psP = P["psP"][slot]
        k1, k2, kk, kp = "t1_%d" % slot, "t2_%d" % slot, "krb_%d" % slot, "psP_%d" % slot
        trk.op("dve", lambda v: v.scalar_tensor_tensor(out=t1[:, 0:ncols], in0=psrc, scalar=scale, in1=ct_ap,
                                                       op0=ALU.mult, op1=ALU.mult), [psk, ctk], [k1])
        trk.op("act", lambda a: a.activation(out=krb[:, 0:ncols], in_=psrc, func=AF.Copy, scale=scale), [psk], [kk])
        mm(trk, psP[:, 0:ncols], [(pm[:, :], krb[:, 0:ncols])], [kk, "pm"], [kp])
        trk.op("dve", lambda v: v.tensor_tensor(out=t2[:, 0:ncols], in0=psP[:, 0:ncols], in1=st_ap, op=ALU.mult),
               [kp, ctk], [k2])
        trk.op("dve", out_ap_writer(out_ap, t1, t2, ncols), [k1, k2], [outk])

    def out_ap_writer(out_ap, t1, t2, ncols):
        nd = len(out_ap.shape)
        if nd == 2:
            return lambda v: v.tensor_tensor(out=out_ap, in0=t1[:, 0:ncols], in1=t2[:, 0:ncols], op=ALU.add)
        nsub = out_ap.shape[1]
        a = t1[:, 0:ncols].rearrange("p (s c) -> p s c", s=nsub)
        b = t2[:, 0:ncols].rearrange("p (s c) -> p s c", s=nsub)
        return lambda v: v.tensor_tensor(out=out_ap, in0=a, in1=b, op=ALU.add)

    def load_w(eng, dst, src, key, nsplit=1):
        v = src.rearrange("(k p) n -> p k n", p=128)
        kc = v.shape[1]
        step = kc // nsplit
        for i in range(nsplit):
            trk.dma(eng, dst[:, i * step:(i + 1) * step, :], v[:, i * step:(i + 1) * step, :], [], [key], sk=("d", key))

    rp_cnt = [0]

    def rope_pipeline(items, wq, rhs_fn, xkeys, ncl, c_ap, s_ap, ckey, skey, psKs, psPs, t1s, t2s, krbs):
        n = len(items)
        slots = []
        for _ in range(n):
            slots.append(rp_cnt[0])
            rp_cnt[0] += 1

        def MM(i):
            c0 = items[i][0]
            pk, pkk = psKs[slots[i] % len(psKs)]
            mm(trk, pk[:, 0:ncl], [(wq[:, kc, c0:c0 + 64], rhs_fn(kc)) for kc in range(8)], xkeys + ["wqkv"], [pkk])

        def R1(i):
            scale = items[i][1]
            pk, pkk = psKs[slots[i] % len(psKs)]
            t1, t1k = t1s[slots[i] % len(t1s)]
            krb, krk = krbs[slots[i] % len(krbs)]
            trk.op("dve", lambda v: v.scalar_tensor_tensor(out=t1[:, 0:ncl], in0=pk[:, 0:ncl], scalar=scale, in1=c_ap,
                                                           op0=ALU.mult, op1=ALU.mult), [pkk, ckey], [t1k])
            trk.op("act", lambda a: a.activation(out=krb[:, 0:ncl], in_=pk[:, 0:ncl], func=AF.Copy, scale=scale), [pkk], [krk])

        def PM(i):
            krb, krk = krbs[slots[i] % len(krbs)]
            pp, ppk = psPs[slots[i] % len(psPs)]
            mm(trk, pp[:, 0:ncl], [(pm[:, :], krb[:, 0:ncl])], [krk, "pm"], [ppk])

        def R2(i):
            out_ap, outk = items[i][2], items[i][3]
            pp, ppk = psPs[slots[i] % len(psPs)]
            t1, t1k = t1s[slots[i] % len(t1s)]
            t2, t2k = t2s[slots[i] % len(t2s)]
            trk.op("dve", lambda v: v.tensor_tensor(out=t2[:, 0:ncl], in0=pp[:, 0:ncl], in1=s_ap, op=ALU.mult), [ppk, skey], [t2k])
            if len(out_ap.shape) == 2:
                trk.op("dve", lambda v: v.tensor_tensor(out=out_ap, in0=t1[:, 0:ncl], in1=t2[:, 0:ncl], op=ALU.add), [t1k, t2k], [outk])
            else:
                trk.op("dve", lambda v: v.tensor_tensor(out=out_ap, in0=t1[:, 0:ncl].rearrange("p (s c) -> p s c", c=128),
                                                        in1=t2[:, 0:ncl].rearrange("p (s c) -> p s c", c=128), op=ALU.add), [t1k, t2k], [outk])

        MM(0)
        for i in range(n):
            R1(i)
            if i + 1 < n:
                MM(i + 1)
            PM(i)
            R2(i)

    with ExitStack() as kv:
        kaug = sb(kv, "kaug", [96, 4, SEQ], BF16)
        vaug = sb(kv, "vaug", [128, 64, 4, 128], BF16)
        with ExitStack() as ph:
            wqkv = sb(ph, "wqkv", [128, 8, 1536], BF16)
            load_w("pool", wqkv, w_qkv[0], "wqkv", nsplit=4)
            for kvh in range(4):
                trk.dma("pool", kaug[64:96, kvh, :], blkind[:, :], [], ["kaug_ind"], sk=("d", "kaug_ind"))
            trk.op("pool", lambda g: g.memset(vaug[:, :, :, :], 1.0), [], ["vaug_init"])
            kms = sb(ph, "kms", [64, 4, 32], F32)
            kmb = sb(ph, "kmb", [64, 4, 32], BF16)
            trk.op("pool", lambda g: g.memset(kms[:, :, :], 0.0), [], ["kms"])
            trk.op("pool", lambda g: g.memset(kmb[:, :, :], 0.0), [], ["kmb"])
            gbias_sb = sb(ph, "gbias_sb", [128, 17, 32], F32)
            cb_sb = sb(ph, "cb_sb", [128, 17, 32], F32)
            trk.dma("sp", gbias_sb[:, :, :].rearrange("p a b -> p (a b)"), gbias_d.partition_broadcast(128), [], ["gbias"])
            trk.dma("sp", cb_sb[:, :, :].rearrange("p a b -> p (a b)"), cb_d.partition_broadcast(128), [], ["cb"])
            xb = [sb(ph, "xb%d" % i, [128, D], BF16) for i in range(2)]
            XT = [sb(ph, "XT%d" % i, [128, 8, 512], BF16) for i in range(1)] * 2
            ctt = [sb(ph, "ct%d" % i, [64, 512], F32) for i in range(1)] * 2
            stt = [sb(ph, "st%d" % i, [64, 512], F32) for i in range(1)] * 2
            P = {
                "t1": [sb(ph, "t1_%d" % i, [64, 512], F32) for i in range(1)] * 2,
                "t2": [sb(ph, "t2_%d" % i, [64, 512], F32) for i in range(1)] * 2,
                "krb": [sb(ph, "krb_%d" % i, [64, 512], BF16) for i in range(2)],
                "psP": [ps(ph, "psP_%d" % i, [64, 512]) for i in range(2)],
            }
            qa = [sb(ph, "qa%d" % i, [96, 4, 16, 128], BF16) for i in range(1)] * 2
            gb = sb(ph, "gb", [128, 16, 32], F32)
            m8 = sb(ph, "m8", [128, 16, 8], F32)
            sel = sb(ph, "sel", [128, 16, 32], F32)
            stage = sb(ph, "stage", [128, 16, 128], BF16)
            trk.op("pool", lambda g: g.memset(stage[:, :, :], 0.0), [], ["stage"])
            psT = [ps(ph, "psT%d" % i, [128, 1024], BF16) for i in range(1)]
            psK = [ps(ph, "psK%d" % i, [64, 512]) for i in range(2)]
            psV = ps(ph, "psV", [128, 512])
            psG = ps(ph, "psG", [128, 512])
            psM = ps(ph, "psM", [128, 8, 128], BF16)
            xi = 0
            ki = 0
            PHA = int(os.environ.get("PHA", "99"))
            for g in range(16 if PHA > 5 else (1 if PHA > 1 else 0)):
                gs = 0
                gcols = slice(g * 512, (g + 1) * 512)
                trk.dma("sp", ctt[gs][:, :], CT[:, gcols], ["CT"], ["ct0"])
                trk.dma("sp", stt[gs][:, :], ST[:, gcols], ["ST"], ["st0"])
                ctk = "ctst%d" % gs
                for sub in range(4):
                    sl = xi % 2
                    tp = 0
                    xi += 1
                    r0 = g * 512 + sub * 128
                    trk.dma("pool", xb[sl][:, :], xin[r0:r0 + 128, :], [], ["xb%d" % sl])
                    transposes(trk, [(psT[tp][:, kc * 128:(kc + 1) * 128], xb[sl][:, kc * 128:(kc + 1) * 128]) for kc in range(8)],
                               identb[:, :], ["xb%d" % sl, "identb"], ["psT%d" % tp])
                    trk.op("act", lambda a: a.activation(out=XT[gs][:, :, sub * 128:(sub + 1) * 128],
                                                         in_=psT[tp][:, :].rearrange("p (k c) -> p k c", k=8), func=AF.Copy),
                           ["psT%d" % tp], ["XT%d_%d" % (gs, sub)])
                xtk = ["XT%d_%d" % (gs, s_) for s_ in range(4)]
                bufsA = dict(psKs=[(psK[0], "psK0"), (psK[1], "psK1")], psPs=[(P["psP"][0], "psP_0"), (P["psP"][1], "psP_1")],
                             t1s=[(P["t1"][0], "t1_0")], t2s=[(P["t2"][0], "t2_0")], krbs=[(P["krb"][0], "krb_0"), (P["krb"][1], "krb_1")])
                if PHA >= 3:
                    rope_pipeline([(1024 + kvh * 64, 1.0, kaug[0:64, kvh, gcols], "kaug_%d_%d" % (g, kvh)) for kvh in range(4)],
                                  wqkv, lambda kc: XT[gs][:, kc, :], xtk, 512, ctt[gs][:, :], stt[gs][:, :], "ct0", "st0", **bufsA)
                for sub in range(4 if PHA >= 4 else 0):
                    mm(trk, psV[:, 0:256], [(XT[gs][:, kc, sub * 128:(sub + 1) * 128], wqkv[:, kc, 1280:1536]) for kc in range(8)],
                       xtk + ["wqkv"], ["psV"])
                    trk.op("act", lambda a: a.activation(out=vaug[:, g * 4 + sub, :, 0:64],
                                                         in_=psV[:, 0:256].rearrange("p (h d) -> p h d", h=4), func=AF.Copy),
                           ["psV", "vaug_init"], ["vaug_%d" % (g * 4 + sub)])
                if PHA < 5:
                    continue
                trk.op("dve", lambda v: v.tensor_reduce(out=kms[:, :, 2 * g:2 * g + 2],
                                                        in_=kaug[0:64, :, gcols].rearrange("p h (b c) -> p h b c", b=2),
                                                        axis=AX.X, op=ALU.add),
                       ["kaug_%d_%d" % (g, k_) for k_ in range(4)] + ["kms"], ["kms"])
                trk.op("dve", lambda v: v.tensor_copy(out=kmb[:, :, :], in_=kms[:, :, :]), ["kms"], ["kmb"])
                if g < 7 or PHA < 7:
                    continue
                subs = [3] if g == 7 else [0, 1, 2, 3]
                c0q = subs[0] * 128
                ncols = len(subs) * 128
                qs = 0
                rope_pipeline([(h * 64, 0.125, qa[qs][0:64, subs[0]:subs[-1] + 1, h, :], "qa%d_q" % qs) for h in range(16)],
                              wqkv, lambda kc: XT[gs][:, kc, c0q:512], xtk, ncols, ctt[gs][:, c0q:512], stt[gs][:, c0q:512],
                              "ct0", "st0", **bufsA)
                for sub in (subs if PHA >= 8 else []):
                    et = g * 4 + sub
                    jj = et // 2 - 15
                    def gate_mm(pe):
                        ins = None
                        for h in range(16):
                            ins = pe.matmul(psG[:, h * 32:(h + 1) * 32], qa[qs][0:64, sub, h, :], kmb[0:64, h // 4, :],
                                            start=True, stop=True)
                        return ins
                    trk.op("pe", gate_mm, ["qa%d_q" % qs, "kmb"], ["psG"])
                    trk.op("dve", lambda v: v.tensor_tensor(out=gb[:, :, :], in0=psG[:, :].rearrange("p (h n) -> p h n", h=16),
                                                            in1=gbias_sb[:, jj, :].unsqueeze(1).broadcast_to([128, 16, 32]), op=ALU.add),
                           ["psG", "gbias"], ["gb"])
                    for h in range(16):
                        trk.op("dve", lambda v: v.max(out=m8[:, h, :], in_=gb[:, h, :]), ["gb"], ["m8_%d" % h])
                    for h in range(16):
                        trk.op("dve", lambda v: v.tensor_scalar(out=sel[:, h, :], in0=gb[:, h, :], scalar1=m8[:, h, 2:3], scalar2=-NEG,
                                                                op0=ALU.is_ge, op1=ALU.mult), ["gb", "m8_%d" % h], ["sel_%d" % h])
                    trk.op("dve", lambda v: v.tensor_tensor(out=stage[:, :, 64:96], in0=sel[:, :, :],
                                                            in1=cb_sb[:, jj, :].unsqueeze(1).broadcast_to([128, 16, 32]), op=ALU.add),
                           ["sel_%d" % h for h in range(16)] + ["cb", "stage"], ["stage"])
                    for rnd in range(2):
                        transposes(trk, [(psM[:, hh, :], stage[:, rnd * 8 + hh, :]) for hh in range(8)], identb[:, :],
                                   ["stage", "identb"], ["psM"])
                        trk.op("act", lambda a: a.activation(out=qa[qs][64:96, sub, rnd * 8:(rnd + 1) * 8, :], in_=psM[64:96, :, :], func=AF.Copy),
                               ["psM"], ["qa%d_m%d_%d" % (qs, sub, rnd)])
                qkeys = ["qa%d_q" % qs] + (["qa%d_m%d_%d" % (qs, s_, r_) for s_ in subs for r_ in range(2)] if PHA >= 8 else [])
                if g == 7:
                    trk.dma("sp", QA[:, 0, :], qa[qs][:, 3, :, :].rearrange("p h c -> p (h c)"), qkeys, ["QA"])
                else:
                    t0 = 1 + 4 * (g - 8)
                    trk.dma("sp", QA[:, t0:t0 + 4, :], qa[qs][:, :, :, :].rearrange("p s h c -> p s (h c)"), qkeys, ["QA"])
            done()
            if debug:
                dk = nc.dram_tensor("dbg_kaug", [96, 4, SEQ], BF16, kind="ExternalOutput").ap()
                dv_ = nc.dram_tensor("dbg_vaug", [128, 64, 4, 128], BF16, kind="ExternalOutput").ap()
                trk.dma("sp", dk[:, :, :], kaug[:, :, :], [], ["dbg_k"])
                trk.dma("sp", dv_[:, :, :, :], vaug[:, :, :, :], [], ["dbg_v"])
                done()
        if stop_after == 1:
            return nc, es, trk
        with ExitStack() as ph:
            wo = sb(ph, "wo", [128, 8, D], BF16)
            load_w("pool", wo, w_o[0], "wo", nsplit=2)
            lng = sb(ph, "lng", [128, D], F32)
            lnb = sb(ph, "lnb", [128, D], F32)
            trk.dma("sp", lng[:, :], ln_mix_g[0:1, :].partition_broadcast(128), [], ["lng"])
            trk.dma("sp", lnb[:, :], ln_mix_b[0:1, :].partition_broadcast(128), [], ["lnb"])
            qt = [sb(ph, "qt%d" % i, [96, 2048], BF16) for i in range(2)]
            PT = [sb(ph, "PT%d" % i, [128, 512], BF16) for i in range(3)]
            rden = [sb(ph, "rden%d" % i, [128, 512], F32) for i in range(2)]
            aT = [sb(ph, "aT%d" % i, [128, 8, 128], BF16) for i in range(2)]
            xf = [sb(ph, "xf%d" % i, [128, D], F32) for i in range(2)]
            yb = [sb(ph, "y%d" % i, [128, D], F32) for i in range(2)]
            x1b = [sb(ph, "x1b%d" % i, [128, D], BF16) for i in range(2)]
            x1T = [sb(ph, "x1T%d" % i, [128, 8, 128], BF16) for i in range(2)]
            LNP = {"st6": [sb(ph, "st6_%d" % i, [128, 12], F32) for i in range(2)],
                   "mv": [sb(ph, "mv_%d" % i, [128, 4], F32) for i in range(2)]}
            psS = [ps(ph, "psS%d" % i, [128, 512]) for i in range(3)]
            psO = [ps(ph, "psO%d" % i, [128, 512]) for i in range(2)]
            psW = ps(ph, "psW", [128, 2, 512])
            psT2 = ps(ph, "psT2", [128, 1024], BF16)
            cnt = {"s": 0, "p": 0, "o": 0}
            if debug:
                dbg_aT = nc.dram_tensor("dbg_aT", [NT, 128, 8, 128], BF16, kind="ExternalOutput").ap()
                dbg_rd = nc.dram_tensor("dbg_rd", [NT, 4, 128, 512], F32, kind="ExternalOutput").ap()

            def attention(ti):
                sl = ti % 2
                et = 31 + ti
                J, s_ = et // 2, et % 2
                trk.dma("sp", qt[sl][:, :], QA[:, ti, :], ["QA"], ["qt%d" % sl])
                r0 = 3968 + 128 * ti
                trk.dma("sp", xf[sl][:, :], xin[r0:r0 + 128, :], [], ["xf%d" % sl])
                ktiles = [(kt, False) for kt in range(2 * J)]
                if s_ == 0:
                    ktiles.append((2 * J, True))
                else:
                    ktiles.append((2 * J, False))
                    ktiles.append((2 * J + 1, True))
                steps = []
                for kvh in range(4):
                    so = cnt["o"] % 2
                    cnt["o"] += 1
                    n = len(ktiles)
                    for idx, (kt, diag) in enumerate(ktiles):
                        ss = cnt["s"] % 3
                        cnt["s"] += 1
                        pp = cnt["p"] % 3
                        cnt["p"] += 1
                        steps.append((kvh, so, idx, n, kt, diag, ss, pp))

                def S(j):
                    kvh, so, idx, n, kt, diag, ss, pp = steps[j]
                    mm(trk, psS[ss][:, :], [(kaug[0:96, kvh, kt * 128:(kt + 1) * 128], qt[sl][0:96, kvh * 512:(kvh + 1) * 512])],
                       ["qt%d" % sl], ["psS%d" % ss])

                def E(j):
                    kvh, so, idx, n, kt, diag, ss, pp = steps[j]
                    trk.op("act", lambda a: a.activation(out=PT[pp][:, :], in_=psS[ss][:, :], func=AF.Exp),
                           ["psS%d" % ss], ["PT%d" % pp])
                    if diag:
                        trk.op("dve", lambda v: v.tensor_tensor(out=PT[pp][:, :].rearrange("p (h q) -> p h q", h=4),
                                                                in0=PT[pp][:, :].rearrange("p (h q) -> p h q", h=4),
                                                                in1=trib[:, :].unsqueeze(1).broadcast_to([128, 4, 128]), op=ALU.mult),
                               ["PT%d" % pp, "trib"], ["PT%d" % pp])

                def PV(j):
                    kvh, so, idx, n, kt, diag, ss, pp = steps[j]
                    def pv(pe):
                        return pe.matmul(psO[so][:, :], vaug[:, kt, kvh, :], PT[pp][:, :], start=(idx == 0), stop=(idx == n - 1))
                    trk.op("pe", pv, ["PT%d" % pp], ["psO%d" % so])
                    if idx != n - 1:
                        return
                    rs = so
                    trk.op("dve", lambda v: v.reciprocal(out=rden[rs][64:128, :], in_=psO[so][64:128, :]), ["psO%d" % so], ["rden%d" % rs])
                    if debug:
                        trk.dma("sp", dbg_rd[ti, kvh, 64:128, :], rden[rs][64:128, :], ["rden%d" % rs], ["dbg_rd"])
                    for i in range(2):
                        pb = i * 64
                        trk.op("dve", lambda v: v.tensor_tensor(out=aT[sl][pb:pb + 64, kvh * 2:kvh * 2 + 2, :],
                                                                in0=psO[so][0:64, :].rearrange("p (i q) -> p i q", i=4)[:, i::2, :],
                                                                in1=rden[rs][64:128, :].rearrange("p (i q) -> p i q", i=4)[:, i::2, :], op=ALU.mult),
                               ["psO%d" % so, "rden%d" % rs], ["aT%d_%d_%d" % (sl, kvh, i)])

                N = len(steps)
                S(0)
                if N > 1:
                    S(1)
                for j in range(N):
                    E(j)
                    if j + 2 < N:
                        S(j + 2)
                    PV(j)


            def tail(ti):
                sl = ti % 2
                akeys = ["aT%d_%d_%d" % (sl, k_, i_) for k_ in range(4) for i_ in range(2)]
                if debug:
                    trk.dma("sp", dbg_aT[ti], aT[sl][:, :, :], akeys, ["dbg_aT"])
                for half in range(2):
                    mm(trk, psW[:, half, :], [(aT[sl][:, c, :], wo[:, c, half * 512:(half + 1) * 512]) for c in range(8)],
                       akeys + ["wo"], ["psW"])
                for half in range(2):
                    trk.op("dve", lambda v: v.scalar_tensor_tensor(out=yb[sl][:, half * 512:(half + 1) * 512],
                                                                   in0=xf[sl][:, half * 512:(half + 1) * 512], scalar=ALPHA,
                                                                   in1=psW[:, half, :], op0=ALU.mult, op1=ALU.add),
                           ["xf%d" % sl, "psW"], ["y%d" % sl])
                layer_norm(trk, LNP, yb[sl], "y%d" % sl, lng, lnb, yb[sl], "y%d" % sl, sl)
                trk.dma("sp", X1[ti * 128:(ti + 1) * 128, :], yb[sl][:, :], ["y%d" % sl], ["X1"])
                trk.op("act", lambda a: a.activation(out=x1b[sl][:, :], in_=yb[sl][:, :], func=AF.Copy), ["y%d" % sl], ["x1b%d" % sl])
                transposes(trk, [(psT2[:, kc * 128:(kc + 1) * 128], x1b[sl][:, kc * 128:(kc + 1) * 128]) for kc in range(8)],
                           identb[:, :], ["x1b%d" % sl, "identb"], ["psT2"])
                trk.op("act", lambda a: a.activation(out=x1T[sl][:, :, :], in_=psT2[:, :].rearrange("p (k c) -> p k c", k=8), func=AF.Copy),
                       ["psT2"], ["x1T%d" % sl])
                trk.dma("sp", X1T[:, :, ti * 128:(ti + 1) * 128], x1T[sl][:, :, :], ["x1T%d" % sl], ["X1T"])

            NTB = int(os.environ.get("NTB", str(NT)))
            for ti in range(NTB):
                attention(ti)
                if ti >= 1:
                    tail(ti - 1)
            tail(NTB - 1)
            done()
    if stop_after == 2:
        return nc, es, trk

    def ffn_layer(layer, XTd, Xd, pd, chunks, experts, use_gates, Yd, YTd):
        for (t0, nt) in chunks:
            with ExitStack() as ck:
                ncol = nt * 128
                XTc = sb(ck, "XTc", [128, 8, ncol], BF16)
                acc = sb(ck, "acc", [128, nt, D], F32)
                for kc in range(8):
                    trk.dma("sp", XTc[:, kc, :], XTd[:, kc, t0 * 128:t0 * 128 + ncol], [], ["XTc"], sk=("d", "XTc"))
                for t in range(nt):
                    r0 = (t0 + t) * 128
                    trk.dma("sp", acc[:, t, :], Xd[r0:r0 + 128, :], [], ["accld"], sk=("d", "accld"))
                for t in range(nt):
                    trk.op("act", lambda a: a.mul(out=acc[:, t, :], in_=acc[:, t, :], mul=ALPHA), ["accld"], ["acc%d" % t])
                if use_gates:
                    gt = sb(ck, "gt", [128, nt, 8], F32)
                    trk.dma("sp", gt[:, :, :], GATES[:, t0:t0 + nt, :], [], ["gt"])
                with ExitStack() as ph:
                    wg = [sb(ph, "wg%d" % i, [128, 8, 512], BF16) for i in range(2)]
                    wu = [sb(ph, "wu%d" % i, [128, 8, 512], BF16) for i in range(2)]
                    wd = [sb(ph, "wd%d" % i, [128, 4, D], BF16) for i in range(2)]
                    sg = [sb(ph, "sg%d" % i, [128, 512], F32) for i in range(2)]
                    hh = [sb(ph, "hh%d" % i, [128, 4, 512], BF16) for i in range(2)]
                    psG = [ps(ph, "psG%d" % i, [128, 512]) for i in range(2)]
                    psU = [ps(ph, "psU%d" % i, [128, 512]) for i in range(2)]
                    psY = [ps(ph, "psY%d" % i, [128, 2, 512]) for i in range(2)]
                    steps = [(e, fb) for e in range(len(experts)) for fb in range(NFB)]

                    def load(i):
                        e, fb = steps[i]
                        sl = i % 2
                        wg_ap, wu_ap, wd_ap = experts[e]
                        fs = slice(fb * 512, (fb + 1) * 512)
                        trk.dma("pool", wg[sl][:, :, :], wg_ap.rearrange("(k p) n -> p k n", p=128)[:, :, fs], [], ["wg%d" % sl])
                        trk.dma("pool", wu[sl][:, :, :], wu_ap.rearrange("(k p) n -> p k n", p=128)[:, :, fs], [], ["wu%d" % sl])
                        trk.dma("pool", wd[sl][:, :, :], wd_ap[fs, :].rearrange("(c p) n -> p c n", p=128), [], ["wd%d" % sl])

                    groups = []
                    tt = 0
                    while tt < nt:
                        gn = min(4, nt - tt)
                        groups.append((tt, gn))
                        tt += gn
                    c1 = 0
                    c2 = 0
                    c3 = 0
                    load(0)
                    for i, (e, fb) in enumerate(steps):
                        if i + 1 < len(steps):
                            load(i + 1)
                        sl = i % 2
                        for (g0, gn) in groups:
                            cols = slice(g0 * 128, (g0 + gn) * 128)
                            ncl = gn * 128
                            hs = c2 % 2
                            c2 += 1
                            for fc in range(4):
                                a_ = c1 % 2
                                c1 += 1
                                fsl = slice(fc * 128, (fc + 1) * 128)
                                mm(trk, psG[a_][:, 0:ncl], [(wg[sl][:, kc, fsl], XTc[:, kc, cols]) for kc in range(8)],
                                   ["wg%d" % sl, "XTc"], ["psG%d" % a_])
                                mm(trk, psU[a_][:, 0:ncl], [(wu[sl][:, kc, fsl], XTc[:, kc, cols]) for kc in range(8)],
                                   ["wu%d" % sl, "XTc"], ["psU%d" % a_])
                                trk.op("act", lambda a: a.activation(out=sg[a_][:, 0:ncl], in_=psG[a_][:, 0:ncl], func=AF.Silu),
                                       ["psG%d" % a_], ["sg%d" % a_])
                                trk.op("dve", lambda v: v.tensor_tensor(out=hh[hs][:, fc, 0:ncl], in0=sg[a_][:, 0:ncl], in1=psU[a_][:, 0:ncl],
                                                                        op=ALU.mult), ["sg%d" % a_, "psU%d" % a_], ["hh%d_%d" % (hs, fc)])
                            hkeys = ["hh%d_%d" % (hs, fc) for fc in range(4)]
                            for tl in range(gn):
                                t = g0 + tl
                                ys = c3 % 2
                                c3 += 1
                                for half in range(2):
                                    mm(trk, psY[ys][:, half, :], [(hh[hs][:, fc, tl * 128:(tl + 1) * 128], wd[sl][:, fc, half * 512:(half + 1) * 512])
                                                                 for fc in range(4)], hkeys + ["wd%d" % sl], ["psY%d" % ys])
                                for half in range(2):
                                    hsl = slice(half * 512, (half + 1) * 512)
                                    if use_gates:
                                        trk.op("dve", lambda v: v.scalar_tensor_tensor(out=acc[:, t, hsl], in0=psY[ys][:, half, :],
                                                                                       scalar=gt[:, t, e:e + 1], in1=acc[:, t, hsl],
                                                                                       op0=ALU.mult, op1=ALU.add),
                                               ["psY%d" % ys, "acc%d" % t, "gt"], ["acc%d" % t])
                                    else:
                                        trk.op("dve", lambda v: v.tensor_tensor(out=acc[:, t, hsl], in0=psY[ys][:, half, :], in1=acc[:, t, hsl],
                                                                                op=ALU.add), ["psY%d" % ys, "acc%d" % t], ["acc%d" % t])
                    done()
                with ExitStack() as ph:
                    wpg = sb(ph, "wpg", [128, 8, D], BF16)
                    wpp = sb(ph, "wpp", [128, 2, D], BF16)
                    load_w("pool", wpg, w_ple_gate[layer], "wpg", nsplit=2)
                    load_w("pool", wpp, w_ple_proj[layer], "wpp")
                    lng = sb(ph, "lng", [128, D], F32)
                    lnb = sb(ph, "lnb", [128, D], F32)
                    trk.dma("sp", lng[:, :], ln_ffn_g[layer:layer + 1, :].partition_broadcast(128), [], ["lng"])
                    trk.dma("sp", lnb[:, :], ln_ffn_b[layer:layer + 1, :].partition_broadcast(128), [], ["lnb"])
                    x2b = [sb(ph, "x2b%d" % i, [128, D], BF16) for i in range(2)]
                    x2T = [sb(ph, "x2T%d" % i, [128, 8, 128], BF16) for i in range(2)]
                    pb = [sb(ph, "pb%d" % i, [128, 256], BF16) for i in range(2)]
                    pT = [sb(ph, "pT%d" % i, [128, 2, 128], BF16) for i in range(2)]
                    sig = [sb(ph, "sig%d" % i, [128, 2, 512], F32) for i in range(2)]
                    ob = [sb(ph, "ob%d" % i, [128, D], BF16) for i in range(2)]
                    oT = [sb(ph, "oT%d" % i, [128, 8, 128], BF16) for i in range(2)]
                    LNP = {"st6": [sb(ph, "st6_%d" % i, [128, 12], F32) for i in range(2)],
                           "mv": [sb(ph, "mv_%d" % i, [128, 4], F32) for i in range(2)]}
                    psT = ps(ph, "psT", [128, 1024], BF16)
                    psA = ps(ph, "psA", [128, 2, 512])
                    psB = ps(ph, "psB", [128, 2, 512])
                    psTp = ps(ph, "psTp", [128, 1024], BF16)
                    psT3 = ps(ph, "psT3", [128, 1024], BF16)
                    for t in range(nt):
                        sl = t % 2
                        r0 = (t0 + t) * 128
                        ak = "acc%d" % t
                        x2 = acc[:, t, :]
                        trk.dma("pool", pb[sl][:, :], pd[r0:r0 + 128, :], [], ["pb%d" % sl])
                        layer_norm(trk, LNP, x2, ak, lng, lnb, x2, ak, sl)
                        trk.op("act", lambda a: a.activation(out=x2b[sl][:, :], in_=x2, func=AF.Copy), [ak], ["x2b%d" % sl])
                        transposes(trk, [(psT[:, kc * 128:(kc + 1) * 128], x2b[sl][:, kc * 128:(kc + 1) * 128]) for kc in range(8)],
                                   identb[:, :], ["x2b%d" % sl, "identb"], ["psT"])
                        trk.op("act", lambda a: a.activation(out=x2T[sl][:, :, :], in_=psT[:, :].rearrange("p (k c) -> p k c", k=8), func=AF.Copy),
                               ["psT"], ["x2T%d" % sl])
                        for half in range(2):
                            mm(trk, psA[:, half, :], [(x2T[sl][:, kc, :], wpg[:, kc, half * 512:(half + 1) * 512]) for kc in range(8)],
                               ["x2T%d" % sl, "wpg"], ["psA"])
                        trk.op("act", lambda a: a.activation(out=sig[sl][:, :, :], in_=psA[:, :, :], func=AF.Sigmoid), ["psA"], ["sig%d" % sl])
                        transposes(trk, [(psTp[:, c * 128:(c + 1) * 128], pb[sl][:, c * 128:(c + 1) * 128]) for c in range(2)],
                                   identb[:, :], ["pb%d" % sl, "identb"], ["psTp"])
                        trk.op("act", lambda a: a.activation(out=pT[sl][:, :, :], in_=psTp[:, 0:256].rearrange("p (k c) -> p k c", k=2), func=AF.Copy),
                               ["psTp"], ["pT%d" % sl])
                        for half in range(2):
                            mm(trk, psB[:, half, :], [(pT[sl][:, c, :], wpp[:, c, half * 512:(half + 1) * 512]) for c in range(2)],
                               ["pT%d" % sl, "wpp"], ["psB"])
                        trk.op("dve", lambda v: v.tensor_tensor(out=sig[sl][:, :, :], in0=psB[:, :, :], in1=sig[sl][:, :, :], op=ALU.mult),
                               ["psB", "sig%d" % sl], ["sig%d" % sl])
                        trk.op("dve", lambda v: v.tensor_tensor(out=x2, in0=x2, in1=sig[sl][:, :, :].rearrange("p a b -> p (a b)"), op=ALU.add),
                               [ak, "sig%d" % sl], [ak])
                        okey = "out" if Yd is out_d else "Yd%d" % layer
                        trk.dma("sp", Yd[r0:r0 + 128, :], x2, [ak], [okey])
                        if YTd is not None:
                            trk.op("act", lambda a: a.activation(out=ob[sl][:, :], in_=x2, func=AF.Copy), [ak], ["ob%d" % sl])
                            transposes(trk, [(psT3[:, kc * 128:(kc + 1) * 128], ob[sl][:, kc * 128:(kc + 1) * 128]) for kc in range(8)],
                                       identb[:, :], ["ob%d" % sl, "identb"], ["psT3"])
                            trk.op("act", lambda a: a.activation(out=oT[sl][:, :, :], in_=psT3[:, :].rearrange("p (k c) -> p k c", k=8), func=AF.Copy),
                                   ["psT3"], ["oT%d" % sl])
                            trk.dma("sp", YTd[:, :, r0:r0 + 128], oT[sl][:, :, :], ["oT%d" % sl], ["YTd%d" % layer])
                    done()

    ffn_layer(0, X1T, X1, p0, [(0, 17), (17, 16)], [(w_ffn_gate[0], w_ffn_up[0], w_ffn_down[0])], False, X3, X3T)
    if stop_after == 3:
        return nc, es, trk

    with ExitStack() as kv1:
        k1 = sb(kv1, "k1", [64, 4, NTOK], BF16)
        v1 = sb(kv1, "v1", [128, NT, 4, 128], BF16)
        with ExitStack() as ph:
            wqkv = sb(ph, "wqkv1", [128, 8, 1536], BF16)
            load_w("pool", wqkv, w_qkv[1], "wqkv", nsplit=4)
            trk.op("pool", lambda g: g.memset(v1[:, :, :, :], 1.0), [], ["v1_init"])
            XT = [sb(ph, "XTd%d" % i, [128, 8, 512], BF16) for i in range(2)]
            ctt = [sb(ph, "ctd%d" % i, [64, 512], F32) for i in range(2)]
            stt = [sb(ph, "std%d" % i, [64, 512], F32) for i in range(2)]
            t1s = [sb(ph, "t1d%d" % i, [64, 512], F32) for i in range(2)]
            t2s = [sb(ph, "t2d%d" % i, [64, 512], F32) for i in range(2)]
            krbs = [sb(ph, "krbd%d" % i, [64, 512], BF16) for i in range(2)]
            qg = [sb(ph, "qg%d" % i, [64, 4, 16, 128], BF16) for i in range(2)]
            psK = [ps(ph, "psK%d" % i, [64, 512]) for i in range(2)]
            psPp = [ps(ph, "psP%d" % i, [64, 512]) for i in range(2)]
            psV = [ps(ph, "psV%d" % i, [128, 512]) for i in range(2)]
            ki = 0
            vi = 0
            groups = [(0, 1)] + [(1 + 4 * i, 4) for i in range(8)]
            for gi, (t0, gn) in enumerate(groups):
                gs = gi % 2
                ncl = gn * 128
                cols = slice(t0 * 128, t0 * 128 + ncl)
                ecols = slice(3968 + t0 * 128, 3968 + t0 * 128 + ncl)
                for kc in range(8):
                    trk.dma("sp", XT[gs][:, kc, 0:ncl], X3T[:, kc, cols], [], ["XT%d" % gs], sk=("d", "XTd%d" % gs))
                trk.dma("sp", ctt[gs][:, 0:ncl], CT[:, ecols], [], ["ct%d" % gs])
                trk.dma("sp", stt[gs][:, 0:ncl], ST[:, ecols], [], ["st%d" % gs])

                def proj_rope(c0, scale, out_ap, outk):
                    nonlocal ki
                    s2 = ki % 2
                    ki += 1
                    t1 = t1s[s2]; t2 = t2s[s2]; krb = krbs[s2]; psP = psPp[s2]
                    mm(trk, psK[s2][:, 0:ncl], [(wqkv[:, kc, c0:c0 + 64], XT[gs][:, kc, 0:ncl]) for kc in range(8)],
                       ["XT%d" % gs, "wqkv"], ["psK%d" % s2])
                    trk.op("dve", lambda v: v.scalar_tensor_tensor(out=t1[:, 0:ncl], in0=psK[s2][:, 0:ncl], scalar=scale,
                                                                   in1=ctt[gs][:, 0:ncl], op0=ALU.mult, op1=ALU.mult),
                           ["psK%d" % s2, "ct%d" % gs], ["t1_%d" % s2])
                    trk.op("act", lambda a: a.activation(out=krb[:, 0:ncl], in_=psK[s2][:, 0:ncl], func=AF.Copy, scale=scale),
                           ["psK%d" % s2], ["krb_%d" % s2])
                    mm(trk, psP[:, 0:ncl], [(pm[:, :], krb[:, 0:ncl])], ["krb_%d" % s2, "pm"], ["psP%d" % s2])
                    trk.op("dve", lambda v: v.tensor_tensor(out=t2[:, 0:ncl], in0=psP[:, 0:ncl], in1=stt[gs][:, 0:ncl], op=ALU.mult),
                           ["psP%d" % s2, "st%d" % gs], ["t2_%d" % s2])
                    if len(out_ap.shape) == 2:
                        trk.op("dve", lambda v: v.tensor_tensor(out=out_ap, in0=t1[:, 0:ncl], in1=t2[:, 0:ncl], op=ALU.add),
                               ["t1_%d" % s2, "t2_%d" % s2], [outk])
                    else:
                        trk.op("dve", lambda v: v.tensor_tensor(out=out_ap, in0=t1[:, 0:ncl].rearrange("p (s c) -> p s c", c=128),
                                                                in1=t2[:, 0:ncl].rearrange("p (s c) -> p s c", c=128), op=ALU.add),
                               ["t1_%d" % s2, "t2_%d" % s2], [outk])

                bufsD = dict(psKs=[(psK[0], "psK0"), (psK[1], "psK1")], psPs=[(psPp[0], "psP0"), (psPp[1], "psP1")],
                             t1s=[(t1s[0], "t1_0"), (t1s[1], "t1_1")], t2s=[(t2s[0], "t2_0"), (t2s[1], "t2_1")],
                             krbs=[(krbs[0], "krb_0"), (krbs[1], "krb_1")])
                rope_pipeline([(1024 + kvh * 64, 1.0, k1[0:64, kvh, cols], "k1_%d_%d" % (gi, kvh)) for kvh in range(4)],
                              wqkv, lambda kc: XT[gs][:, kc, 0:ncl], ["XT%d" % gs], ncl, ctt[gs][:, 0:ncl], stt[gs][:, 0:ncl],
                              "ct%d" % gs, "st%d" % gs, **bufsD)
                for tl in range(gn):
                    vs = vi % 2
                    vi += 1
                    mm(trk, psV[vs][:, 0:256], [(XT[gs][:, kc, tl * 128:(tl + 1) * 128], wqkv[:, kc, 1280:1536]) for kc in range(8)],
                       ["XT%d" % gs, "wqkv"], ["psV%d" % vs])
                    trk.op("act", lambda a: a.activation(out=v1[:, t0 + tl, :, 0:64], in_=psV[vs][:, 0:256].rearrange("p (h d) -> p h d", h=4),
                                                         func=AF.Copy), ["psV%d" % vs, "v1_init"], ["v1_%d" % (t0 + tl)])
                if t0 == 0:
                    continue
                qs = gi % 2
                rope_pipeline([(h * 64, 0.125, qg[qs][0:64, :, h, :], "qg%d" % qs) for h in range(16)],
                              wqkv, lambda kc: XT[gs][:, kc, 0:ncl], ["XT%d" % gs], ncl, ctt[gs][:, 0:ncl], stt[gs][:, 0:ncl],
                              "ct%d" % gs, "st%d" % gs, **bufsD)
                trk.dma("sp", Q1[:, t0 - 1:t0 + 3, :], qg[qs][:, :, :, :].rearrange("p s h c -> p s (h c)"), ["qg%d" % qs], ["Q1"])
            done()
        if stop_after == 4:
            return nc, es, trk
        with ExitStack() as ph:
            wo = sb(ph, "wo1", [128, 8, D], BF16)
            load_w("pool", wo, w_o[1], "wo", nsplit=2)
            lng = sb(ph, "lng1", [128, D], F32)
            lnb = sb(ph, "lnb1", [128, D], F32)
            trk.dma("sp", lng[:, :], ln_mix_g[1:2, :].partition_broadcast(128), [], ["lng"])
            trk.dma("sp", lnb[:, :], ln_mix_b[1:2, :].partition_broadcast(128), [], ["lnb"])
            wr = sb(ph, "wr", [128, 8, 8], F32)
            trk.dma("sp", wr[:, :, :], w_router[0].rearrange("(k p) n -> p k n", p=128), [], ["wr"])
            esr = sb(ph, "esr", [128, 16], F32)
            esink = sb(ph, "esink", [128, 16], F32)
            trk.dma("sp", esr[:, :], sinks[0:1, :].partition_broadcast(128), [], ["esr"])
            trk.op("act", lambda a: a.activation(out=esink[:, :], in_=esr[:, :], func=AF.Exp), ["esr"], ["esink"])
            esink512 = sb(ph, "esink512", [128, 16, 128], F32)
            trk.op("dve", lambda v: v.tensor_copy(out=esink512[:, :, :], in_=esink[:, :].unsqueeze(2).broadcast_to([128, 16, 128])),
                   ["esink"], ["esink512"])
            qt = [sb(ph, "qt1_%d" % i, [64, 2048], BF16) for i in range(2)]
            PT = [sb(ph, "PT1_%d" % i, [128, 512], BF16) for i in range(4)]
            den = [sb(ph, "den%d" % i, [128, 512], F32) for i in range(2)]
            rden = [sb(ph, "rden1_%d" % i, [128, 512], F32) for i in range(2)]
            aT = [sb(ph, "aT1_%d" % i, [128, 8, 128], BF16) for i in range(2)]
            xf = [sb(ph, "xf1_%d" % i, [128, D], F32) for i in range(2)]
            yb = [sb(ph, "y1_%d" % i, [128, D], F32) for i in range(2)]
            x4b = [sb(ph, "x4b%d" % i, [128, D], BF16) for i in range(2)]
            x4T = [sb(ph, "x4T%d" % i, [128, 8, 128], BF16) for i in range(2)]
            x4T32 = sb(ph, "x4T32", [128, 8, 128], F32)
            lg = sb(ph, "lg", [128, 8], F32)
            m8 = sb(ph, "m8r", [128, 8], F32)
            d8 = sb(ph, "d8", [128, 8], F32)
            e8 = sb(ph, "e8", [128, 8], F32)
            msk = sb(ph, "msk", [128, 8], F32)
            ss = sb(ph, "ss", [128, 2], F32)
            gtl = [sb(ph, "gtl%d" % i, [128, 8], F32) for i in range(2)]
            LNP = {"st6": [sb(ph, "st6d_%d" % i, [128, 12], F32) for i in range(2)],
                   "mv": [sb(ph, "mvd_%d" % i, [128, 4], F32) for i in range(2)]}
            psS = [ps(ph, "psS%d" % i, [128, 512]) for i in range(2)]
            psO = [ps(ph, "psO%d" % i, [128, 512]) for i in range(2)]
            psW = ps(ph, "psW", [128, 2, 512])
            psT2 = ps(ph, "psT2", [128, 1024], BF16)
            psL = ps(ph, "psL", [128, 512])
            cnt = {"s": 0, "p": 0, "o": 0}

            def attention1(ti):
                sl = ti % 2
                o_ = ti - 1
                trk.dma("sp", qt[sl][:, :], Q1[:, o_, :], [], ["qt%d" % sl])
                trk.dma("sp", xf[sl][:, :], X3[ti * 128:(ti + 1) * 128, :], [], ["xf%d" % sl])
                steps = []
                for kvh in range(4):
                    so = cnt["o"] % 2
                    cnt["o"] += 1
                    parts = [(ti - 1, tricfv if ti == 1 else tricb, "tricfv" if ti == 1 else "tricb"), (ti, trib, "trib")]
                    for idx, (kt, mask, mkey) in enumerate(parts):
                        ss_ = cnt["s"] % 2
                        cnt["s"] += 1
                        pp = cnt["p"] % 4
                        cnt["p"] += 1
                        steps.append((kvh, so, idx, kt, mask, mkey, ss_, pp))

                def S(j):
                    kvh, so, idx, kt, mask, mkey, ss_, pp = steps[j]
                    mm(trk, psS[ss_][:, :], [(k1[0:64, kvh, kt * 128:(kt + 1) * 128], qt[sl][0:64, kvh * 512:(kvh + 1) * 512])],
                       ["qt%d" % sl], ["psS%d" % ss_])

                def E(j):
                    kvh, so, idx, kt, mask, mkey, ss_, pp = steps[j]
                    trk.op("act", lambda a: a.activation(out=PT[pp][:, :], in_=psS[ss_][:, :], func=AF.Exp), ["psS%d" % ss_], ["PT%d" % pp])
                    trk.op("pool", lambda v: v.tensor_tensor(out=PT[pp][:, :].rearrange("p (h q) -> p h q", h=4),
                                                            in0=PT[pp][:, :].rearrange("p (h q) -> p h q", h=4),
                                                            in1=mask[:, :].unsqueeze(1).broadcast_to([128, 4, 128]), op=ALU.mult),
                           ["PT%d" % pp, mkey], ["PT%d" % pp])

                def PV(j):
                    kvh, so, idx, kt, mask, mkey, ss_, pp = steps[j]
                    def pv(pe):
                        return pe.matmul(psO[so][:, :], v1[:, kt, kvh, :], PT[pp][:, :], start=(idx == 0), stop=(idx == 1))
                    trk.op("pe", pv, ["PT%d" % pp], ["psO%d" % so])
                    if idx != 1:
                        return
                    rs = so
                    trk.op("dve", lambda v: v.tensor_tensor(out=den[rs][64:128, :], in0=psO[so][64:128, :],
                                                            in1=esink512[64:128, kvh * 4:(kvh + 1) * 4, :].rearrange("p h q -> p (h q)"), op=ALU.add),
                           ["psO%d" % so, "esink512"], ["den%d" % rs])
                    trk.op("dve", lambda v: v.reciprocal(out=rden[rs][64:128, :], in_=den[rs][64:128, :]), ["den%d" % rs], ["rden%d" % rs])
                    for i in range(2):
                        pb_ = i * 64
                        trk.op("dve", lambda v: v.tensor_tensor(out=aT[sl][pb_:pb_ + 64, kvh * 2:kvh * 2 + 2, :],
                                                                in0=psO[so][0:64, :].rearrange("p (i q) -> p i q", i=4)[:, i::2, :],
                                                                in1=rden[rs][64:128, :].rearrange("p (i q) -> p i q", i=4)[:, i::2, :], op=ALU.mult),
                               ["psO%d" % so, "rden%d" % rs], ["aT%d_%d_%d" % (sl, kvh, i)])

                N = len(steps)
                S(0)
                S(1)
                for j in range(N):
                    E(j)
                    if j + 2 < N:
                        S(j + 2)
                    PV(j)

            def tail1(ti):
                sl = ti % 2
                o_ = ti - 1
                akeys = ["aT%d_%d_%d" % (sl, k_, i_) for k_ in range(4) for i_ in range(2)]
                for half in range(2):
                    mm(trk, psW[:, half, :], [(aT[sl][:, c, :], wo[:, c, half * 512:(half + 1) * 512]) for c in range(8)],
                       akeys + ["wo"], ["psW"])
                for half in range(2):
                    trk.op("dve", lambda v: v.scalar_tensor_tensor(out=yb[sl][:, half * 512:(half + 1) * 512],
                                                                   in0=xf[sl][:, half * 512:(half + 1) * 512], scalar=ALPHA,
                                                                   in1=psW[:, half, :], op0=ALU.mult, op1=ALU.add),
                           ["xf%d" % sl, "psW"], ["y%d" % sl])
                layer_norm(trk, LNP, yb[sl], "y%d" % sl, lng, lnb, yb[sl], "y%d" % sl, sl)
                trk.dma("sp", X4[o_ * 128:(o_ + 1) * 128, :], yb[sl][:, :], ["y%d" % sl], ["X4"])
                trk.op("act", lambda a: a.activation(out=x4b[sl][:, :], in_=yb[sl][:, :], func=AF.Copy), ["y%d" % sl], ["x4b%d" % sl])
                transposes(trk, [(psT2[:, kc * 128:(kc + 1) * 128], x4b[sl][:, kc * 128:(kc + 1) * 128]) for kc in range(8)],
                           identb[:, :], ["x4b%d" % sl, "identb"], ["psT2"])
                trk.op("act", lambda a: a.activation(out=x4T[sl][:, :, :], in_=psT2[:, :].rearrange("p (k c) -> p k c", k=8), func=AF.Copy),
                       ["psT2"], ["x4T%d" % sl])
                trk.dma("sp", X4T[:, :, o_ * 128:(o_ + 1) * 128], x4T[sl][:, :, :], ["x4T%d" % sl], ["X4T"])
                transposes(trk, [(psW[:, kc // 4, (kc % 4) * 128:(kc % 4 + 1) * 128], yb[sl][:, kc * 128:(kc + 1) * 128]) for kc in range(8)],
                           identf[:, :], ["y%d" % sl, "identf"], ["psW"])
                trk.op("act", lambda a: a.activation(out=x4T32[:, :, :].rearrange("p (a b) c -> p a b c", a=2),
                                                     in_=psW[:, :, :].rearrange("p a (b c) -> p a b c", b=4), func=AF.Copy),
                       ["psW"], ["x4T32"])
                mm(trk, psL[:, 0:8], [(x4T32[:, kc, :], wr[:, kc, :]) for kc in range(8)], ["x4T32", "wr"], ["psL"])
                trk.op("dve", lambda v: v.tensor_copy(out=lg[:, :], in_=psL[:, 0:8]), ["psL"], ["lg"])
                trk.op("dve", lambda v: v.max(out=m8[:, :], in_=lg[:, :]), ["lg"], ["m8r"])
                trk.op("dve", lambda v: v.tensor_scalar(out=d8[:, :], in0=lg[:, :], scalar1=m8[:, 0:1], scalar2=None, op0=ALU.subtract),
                       ["lg", "m8r"], ["d8"])
                trk.op("act", lambda a: a.activation(out=e8[:, :], in_=d8[:, :], func=AF.Exp), ["d8"], ["e8"])
                trk.op("dve", lambda v: v.tensor_scalar(out=msk[:, :], in0=lg[:, :], scalar1=m8[:, 1:2], scalar2=None, op0=ALU.is_ge),
                       ["lg", "m8r"], ["msk"])
                trk.op("dve", lambda v: v.tensor_tensor(out=e8[:, :], in0=e8[:, :], in1=msk[:, :], op=ALU.mult), ["e8", "msk"], ["e8"])
                trk.op("dve", lambda v: v.tensor_reduce(out=ss[:, 0:1], in_=e8[:, :], axis=AX.X, op=ALU.add), ["e8"], ["ss0"])
                trk.op("dve", lambda v: v.reciprocal(out=ss[:, 1:2], in_=ss[:, 0:1]), ["ss0"], ["ss1"])
                trk.op("dve", lambda v: v.tensor_scalar(out=gtl[sl][:, :], in0=e8[:, :], scalar1=ss[:, 1:2], scalar2=None, op0=ALU.mult),
                       ["e8", "ss1"], ["gtl%d" % sl])
                trk.dma("sp", GATES[:, o_, :], gtl[sl][:, :], ["gtl%d" % sl], ["GATES"])

            for ti in range(1, NT):
                attention1(ti)
                if ti >= 2:
                    tail1(ti - 1)
            tail1(NT - 1)
            done()
    if stop_after == 5:
        return nc, es, trk

    experts = [(w_exp_gate[0, e], w_exp_up[0, e], w_exp_down[0, e]) for e in range(8)]
    ffn_layer(1, X4T, X4, p1, [(0, 16), (16, 16)], experts, True, out_d, None)
    return nc, es, trk


def _consts():
    d = np.arange(64)
    inv = np.zeros(64, np.float64)
    i = d % 8
    invf = 1.0 / (500000.0 ** (np.arange(0, 16, 2, dtype=np.float32) / np.float32(16)))
    inv[:16] = invf.astype(np.float64)[i[:16]]
    sgn = np.zeros(64); sgn[:8] = -1.0; sgn[8:16] = 1.0
    c64 = np.zeros((64, 4), np.float32)
    c64[:, 0] = (inv / (2 * np.pi)).astype(np.float32)
    c64[:, 1] = (sgn * 2 * np.pi).astype(np.float32)
    c64[:, 2] = sgn.astype(np.float32)
    pmat = np.zeros((64, 64), np.float32)
    for a in range(8):
        pmat[a + 8, a] = 1.0
        pmat[a, a + 8] = 1.0
    ident = np.eye(128, dtype=np.float32)
    blk = np.zeros((32, SEQ), np.float32)
    for n in range(32):
        blk[n, n * 256:(n + 1) * 256] = 1.0
    k = np.arange(128)[:, None]; q = np.arange(128)[None, :]
    tri = (q >= k).astype(np.float32)
    return c64, pmat, ident, blk, tri, (1.0 - tri).astype(np.float32)


def _core_inputs(inputs, c, consts, stop_after=None):
    b, h = c // 2, c % 2
    c64, pmat, ident, blk, tri, tric = consts
    x = inputs["x"][b]
    posb = inputs["positions"][b]
    lo = slice(0, HALF)
    own = slice(h * HALF, (h + 1) * HALF)
    xin = np.concatenate([x[lo], x[own]], axis=0)
    pos = np.concatenate([posb[lo], posb[own]])[None, :].astype(np.int32)
    p0e = np.concatenate([inputs["p"][0, b][lo], inputs["p"][0, b][own]], axis=0)[SEQ - NTOK:]
    p1 = inputs["p"][1, b][own]
    gbias = np.zeros((17, 32), np.float32)
    cb = np.zeros((17, 32), np.float32)
    for jj in range(17):
        J = 15 + jj
        for n in range(32):
            valid = (n < J) and (n >= 16 or h == 1)
            gbias[jj, n] = 0.0 if valid else -1e9
            cb[jj, n] = NEG if valid else 3 * NEG
        gbias[jj, J] = -2e9
        cb[jj, J] = 0.0
    m = {
        "xin": np.ascontiguousarray(xin), "pos": pos, "p0": np.ascontiguousarray(p0e), "p1": np.ascontiguousarray(p1),
        "cst64": c64, "pmat": pmat, "ident": ident, "blkind": blk, "tri": tri, "tric": tric,
        "gbias": gbias.reshape(1, -1), "cb": cb.reshape(1, -1), "fv": np.full((128, 1), float(h), np.float32),
    }
    for k in ("w_qkv", "w_o", "ln_mix_g", "ln_mix_b", "ln_ffn_g", "ln_ffn_b", "sinks", "w_ffn_gate", "w_ffn_up",
              "w_ffn_down", "w_router", "w_exp_gate", "w_exp_up", "w_exp_down", "w_ple_proj", "w_ple_gate"):
        if k.startswith("w_exp") and not (stop_after is None or stop_after >= 5):
            continue
        m[k] = np.ascontiguousarray(inputs[k], dtype=np.float32)
    return m


def run(inputs, stop_after=None, debug=False, cores=8, trace=False):
    inputs = {k: np.asarray(v) for k, v in inputs.items()}
    consts = _consts()
    nc, es, trk = build_program(stop_after=stop_after, debug=debug)
    in_maps = [_core_inputs(inputs, c, consts, stop_after) for c in range(cores)]
    res = run_bass_kernel_spmd(nc, in_maps, core_ids=list(range(cores)), trace=trace)
    es.close()
    return res


def kernel(**inputs):
    res = run(inputs)
    out = np.zeros((4, SEQ, D), np.float32)
    for c in range(8):
        b, h = c // 2, c % 2
        out[b, h * HALF:(h + 1) * HALF] = res.results[c]["out"]
    return out
```
